# Optimizing a Trainium2 kernel written in Bass

```python
import math
import jax, jax.numpy as jnp
from jax import lax
import numpy as np

D_MODEL = 1024
BATCH = 8
SEQ = 4096
DEPTH = 1

CONV_CH = D_MODEL // 2
CONV_WIDTH = 31
DA_HEADS = 4
DA_HEAD_DIM = 64
DA_V_DIM = 2 * DA_HEAD_DIM
ATTN_W = DA_HEADS * DA_V_DIM
N_BRANCH = 2
COL_CONV = 2 * CONV_CH
COL_Q = DA_HEADS * 2 * DA_HEAD_DIM
COL_K = DA_HEADS * 2 * DA_HEAD_DIM
COL_V = ATTN_W
COL_GATE = N_BRANCH * D_MODEL
IN_COLS = COL_CONV + COL_Q + COL_K + COL_V + COL_GATE
N_GROUPS = 4
EXPERTS_PER_GROUP = 8
N_EXPERTS = N_GROUPS * EXPERTS_PER_GROUP
TOP_K_IN_GROUP = 2
D_EXPERT = D_MODEL // 2

Q_BLOCK = 128
ROW_BLOCK = 128
EPS = 1e-6

kernel_name = "hybrid_conv_diffattn_hmoe_block"


def rms_norm(x, g):
    xf = x.astype(jnp.float32)
    y = xf * lax.rsqrt(jnp.mean(xf * xf, axis=-1, keepdims=True) + EPS)
    return (y * g.astype(jnp.float32)).astype(x.dtype)


def layer_norm(x, g, b):
    xf = x.astype(jnp.float32)
    mu = jnp.mean(xf, axis=-1, keepdims=True)
    var = jnp.mean(jnp.square(xf - mu), axis=-1, keepdims=True)
    y = (xf - mu) * lax.rsqrt(var + EPS)
    return (y * g.astype(jnp.float32) + b.astype(jnp.float32)).astype(x.dtype)


def conformer_conv(u, dw_w, dw_b, ln_g, ln_b):
    a, gate = jnp.split(u, 2, axis=-1)
    y = a * jax.nn.sigmoid(gate)
    y = lax.conv_general_dilated(
        y, dw_w[:, None, :].astype(y.dtype), window_strides=(1,),
        padding=[(CONV_WIDTH - 1, 0)],
        dimension_numbers=("NWC", "WIO", "NWC"),
        feature_group_count=CONV_CH) + dw_b
    y = layer_norm(y, ln_g, ln_b)
    return jax.nn.silu(y)


def diff_attention(q, k, v, lam, subln_g, lam_init):
    B, S = q.shape[0], q.shape[1]
    nb = S // Q_BLOCK
    scale = DA_HEAD_DIM ** -0.5
    qb = q.reshape(B, nb, Q_BLOCK, DA_HEADS, 2, DA_HEAD_DIM).transpose(1, 0, 2, 3, 4, 5)
    key_pos = jnp.arange(S)

    def block(args):
        qi, bi = args
        s = jnp.einsum("bqhcd,bkhcd->bhcqk", qi, k,
                       preferred_element_type=jnp.float32) * scale
        q_pos = bi * Q_BLOCK + jnp.arange(Q_BLOCK)
        mask = key_pos[None, :] <= q_pos[:, None]
        s = jnp.where(mask, s, -jnp.inf)
        p = jax.nn.softmax(s, axis=-1)
        a = (p[:, :, 0] - lam * p[:, :, 1]).astype(v.dtype)
        return jnp.einsum("bhqk,bkhe->bqhe", a, v)

    o = lax.map(block, (qb, jnp.arange(nb)))
    o = o.transpose(1, 0, 2, 3, 4).reshape(B, S, DA_HEADS, DA_V_DIM)
    o = rms_norm(o, subln_g) * (1.0 - lam_init)
    return o.reshape(B, S, ATTN_W)


def hierarchical_moe(h, w_rg, b_rg, w_re, b_re, w_g, w_u, w_d):
    B, S, D = h.shape
    T = B * S
    hf = h.reshape(T, D)
    g_logits = jnp.matmul(hf, w_rg, preferred_element_type=jnp.float32) + b_rg.astype(jnp.float32)
    g_prob = jax.nn.softmax(g_logits, axis=-1)
    g_sel = jnp.argmax(g_logits, axis=-1)
    g_w = jnp.take_along_axis(g_prob, g_sel[:, None], axis=1)
    e_logits = (jnp.matmul(hf, w_re, preferred_element_type=jnp.float32)
                + b_re.astype(jnp.float32)).reshape(T, N_GROUPS, EXPERTS_PER_GROUP)
    e_logits = jnp.take_along_axis(e_logits, g_sel[:, None, None], axis=1)[:, 0]
    e_prob = jax.nn.softmax(e_logits, axis=-1)
    top_p, top_i = lax.top_k(e_prob, TOP_K_IN_GROUP)
    wts = top_p / jnp.sum(top_p, axis=-1, keepdims=True) * g_w
    eidx = g_sel[:, None] * EXPERTS_PER_GROUP + top_i

    A = T * TOP_K_IN_GROUP
    e_flat = eidx.reshape(A).astype(jnp.int32)
    tok_flat = jnp.repeat(jnp.arange(T, dtype=jnp.int32), TOP_K_IN_GROUP)
    w_flat = wts.reshape(A).astype(h.dtype)
    order = jnp.argsort(e_flat, stable=True)
    e_s, tok_s, w_s = e_flat[order], tok_flat[order], w_flat[order]
    counts = jnp.zeros((N_EXPERTS,), jnp.int32).at[e_flat].add(1)
    padded = (counts + ROW_BLOCK - 1) // ROW_BLOCK * ROW_BLOCK
    start = jnp.cumsum(counts) - counts
    pad_end = jnp.cumsum(padded)
    pstart = pad_end - padded
    dest = pstart[e_s] + jnp.arange(A, dtype=jnp.int32) - start[e_s]
    n_rows = A + N_EXPERTS * ROW_BLOCK
    n_blk = n_rows // ROW_BLOCK
    row_tok = jnp.zeros((n_rows,), jnp.int32).at[dest].set(tok_s)
    row_w = jnp.zeros((n_rows,), h.dtype).at[dest].set(w_s)
    blk_exp = jnp.clip(jnp.searchsorted(pad_end, jnp.arange(n_blk, dtype=jnp.int32) * ROW_BLOCK,
                                        side="right"), 0, N_EXPERTS - 1)

    def expert_block(args):
        toks, wb, e = args
        xb = hf[toks]
        hid = jax.nn.silu(jnp.matmul(xb, w_g[e])) * jnp.matmul(xb, w_u[e])
        return jnp.matmul(hid, w_d[e]) * wb[:, None]

    yb = lax.map(expert_block, (row_tok.reshape(n_blk, ROW_BLOCK),
                                row_w.reshape(n_blk, ROW_BLOCK), blk_exp))
    y = jnp.zeros_like(hf).at[row_tok].add(yb.reshape(n_rows, D))
    return y.reshape(B, S, D)


def setup_inputs(seed: int = 0) -> dict:
    key = jax.random.key(seed)
    ks = jax.random.split(key, 26)
    f32 = jnp.float32

    def nrm(k, shape, scale):
        return jax.random.normal(k, shape, f32) * scale

    L, D, C = DEPTH, D_MODEL, CONV_CH
    return {
        "x": nrm(ks[0], (BATCH, SEQ, D), 1.0),
        "attn_norm_g": 1.0 + nrm(ks[1], (L, D), 0.05),
        "w_in": nrm(ks[2], (L, D, IN_COLS), D ** -0.5),
        "conv_dw_w": nrm(ks[3], (L, CONV_WIDTH, C), CONV_WIDTH ** -0.5),
        "conv_dw_b": nrm(ks[4], (L, C), 0.02),
        "conv_ln_g": 1.0 + nrm(ks[5], (L, C), 0.05),
        "conv_ln_b": nrm(ks[6], (L, C), 0.02),
        "w_conv_out": nrm(ks[7], (L, C, D), C ** -0.5),
        "q_norm_g": 1.0 + nrm(ks[8], (L, DA_HEAD_DIM), 0.05),
        "k_norm_g": 1.0 + nrm(ks[9], (L, DA_HEAD_DIM), 0.05),
        "lambda_q1": nrm(ks[10], (L, DA_HEAD_DIM), 0.1),
        "lambda_k1": nrm(ks[11], (L, DA_HEAD_DIM), 0.1),
        "lambda_q2": nrm(ks[12], (L, DA_HEAD_DIM), 0.1),
        "lambda_k2": nrm(ks[13], (L, DA_HEAD_DIM), 0.1),
        "subln_g": 1.0 + nrm(ks[14], (L, DA_V_DIM), 0.05),
        "w_attn_out": nrm(ks[15], (L, ATTN_W, D), ATTN_W ** -0.5),
        "w_out": nrm(ks[16], (L, D, D), D ** -0.5),
        "ffn_norm_g": 1.0 + nrm(ks[17], (L, D), 0.05),
        "w_router_group": nrm(ks[18], (L, D, N_GROUPS), D ** -0.5),
        "b_router_group": nrm(ks[19], (L, N_GROUPS), 0.01),
        "w_router_expert": nrm(ks[20], (L, D, N_EXPERTS), D ** -0.5),
        "b_router_expert": nrm(ks[21], (L, N_EXPERTS), 0.01),
        "w_gate_e": nrm(ks[22], (L, N_EXPERTS, D, D_EXPERT), D ** -0.5),
        "w_up_e": nrm(ks[23], (L, N_EXPERTS, D, D_EXPERT), D ** -0.5),
        "w_down_e": nrm(ks[24], (L, N_EXPERTS, D_EXPERT, D), D_EXPERT ** -0.5),
    }


def reference(x, attn_norm_g, w_in, conv_dw_w, conv_dw_b, conv_ln_g, conv_ln_b, w_conv_out,
              q_norm_g, k_norm_g, lambda_q1, lambda_k1, lambda_q2, lambda_k2, subln_g,
              w_attn_out, w_out, ffn_norm_g, w_router_group, b_router_group,
              w_router_expert, b_router_expert, w_gate_e, w_up_e, w_down_e):
    B, S, D = x.shape
    for l in range(DEPTH):
        lam_init = 0.8 - 0.6 * math.exp(-0.3 * l)
        h = rms_norm(x, attn_norm_g[l])
        proj = jnp.matmul(h, w_in[l])
        o0 = COL_CONV
        o1 = o0 + COL_Q
        o2 = o1 + COL_K
        o3 = o2 + COL_V
        u_conv = proj[..., :o0]
        q = proj[..., o0:o1].reshape(B, S, DA_HEADS, 2, DA_HEAD_DIM)
        k = proj[..., o1:o2].reshape(B, S, DA_HEADS, 2, DA_HEAD_DIM)
        v = proj[..., o2:o3].reshape(B, S, DA_HEADS, DA_V_DIM)
        gates = jax.nn.sigmoid(proj[..., o3:]).reshape(B, S, N_BRANCH, D)

        conv_out = conformer_conv(u_conv, conv_dw_w[l], conv_dw_b[l], conv_ln_g[l], conv_ln_b[l])

        q = rms_norm(q, q_norm_g[l])
        k = rms_norm(k, k_norm_g[l])
        lam = (jnp.exp(jnp.sum(lambda_q1[l].astype(jnp.float32) * lambda_k1[l].astype(jnp.float32)))
               - jnp.exp(jnp.sum(lambda_q2[l].astype(jnp.float32) * lambda_k2[l].astype(jnp.float32)))
               + lam_init)
        attn_out = diff_attention(q, k, v, lam, subln_g[l], lam_init)

        merged = (gates[:, :, 0] * jnp.matmul(conv_out, w_conv_out[l])
                  + gates[:, :, 1] * jnp.matmul(attn_out, w_attn_out[l]))
        x = x + jnp.matmul(merged, w_out[l])
        h2 = rms_norm(x, ffn_norm_g[l])
        x = x + hierarchical_moe(h2, w_router_group[l], b_router_group[l], w_router_expert[l],
                                 b_router_expert[l], w_gate_e[l], w_up_e[l], w_down_e[l])
    return x
```

```python
import contextlib
import numpy as np
import concourse.bass as bass
import concourse.mybir as mybir
from concourse.bass_utils import run_bass_kernel_spmd

F32 = mybir.dt.float32
BF16 = mybir.dt.bfloat16
I32 = mybir.dt.int32
ALU = mybir.AluOpType
AF = mybir.ActivationFunctionType
AX = mybir.AxisListType

S = 4096
D = 1024
NST = 8
EPS = 1e-6
LAM_INIT = 0.8 - 0.6 * 1.0
CAP = 512
NE = 32


class Sched:
    ENGS = ["sync", "scalar", "vector", "gpsimd", "tensor"]

    def __init__(self, nc, stack, n_dma_sems=80):
        self.nc = nc
        self.streams = {e: [] for e in self.ENGS}
        self.sems = {}
        self.count = {}
        for e in self.ENGS:
            self.sems["e:" + e] = stack.enter_context(nc.semaphore("sem_" + e))
            self.count["e:" + e] = 0
        self.free_dma = [stack.enter_context(nc.semaphore("dsem%d" % i)) for i in range(n_dma_sems)]
        self.waited = {e: {} for e in self.ENGS}
        self.last_w = {}
        self.readers = {}

    def _dsem(self, key):
        k = "d:" + key
        if k not in self.sems:
            self.sems[k] = self.free_dma.pop()
            self.count[k] = 0
        return k

    def _deps(self, reads, writes):
        toks = []
        for k in reads:
            t = self.last_w.get(k)
            if t is not None:
                toks.append(t)
        for k in writes:
            t = self.last_w.get(k)
            if t is not None:
                toks.append(t)
            for sk, v in self.readers.get(k, {}).items():
                toks.append((sk, v))
        return toks

    def _wait(self, eng, toks):
        w = self.waited[eng]
        need = {}
        for sk, v in toks:
            if w.get(sk, 0) >= v:
                continue
            if eng == "tensor" and sk == "e:tensor":
                continue
            if need.get(sk, 0) < v:
                need[sk] = v
        for sk, v in need.items():
            w[sk] = v
            sem = self.sems[sk]
            self.streams[eng].append(lambda e, sem=sem, v=v: e.wait_ge(sem, v))

    def _commit(self, tok, reads, writes):
        sk, v = tok
        for k in reads:
            r = self.readers.setdefault(k, {})
            if r.get(sk, 0) < v:
                r[sk] = v
        for k in writes:
            self.last_w[k] = tok
            self.readers[k] = {}

    def op(self, eng, fn, reads=(), writes=()):
        self._wait(eng, self._deps(reads, writes))
        sk = "e:" + eng
        self.count[sk] += 1
        n = self.count[sk]
        sem = self.sems[sk]
        self.streams[eng].append(lambda e, fn=fn, sem=sem: fn(e).then_inc(sem, 1))
        tok = (sk, n)
        self._commit(tok, reads, writes)
        return tok

    def dma(self, q, semkey, fn, reads=(), writes=()):
        self._wait(q, self._deps(reads, writes))
        sk = self._dsem(semkey)
        self.count[sk] += 16
        n = self.count[sk]
        sem = self.sems[sk]
        self.streams[q].append(lambda e, fn=fn, sem=sem: fn(e).then_inc(sem, 16))
        tok = (sk, n)
        self._commit(tok, reads, writes)
        return tok

    def wait_all(self, eng):
        toks = []
        for k, t in self.last_w.items():
            toks.append(t)
        for k, r in self.readers.items():
            for sk, v in r.items():
                toks.append((sk, v))
        self._wait(eng, toks)

    def flush(self):
        with self.nc.Block() as block:
            for name in self.ENGS:
                fns = self.streams[name]
                if not fns:
                    continue

                def body(e, fns=fns):
                    for f in fns:
                        f(e)
                getattr(block, name)(body)
        self.streams = {e: [] for e in self.ENGS}


def build_program(dbg=False, phases=("A", "B", "C", "D"), nst=NST, stop=99, nexp=NE, bstop=99):
    nc = bass.Bass("TRN2", target_bir_lowering=False)
    okind = "ExternalOutput" if dbg else "Internal"

    def din(name, shape, dt=F32):
        return nc.dram_tensor(name, list(shape), dt, kind="ExternalInput")

    x_d = din("x", [S, D])
    w_in_d = din("w_in", [D, 4608])
    cst_d = din("cst", [128, 128 * 4])
    g1T_d = din("g1T", [128, 8])
    cw_d = din("cw", [128, 4, 31])
    cvec_d = din("cvec", [128, 4, 3])
    qkg_d = din("qkg", [128, 2])
    w_co_d = din("w_co", [512, D])
    w_ao_d = din("w_ao", [512, D])
    w_out_d = din("w_out", [D, D])
    g2bc_d = din("g2bc", [128, D])
    lamv_d = din("lamv", [128, 4, 64])
    sgbc_d = din("sgbc", [128, 128])
    wr_d = din("wr", [D, 36])
    rbbc_d = din("rbbc", [128, 36])
    eoff_d = din("eoff", [128, NE])
    w_g_d = din("w_g", [NE, D, 512])
    w_u_d = din("w_u", [NE, D, 512])
    w_d_d = din("w_d", [NE, 512, D])
    out_d = nc.dram_tensor("out", [S, D], F32, kind="ExternalOutput")
    x1_s = nc.dram_tensor("x1_s", [S, D], F32, kind=okind)
    h2_s = nc.dram_tensor("h2_s", [S, D], BF16, kind=okind)
    XS = nc.dram_tensor("XS", [NE * CAP, D], BF16, kind="Internal")
    YS = nc.dram_tensor("YS", [NE * CAP, D], F32, kind="Internal")
    if dbg:
        lg_o = nc.dram_tensor("lg_o", [128, 32, 36], F32, kind="ExternalOutput")
        rt_o = nc.dram_tensor("rt_o", [128, 4, 32], F32, kind="ExternalOutput")
    qT_s = nc.dram_tensor("qT_s", [128, 4, S], BF16, kind=okind)
    cvT_s = nc.dram_tensor("cvT_s", [128, 4, S], BF16, kind=okind)
    kT_s = nc.dram_tensor("kT_s", [128, 4, S], BF16, kind=okind)
    if dbg:
        v_o = nc.dram_tensor("v_o", [128, 32, 4, 130], BF16, kind="ExternalOutput")

    with contextlib.ExitStack() as st:
        sc = Sched(nc, st)
        sb = lambda name, shape, dt=F32: st.enter_context(nc.sbuf_tensor("sb_" + name, list(shape), dt))
        cst = sb("cst", [128, 512])
        ident_b = sb("ident_b", [128, 128], BF16)
        V = sb("V", [128, 32, 4, 130], BF16)
        PS = [st.enter_context(nc.psum_tensor("ps%d" % i, [128, 512], F32)) for i in range(6)]
        PB = [st.enter_context(nc.psum_tensor("pb%d" % i, [128, 1024], BF16)) for i in range(2)]
        nhalf = sb("nhalf", [128, 512])
        bnd = {}

        def _mk_bnd(e):
            bnd["r"] = e.alloc_register("bnd")
            e.reg_mov(bnd["r"], NE * CAP - 1)
        sc.streams["gpsimd"].append(_mk_bnd)
        LG = sb("LG", [128, 32, 36])
        dest_i = sb("dest_i", [128, 2, 32], I32)
        wts = sb("wts", [128, 2, 32])
        if dbg:
            sc.op("vector", lambda e: e.memset(LG[:], 0.0), writes=["LG%d" % i for i in range(32)])
        zeros_b = sb("zeros_b", [128, D], BF16)
        sc.op("vector", lambda e: e.memset(zeros_b[:], 0.0), writes=["zeros_b"])
        XS_v = XS.ap().rearrange("(r p) d -> r p d", p=128)
        for r in range(NE * CAP // 128):
            sc.dma("sync", "xs_zero", lambda e, r=r: e.dma_start(out=XS_v[r], in_=zeros_b[:]),
                   reads=["zeros_b"], writes=["XS"])
        sc.op("gpsimd", lambda e: e.memset(nhalf[:], -0.5), writes=["nhalf"])

        sc.dma("sync", "cst", lambda e: e.dma_start(out=cst[:], in_=cst_d.ap()), writes=["cst"])
        sc.op("vector", lambda e: e.tensor_copy(out=ident_b[:], in_=cst[:, 0:128]), reads=["cst"], writes=["ident_b"])
        sc.op("gpsimd", lambda e: e.memset(V[:], 1.0), writes=["Vones"] + ["V%d" % i for i in range(NST)])

        if "A" in phases:
            phase_a(nc, sc, locals())
        if "B" in phases:
            phase_b(nc, sc, locals())
        if dbg and "B" in phases:
            sc.dma("sync", "dbg2", lambda e: e.dma_start(out=lg_o.ap(), in_=LG[:]),
                   reads=["LG%d" % i for i in range(32)], writes=["lg_o"])
        if "C" in phases:
            phase_c(nc, sc, locals())
        if "D" in phases:
            phase_d(nc, sc, locals())

        if dbg:
            sc.dma("sync", "dbg", lambda e: e.dma_start(out=v_o.ap(), in_=V[:]),
                   reads=["V%d" % i for i in range(NST)] + ["Vones"], writes=["v_o"])
        sc.wait_all("sync")
        sc.flush()
    return nc


def phase_a(nc, sc, G):
    x_d, w_in_d, g1T_d, cw_d, cvec_d, qkg_d = (G[k] for k in ["x_d", "w_in_d", "g1T_d", "cw_d", "cvec_d", "qkg_d"])
    cst, ident_b, V, PS, PB, qT_s, cvT_s, kT_s, nhalf = (G[k] for k in ["cst", "ident_b", "V", "PS", "PB", "qT_s", "cvT_s", "kT_s", "nhalf"])
    with contextlib.ExitStack() as st:
        sb = lambda name, shape, dt=F32: st.enter_context(nc.sbuf_tensor("sb_" + name, list(shape), dt))
        NCA = 2560
        w_bf = sb("a_wbf", [128, 8, NCA], BF16)
        stage = [sb("a_stage%d" % i, [128, 8, 128]) for i in range(2)]
        g1T = sb("a_g1T", [128, 8])
        cw = sb("a_cw", [128, 4, 31])
        cvec = sb("a_cvec", [128, 4, 3])
        qkg = sb("a_qkg", [128, 2])
        diag = sb("a_diag", [128, 4, 31, 128], BF16)
        onesN = sb("a_onesN", [128, 128], BF16)
        blk = sb("a_blk", [128, 128], BF16)
        xt = [sb("a_xt0", [128, 4, D])] * 2
        ss = sb("a_ss", [128, 8])
        rstd = sb("a_rstd", [128, 8])
        xn = sb("a_xn", [128, 4, D], BF16)
        hT = sb("a_hT", [128, 8, 512], BF16)
        ybuf = [sb("a_ybuf%d" % i, [128, 4, 544], BF16) for i in range(2)]
        ybuf1 = [sb("a_ybuf1_%d" % i, [128, 4, 544], BF16) for i in range(2)]
        sg = [sb("a_sg0", [128, 512])] * 2
        ycb = sb("a_ycb", [128, 4, 512], BF16)
        ysq = sb("a_ysq", [128, 4, 512], BF16)
        mean_sb = sb("a_mean", [128, 512])
        lrstd = sb("a_lrstd", [128, 512])
        zt = [sb("a_zt0", [128, 512])] * 2
        cvT = sb("a_cvT", [128, 4, 512], BF16)
        sq = [sb("a_sq%d" % i, [128, 512], BF16) for i in range(2)]
        qr = [sb("a_qr%d" % i, [128, 512]) for i in range(2)]
        qkT = [sb("a_qT", [128, 4, 512], BF16), sb("a_kT", [128, 4, 512], BF16)]

        sc.dma("sync", "g1T", lambda e: e.dma_start(out=g1T[:], in_=g1T_d.ap()), writes=["g1T"])
        sc.dma("sync", "cw", lambda e: e.dma_start(out=cw[:], in_=cw_d.ap()), writes=["cw"])
        sc.dma("sync", "cvec", lambda e: e.dma_start(out=cvec[:], in_=cvec_d.ap()), writes=["cvec"])
        sc.dma("sync", "qkg", lambda e: e.dma_start(out=qkg[:], in_=qkg_d.ap()), writes=["qkg"])
        w_in_v = w_in_d.ap().rearrange("(kc p) n -> p kc n", p=128)
        for cg in range(NCA // 128):
            sl = stage[cg % 2]
            key = "stage%d" % (cg % 2)
            sc.dma("sync", key, lambda e, sl=sl, cg=cg: e.dma_start(out=sl[:], in_=w_in_v[:, :, cg * 128:(cg + 1) * 128]),
                   writes=[key])
            for kc in range(8):
                if kc % 2 == 0:
                    sc.op("vector", lambda e, sl=sl, cg=cg, kc=kc: e.tensor_scalar(
                        out=w_bf[:, kc, cg * 128:(cg + 1) * 128], in0=sl[:, kc, :], scalar1=g1T[:, kc:kc + 1],
                        scalar2=None, op0=ALU.mult), reads=[key, "g1T"], writes=["wbf%d_%d" % (cg // 4, kc)])
                else:
                    sc.op("scalar", lambda e, sl=sl, cg=cg, kc=kc: e.activation(
                        out=w_bf[:, kc, cg * 128:(cg + 1) * 128], in_=sl[:, kc, :], func=AF.Copy,
                        scale=g1T[:, kc:kc + 1]), reads=[key, "g1T"], writes=["wbf%d_%d" % (cg // 4, kc)])
        wkeys = lambda cg: ["wbf%d_%d" % (cg, kc) for kc in range(8)]
        for ch in range(4):
            for k in range(31):
                if k % 2 == 0:
                    sc.op("vector", lambda e, ch=ch, k=k: e.tensor_scalar(
                        out=diag[:, ch, k, :], in0=cst[:, 0:128], scalar1=cw[:, ch, k:k + 1], scalar2=None,
                        op0=ALU.mult), reads=["cst", "cw"], writes=["diag"])
                else:
                    sc.op("scalar", lambda e, ch=ch, k=k: e.activation(
                        out=diag[:, ch, k, :], in_=cst[:, 0:128], func=AF.Copy, scale=cw[:, ch, k:k + 1]),
                        reads=["cst", "cw"], writes=["diag"])
        sc.op("vector", lambda e: e.memset(onesN[:], 1.0 / 512.0), writes=["onesN"])
        sc.op("vector", lambda e: e.tensor_scalar(out=blk[:], in0=cst[:, 384:512], scalar1=1.0 / 64.0, scalar2=None,
                                                  op0=ALU.mult), reads=["cst"], writes=["blk"])
        for i in range(2):
            sc.op("gpsimd", lambda e, i=i: e.memset(ybuf[i][:], 0.0), writes=["ybuf%d" % i])
            sc.op("gpsimd", lambda e, i=i: e.memset(ybuf1[i][:], 0.0), writes=["ybuf%d" % i])

        x_v = x_d.ap().rearrange("(s j p) d -> s p j d", j=4, p=128)
        bank = [0]

        def nb():
            b = bank[0]
            bank[0] = (b + 1) % 6
            return b

        def load_x(s):
            sc.dma("sync", "xt0", lambda e, s=s: e.dma_start(out=xt[0][:], in_=x_v[s]), writes=["xt0"])

        def tile(s):
            stop = G.get("stop", 99)
            if s == 0:
                load_x(0)
            xs = xt[0]
            xk = "xt0"
            sc.op("vector", lambda e: e.memset(ss[:, 0:4], 0.0), writes=["ss"])
            for j in range(4):
                sc.op("scalar", lambda e, j=j: e.activation(out=xn[:, j, :], in_=xs[:, j, :], func=AF.Square,
                                                            scale=1.0 / 32.0, accum_out=ss[:, j:j + 1]),
                      reads=[xk, "ss"], writes=["xn%d" % j, "ss"])
            sc.op("vector", lambda e: e.tensor_scalar(out=rstd[:, 0:4], in0=ss[:, 0:4], scalar1=EPS, scalar2=None,
                                                      op0=ALU.add), reads=["ss"], writes=["rstd"])
            sc.op("gpsimd", lambda e: e.tensor_tensor(out=rstd[:, 0:4], in0=rstd[:, 0:4], in1=nhalf[:, 0:4], op=ALU.pow),
                  reads=["rstd", "nhalf"], writes=["rstd"])
            for j in range(4):
                sc.op("vector", lambda e, j=j: e.tensor_scalar(
                    out=xn[:, j, :], in0=xs[:, j, :], scalar1=rstd[:, j:j + 1], scalar2=None, op0=ALU.mult),
                    reads=[xk, "rstd"], writes=["xn%d" % j])
            if s + 1 < G["nst"]:
                load_x(s + 1)
            if stop < 2:
                return
            for j in range(4):
                pb = PB[j % 2]
                pk = "pb%d" % (j % 2)

                def tr(e, j=j, pb=pb):
                    for kc in range(8):
                        ins = e.transpose(out=pb[:, kc * 128:(kc + 1) * 128], in_=xn[:, j, kc * 128:(kc + 1) * 128],
                                          identity=ident_b[:])
                    return ins
                sc.op("tensor", tr, reads=["xn%d" % j, "ident_b"], writes=[pk])
                sc.op("vector" if j % 2 else "scalar",
                      (lambda e, j=j, pb=pb: e.tensor_copy(out=hT[:, :, j * 128:(j + 1) * 128],
                                                           in_=pb[:].rearrange("p (k t) -> p k t", k=8))) if j % 2 else
                      (lambda e, j=j, pb=pb: e.copy(out=hT[:, :, j * 128:(j + 1) * 128],
                                                    in_=pb[:].rearrange("p (k t) -> p k t", k=8))),
                      reads=[pk], writes=["hT%d" % j])
            hkeys = ["hT%d" % j for j in range(4)]

            def fm_matmul(m):
                b = nb()

                def f(e, m=m, b=b):
                    for kc in range(8):
                        ins = e.matmul(PS[b][:, :], lhsT=w_bf[:, kc, m * 128:(m + 1) * 128], rhs=hT[:, kc, :],
                                       start=(kc == 0), stop=(kc == 7))
                    return ins
                sc.op("tensor", f, reads=hkeys + wkeys(m // 4), writes=["ps%d" % b])
                return b

            if stop < 2.1:
                return
            yb = ybuf[s % 2]
            ybk = "ybuf%d" % (s % 2)
            ybo = ybuf[(s + 1) % 2]
            yb1 = ybuf1[s % 2]
            ybo1 = ybuf1[(s + 1) % 2]
            ybok = "ybuf%d" % ((s + 1) % 2)
            for ch in range(4):
                bg = fm_matmul(4 + ch)
                ba = fm_matmul(ch)
                sgt = sg[ch % 2]
                if stop < 2.3:
                    continue
                sc.op("scalar", lambda e, bg=bg, sgt=sgt: e.activation(out=sgt[:], in_=PS[bg][:, :], func=AF.Sigmoid),
                      reads=["ps%d" % bg], writes=["sg0"])
                if stop < 2.6:
                    continue
                if s > 0:
                    sc.op("gpsimd", lambda e, ch=ch: e.tensor_copy(out=yb[:, ch, 0:30], in_=ybo[:, ch, 512:542]),
                          reads=[ybok + "_%d" % ch], writes=[ybk + "_%d" % ch])
                    sc.op("gpsimd", lambda e, ch=ch: e.tensor_copy(out=yb1[:, ch, 0:29], in_=ybo1[:, ch, 512:541]),
                          reads=[ybok + "_%d" % ch], writes=[ybk + "_%d" % ch])
                sc.op("vector", lambda e, ba=ba, sgt=sgt, ch=ch: e.tensor_tensor(
                    out=yb[:, ch, 30:542], in0=PS[ba][:, :], in1=sgt[:], op=ALU.mult),
                    reads=["ps%d" % ba, "sg0", ybk], writes=[ybk + "_%d" % ch])
                sc.op("gpsimd", lambda e, ch=ch: e.tensor_copy(out=yb1[:, ch, 29:541], in_=yb[:, ch, 30:542]),
                      reads=[ybk + "_%d" % ch], writes=[ybk + "_%d" % ch])
            if stop < 3.5:
                return
            for ch in range(4):
                b = nb()

                def cf(e, ch=ch, b=b):
                    for k in (range(31) if stop != 3.6 else [0, 16, 30]):
                        rhs = yb[:, ch, k:k + 512] if k % 2 == 0 else yb1[:, ch, k - 1:k - 1 + 512]
                        ins = e.matmul(PS[b][:, :], lhsT=diag[:, ch, k, :], rhs=rhs, start=(k == 0), stop=(k == 30))
                    return ins
                sc.op("tensor", cf, reads=[ybk + "_%d" % ch, "diag", ybk], writes=["ps%d" % b])
                if stop < 3.8:
                    continue
                sc.op("vector", lambda e, ch=ch, b=b: e.tensor_scalar(
                    out=ycb[:, ch, :], in0=PS[b][:, :], scalar1=cvec[:, ch, 0:1], scalar2=None, op0=ALU.add),
                    reads=["ps%d" % b, "cvec"], writes=["ycb%d" % ch])
                if stop < 3.9:
                    continue
                sc.op("scalar", lambda e, ch=ch, b=b: e.activation(
                    out=ysq[:, ch, :], in_=ycb[:, ch, :], func=AF.Square),
                    reads=["ycb%d" % ch], writes=["ysq%d" % ch])
            if stop < 5:
                return
            bm = nb()
            bq = nb()

            def stf(e, bm=bm, bq=bq):
                for ch in range(4):
                    e.matmul(PS[bm][:, :], lhsT=onesN[:], rhs=ycb[:, ch, :], start=(ch == 0), stop=(ch == 3))
                for ch in range(4):
                    ins = e.matmul(PS[bq][:, :], lhsT=onesN[:], rhs=ysq[:, ch, :], start=(ch == 0), stop=(ch == 3))
                return ins
            sc.op("tensor", stf, reads=["ycb%d" % c for c in range(4)] + ["ysq%d" % c for c in range(4)] + ["onesN"],
                  writes=["ps%d" % bm, "ps%d" % bq])
            sc.op("scalar", lambda e, bm=bm: e.copy(out=mean_sb[:], in_=PS[bm][:, :]), reads=["ps%d" % bm], writes=["mean"])
            sc.op("vector", lambda e: e.tensor_tensor(out=lrstd[:], in0=mean_sb[:], in1=mean_sb[:], op=ALU.mult),
                  reads=["mean"], writes=["lrstd"])
            sc.op("vector", lambda e, bq=bq: e.tensor_tensor(out=lrstd[:], in0=PS[bq][:, :], in1=lrstd[:], op=ALU.subtract),
                  reads=["ps%d" % bq, "lrstd"], writes=["lrstd"])
            sc.op("vector", lambda e: e.tensor_scalar(out=lrstd[:], in0=lrstd[:], scalar1=EPS, scalar2=None,
                                                      op0=ALU.add), reads=["lrstd"], writes=["lrstd"])
            sc.op("scalar", lambda e: e.activation(out=lrstd[:], in_=lrstd[:], func=AF.Sqrt), reads=["lrstd"], writes=["lrstd"])
            sc.op("vector", lambda e: e.reciprocal(out=lrstd[:], in_=lrstd[:]), reads=["lrstd"], writes=["lrstd"])
            for ch in range(4):
                z = zt[ch % 2]
                zk = "zt0"
                sc.op("gpsimd", lambda e, ch=ch, z=z: e.tensor_tensor(out=z[:], in0=ycb[:, ch, :], in1=mean_sb[:],
                                                                      op=ALU.subtract),
                      reads=["ycb%d" % ch, "mean"], writes=[zk])
                sc.op("vector", lambda e, z=z: e.tensor_tensor(out=z[:], in0=z[:], in1=lrstd[:], op=ALU.mult),
                      reads=[zk, "lrstd"], writes=[zk])
                sc.op("vector", lambda e, ch=ch, z=z: e.tensor_scalar(out=z[:], in0=z[:], scalar1=cvec[:, ch, 1:2],
                                                                      scalar2=cvec[:, ch, 2:3], op0=ALU.mult, op1=ALU.add),
                      reads=[zk, "cvec"], writes=[zk])
                sc.op("scalar", lambda e, ch=ch, z=z: e.activation(out=cvT[:, ch, :], in_=z[:], func=AF.Silu),
                      reads=[zk], writes=["cvT"])
            sc.dma("sync", "cvT_st", lambda e, s=s: e.dma_start(out=cvT_s.ap()[:, :, s * 512:(s + 1) * 512], in_=cvT[:]),
                   reads=["cvT"], writes=["cvT_s%d" % s])
            if stop < 6:
                return
            for which in range(2):
                for h in range(4):
                    b = fm_matmul(8 + which * 4 + h)
                    i2 = h % 2
                    sc.op("scalar", lambda e, b=b, i2=i2: e.activation(out=sq[i2][:], in_=PS[b][:, :], func=AF.Square),
                          reads=["ps%d" % b], writes=["sq%d" % i2])
                    b2 = nb()
                    sc.op("tensor", lambda e, b2=b2, i2=i2: e.matmul(PS[b2][:, :], lhsT=blk[:], rhs=sq[i2][:],
                                                                    start=True, stop=True),
                          reads=["sq%d" % i2, "blk"], writes=["ps%d" % b2])
                    sc.op("vector", lambda e, b2=b2, i2=i2: e.tensor_scalar(
                        out=qr[i2][:], in0=PS[b2][:, :], scalar1=EPS, scalar2=None, op0=ALU.add),
                        reads=["ps%d" % b2], writes=["qr%d" % i2])
                    sc.op("scalar", lambda e, i2=i2: e.activation(out=qr[i2][:], in_=qr[i2][:], func=AF.Sqrt),
                          reads=["qr%d" % i2], writes=["qr%d" % i2])
                    sc.op("vector", lambda e, i2=i2: e.reciprocal(out=qr[i2][:], in_=qr[i2][:]),
                          reads=["qr%d" % i2], writes=["qr%d" % i2])
                    dst = qkT[which][:, h, :]
                    wk = ["qkT%d" % which]
                    sc.op("vector", lambda e, b=b, i2=i2, dst=dst, which=which: e.scalar_tensor_tensor(
                        out=dst, in0=PS[b][:, :], scalar=qkg[:, which:which + 1], in1=qr[i2][:],
                        op0=ALU.mult, op1=ALU.mult), reads=["ps%d" % b, "qr%d" % i2, "qkg"], writes=wk)
                dd = qT_s if which == 0 else kT_s
                sc.dma("sync", "qk_st%d" % which, lambda e, s=s, dd=dd, which=which: e.dma_start(
                    out=dd.ap()[:, :, s * 512:(s + 1) * 512], in_=qkT[which][:]),
                    reads=["qkT%d" % which], writes=["qk_s%d_%d" % (which, s)])
            if stop < 7:
                return
            for j in range(4):
                b = nb()

                def vf(e, j=j, b=b):
                    for kc in range(8):
                        ins = e.matmul(PS[b][:, :], lhsT=hT[:, kc, j * 128:(j + 1) * 128], rhs=w_bf[:, kc, 2048:2560],
                                       start=(kc == 0), stop=(kc == 7))
                    return ins
                sc.op("tensor", vf, reads=["hT%d" % j] + wkeys(4), writes=["ps%d" % b])
                sc.op("scalar" if j % 2 else "vector",
                      (lambda e, j=j, b=b: e.copy(out=V[:, s * 4 + j, :, 0:128],
                                                  in_=PS[b][:, :].rearrange("p (h e) -> p h e", h=4))) if j % 2 else
                      (lambda e, j=j, b=b: e.tensor_copy(out=V[:, s * 4 + j, :, 0:128],
                                                         in_=PS[b][:, :].rearrange("p (h e) -> p h e", h=4))),
                      reads=["ps%d" % b], writes=["V%d" % s])
        for s in range(G["nst"]):
            tile(s)
        for en in sc.ENGS:
            sc.wait_all(en)


def load_cast(sc, nc, stage, stage_key, src_view, dst, ncols, kcn, dkey, scale=None):
    for cg in range(ncols // 128):
        sl = stage[cg % 2]
        key = stage_key + str(cg % 2)
        sc.dma("sync", key, lambda e, sl=sl, cg=cg: e.dma_start(out=sl[:, 0:kcn, :], in_=src_view[:, :, cg * 128:(cg + 1) * 128]),
               writes=[key])
        if scale is None:
            eng = ["vector", "gpsimd"][cg % 2]
            sc.op(eng, lambda e, sl=sl, cg=cg: e.tensor_copy(out=dst[:, :, cg * 128:(cg + 1) * 128], in_=sl[:, 0:kcn, :]),
                  reads=[key], writes=[dkey])
        else:
            for kc in range(kcn):
                if kc % 2 == 0:
                    sc.op("vector", lambda e, sl=sl, cg=cg, kc=kc: e.tensor_scalar(
                        out=dst[:, kc, cg * 128:(cg + 1) * 128], in0=sl[:, kc, :], scalar1=scale[:, kc:kc + 1],
                        scalar2=None, op0=ALU.mult), reads=[key, "g1T"], writes=[dkey])
                else:
                    sc.op("scalar", lambda e, sl=sl, cg=cg, kc=kc: e.activation(
                        out=dst[:, kc, cg * 128:(cg + 1) * 128], in_=sl[:, kc, :], func=AF.Copy,
                        scale=scale[:, kc:kc + 1]), reads=[key, "g1T"], writes=[dkey])


def phase_b(nc, sc, G):
    names = ["x_d", "w_in_d", "g1T_d", "w_co_d", "w_ao_d", "w_out_d", "g2bc_d", "lamv_d", "sgbc_d", "wr_d", "rbbc_d",
             "cst", "ident_b", "V", "PS", "PB", "qT_s", "cvT_s", "kT_s", "nhalf", "LG", "x1_s", "h2_s"]
    (x_d, w_in_d, g1T_d, w_co_d, w_ao_d, w_out_d, g2bc_d, lamv_d, sgbc_d, wr_d, rbbc_d,
     cst, ident_b, V, PS, PB, qT_s, cvT_s, kT_s, nhalf, LG, x1_s, h2_s) = (G[k] for k in names)
    nst = G["nst"]
    with contextlib.ExitStack() as st:
        sb = lambda name, shape, dt=F32: st.enter_context(nc.sbuf_tensor("sb_" + name, list(shape), dt))
        KT = sb("b_KT", [128, 4, S], BF16)
        wgt = sb("b_wgt", [128, 8, 2048], BF16)
        wco = sb("b_wco", [128, 4, D], BF16)
        wao = sb("b_wao", [128, 4, D], BF16)
        wout = sb("b_wout", [128, 8, D], BF16)
        wr = sb("b_wr", [128, 8, 36])
        stage_raw = [sb("b_stage%d" % i, [128, 1024]) for i in range(2)]
        stage = [t[:].rearrange("p (k c) -> p k c", k=8) for t in stage_raw]
        g1T = sb("b_g1T", [128, 8])
        g2bc = sb("b_g2bc", [128, D])
        lamv = sb("b_lamv", [128, 4, 64])
        sgbc = sb("b_sgbc", [128, 128])
        rbbc = sb("b_rbbc", [128, 36])
        tri_b = sb("b_tri", [128, 128], BF16)
        lam2 = sb("b_lam2", [128, 4])
        nlam = sb("b_nlam", [128, 1])
        xr = [sb("b_xr%d" % i, [128, D]) for i in range(2)]
        xm = sb("b_xm", [128, 4 * D], BF16)
        xn = xm[:].rearrange("p (j d) -> p j d", j=4)
        mT = xm[:].rearrange("p (k t) -> p k t", k=8)
        hT = sb("b_hT", [128, 8, 512], BF16)
        ss = sb("b_ss", [128, 8])
        rstd = sb("b_rstd", [128, 8])
        qT = sb("b_qT", [128, 4, 512], BF16)
        cvT = sb("b_cvT", [128, 4, 512], BF16)
        Et_t = sb("b_Et", [128, 4, 512], BF16)
        Et = [Et_t[:, i, :] for i in range(4)]
        aoT = sb("b_aoT", [128, 4, 512], BF16)
        Osb = sb("b_Osb", [128, 3, 3, 130])
        rr = sb("b_rr", [128, 4])
        t1 = sb("b_t1", [128, 128])
        ot = sb("b_ot", [128, 128])
        on4 = sb("b_on", [128, 4, 128], BF16)
        junk = sb("b_junk", [128, 128], BF16)
        sgs = [stage_raw[0][:, 0:512], stage_raw[0][:, 512:1024]]
        tu = [stage_raw[1][:, 0:512], stage_raw[1][:, 512:1024]]
        h2 = sb("b_h2", [128, D])
        h2b = Et_t[:, 0:2, :].rearrange("p a b -> p (a b)")
        h2T = Osb[:].rearrange("p a b c -> p (a b c)")[:, 0:1024].rearrange("p (k t) -> p k t", k=8)

        sc.dma("sync", "b_kt", lambda e: e.dma_start(out=KT[:, :, 0:nst * 512], in_=kT_s.ap()[:, :, 0:nst * 512]),
               reads=["qk_s1_%d" % i for i in range(nst)], writes=["KT"])
        sc.dma("sync", "b_g1T", lambda e: e.dma_start(out=g1T[:], in_=g1T_d.ap()), writes=["g1T"])
        sc.dma("sync", "b_g2bc", lambda e: e.dma_start(out=g2bc[:], in_=g2bc_d.ap()), writes=["g2bc"])
        sc.dma("sync", "b_lamv", lambda e: e.dma_start(out=lamv[:], in_=lamv_d.ap()), writes=["lamv"])
        sc.dma("sync", "b_sgbc", lambda e: e.dma_start(out=sgbc[:], in_=sgbc_d.ap()), writes=["sgbc"])
        sc.dma("sync", "b_rbbc", lambda e: e.dma_start(out=rbbc[:], in_=rbbc_d.ap()), writes=["rbbc"])
        sc.dma("sync", "b_wr", lambda e: e.dma_start(out=wr[:], in_=wr_d.ap().rearrange("(kc p) n -> p kc n", p=128)),
               writes=["wr"])
        w_in_v = w_in_d.ap().rearrange("(kc p) n -> p kc n", p=128)[:, :, 2560:4608]
        load_cast(sc, nc, stage, "b_stage", w_in_v, wgt, 2048, 8, "wgt", scale=g1T)
        load_cast(sc, nc, stage, "b_stage", w_co_d.ap().rearrange("(kc p) n -> p kc n", p=128), wco, D, 4, "wco")
        load_cast(sc, nc, stage, "b_stage", w_ao_d.ap().rearrange("(kc p) n -> p kc n", p=128), wao, D, 4, "wao")
        load_cast(sc, nc, stage, "b_stage", w_out_d.ap().rearrange("(kc p) n -> p kc n", p=128), wout, D, 8, "wout")
        sc.op("vector", lambda e: e.tensor_copy(out=tri_b[:], in_=cst[:, 128:256]), reads=["cst"], writes=["tri_b"])
        sc.op("vector", lambda e: e.tensor_scalar(out=sgbc[:], in0=sgbc[:], scalar1=1.0 - LAM_INIT, scalar2=None,
                                                  op0=ALU.mult), reads=["sgbc"], writes=["sgbc"])
        sc.op("vector", lambda e: e.tensor_tensor(out=lamv[:, 0, :], in0=lamv[:, 0, :], in1=lamv[:, 1, :], op=ALU.mult),
              reads=["lamv"], writes=["lamv"])
        sc.op("vector", lambda e: e.tensor_tensor(out=lamv[:, 2, :], in0=lamv[:, 2, :], in1=lamv[:, 3, :], op=ALU.mult),
              reads=["lamv"], writes=["lamv"])
        sc.op("vector", lambda e: e.reduce_sum(out=lam2[:, 0:1], in_=lamv[:, 0, :], axis=AX.X), reads=["lamv"], writes=["lam2"])
        sc.op("vector", lambda e: e.reduce_sum(out=lam2[:, 1:2], in_=lamv[:, 2, :], axis=AX.X), reads=["lamv", "lam2"],
              writes=["lam2"])
        sc.op("scalar", lambda e: e.activation(out=lam2[:, 2:4], in_=lam2[:, 0:2], func=AF.Exp), reads=["lam2"], writes=["lam2"])
        sc.op("vector", lambda e: e.tensor_tensor(out=nlam[:], in0=lam2[:, 3:4], in1=lam2[:, 2:3], op=ALU.subtract),
              reads=["lam2"], writes=["nlam"])
        sc.op("vector", lambda e: e.tensor_scalar(out=nlam[:], in0=nlam[:], scalar1=-LAM_INIT, scalar2=None, op0=ALU.add),
              reads=["nlam"], writes=["nlam"])

        bstop = G.get("bstop", 99)
        x_v = x_d.ap().rearrange("(t p) d -> t p d", p=128)
        x1_v = x1_s.ap().rearrange("(t p) d -> t p d", p=128)
        h2_v = h2_s.ap().rearrange("(t p) d -> t p d", p=128)
        bank = [0]

        def nb():
            b = bank[0]
            bank[0] = (b + 1) % 6
            return b

        def tile(s):
            def load_q(s2):
                sc.dma("sync", "b_qT", lambda e: e.dma_start(out=qT[:], in_=qT_s.ap()[:, :, s2 * 512:(s2 + 1) * 512]),
                       reads=["qk_s0_%d" % s2], writes=["qT"])

            def load_cv(s2):
                sc.dma("sync", "b_cvT", lambda e: e.dma_start(out=cvT[:], in_=cvT_s.ap()[:, :, s2 * 512:(s2 + 1) * 512]),
                       reads=["cvT_s%d" % s2], writes=["cvT"])
            if s == 0:
                load_q(0)
                load_cv(0)
            for j in range(4):
                xj = xr[j % 2]
                xk = "xr%d" % (j % 2)
                sc.dma("sync", "b_" + xk, lambda e, j=j, xj=xj: e.dma_start(out=xj[:], in_=x_v[s * 4 + j]), writes=[xk])
                sc.op("vector", lambda e, j=j: e.memset(ss[:, j:j + 1], 0.0), writes=["ss%d" % j])
                sc.op("scalar", lambda e, j=j, xj=xj: e.activation(out=xn[:, j, :], in_=xj[:], func=AF.Square,
                                                                   scale=1.0 / 32.0, accum_out=ss[:, j:j + 1]),
                      reads=[xk, "ss%d" % j], writes=["xn%d" % j, "ss%d" % j])
                sc.op("vector", lambda e, j=j: e.tensor_scalar(out=rstd[:, j:j + 1], in0=ss[:, j:j + 1], scalar1=EPS,
                                                               scalar2=None, op0=ALU.add), reads=["ss%d" % j], writes=["rstd%d" % j])
                sc.op("gpsimd", lambda e, j=j: e.tensor_tensor(out=rstd[:, j:j + 1], in0=rstd[:, j:j + 1], in1=nhalf[:, 0:1],
                                                               op=ALU.pow), reads=["rstd%d" % j, "nhalf"], writes=["rstd%d" % j])
                sc.op("vector", lambda e, j=j, xj=xj: e.tensor_scalar(
                    out=xn[:, j, :], in0=xj[:], scalar1=rstd[:, j:j + 1], scalar2=None, op0=ALU.mult),
                    reads=[xk, "rstd%d" % j], writes=["xn%d" % j])
            if bstop < 3:
                return
            ei = [0]
            deferred = []
            for h in range(4):
                nkb = 4 * s + 4
                items = [(c, kb) for c in range(2) for kb in range(nkb)]

                def emit_s(idx, h=h):
                    c, kb = items[idx]
                    r0, r1 = c * 64, (c + 1) * 64
                    jmin = max(0, kb - 4 * s)
                    q0 = jmin * 128
                    sb_ = 3 + (idx % 3)
                    sc.op("tensor", lambda e, kb=kb, q0=q0, sb_=sb_, r0=r0, r1=r1, h=h: e.matmul(
                        PS[sb_][:, q0:512], lhsT=KT[r0:r1, h, kb * 128:(kb + 1) * 128], rhs=qT[r0:r1, h, q0:512],
                        start=True, stop=True), reads=["KT", "qT"], writes=["ps%d" % sb_])
                    E = Et[ei[0] % 4]
                    ek = "E%d" % (ei[0] % 4)
                    ei[0] += 1
                    sc.op("scalar", lambda e, E=E, q0=q0, sb_=sb_: e.activation(
                        out=E[:, q0:512], in_=PS[sb_][:, q0:512], func=AF.Exp, scale=0.125),
                        reads=["ps%d" % sb_], writes=[ek])
                    if kb >= 4 * s:
                        sc.op("gpsimd", lambda e, E=E, q0=q0: e.tensor_tensor(
                            out=E[:, q0:q0 + 128], in0=E[:, q0:q0 + 128], in1=tri_b[:], op=ALU.mult),
                            reads=[ek, "tri_b"], writes=[ek])
                    return (E, ek, jmin)

                def emit_av(idx, st_, h=h):
                    c, kb = items[idx]
                    E, ek, jmin = st_

                    def av(e, c=c, kb=kb, jmin=jmin, E=E, h=h, s=s):
                        for j in range(jmin, 4):
                            g = c * 4 + j
                            ob = g // 3
                            o0 = (g % 3) * 160
                            ins = e.matmul(PS[ob][:, o0:o0 + 129], lhsT=E[:, j * 128:(j + 1) * 128],
                                           rhs=V[:, kb, h, 0:129], start=(kb == 0 and g % 3 == 0),
                                           stop=(kb == 4 * s + j), skip_group_check=True)
                        return ins
                    sc.op("tensor", av, reads=[ek, "V%d" % (kb // 4), "Vones"],
                          writes=["ps0", "ps1"] if c == 0 else ["ps1", "ps2"])

                pend = [emit_s(0), emit_s(1)]
                for idx in range(len(items)):
                    if idx + 2 < len(items):
                        pend.append(emit_s(idx + 2))
                    emit_av(idx, pend.pop(0))
                if bstop < 4:
                    continue
                while deferred:
                    deferred.pop(0)()
                for b in range(3):
                    src = lambda b=b: PS[b][:, 0:480].rearrange("p (r c) -> p r c", r=3)[:, :, 0:129]
                    if b % 2 == 0:
                        sc.op("scalar", lambda e, b=b, src=src: e.copy(out=Osb[:, b, :, 0:129], in_=src()),
                              reads=["ps%d" % b], writes=["Osb%d" % b])
                    else:
                        sc.op("vector", lambda e, b=b, src=src: e.tensor_copy(out=Osb[:, b, :, 0:129], in_=src()),
                              reads=["ps%d" % b], writes=["Osb%d" % b])
                for j in range(4):
                    b0, r_ = j // 3, j % 3
                    b1, r1_ = (4 + j) // 3, (4 + j) % 3
                    sc.op("vector", lambda e, b0=b0, r_=r_: e.reciprocal(out=rr[:, 0:1], in_=Osb[:, b0, r_, 128:129]),
                          reads=["Osb%d" % b0], writes=["rr"])
                    sc.op("vector", lambda e, b1=b1, r1_=r1_: e.reciprocal(out=rr[:, 1:2], in_=Osb[:, b1, r1_, 128:129]),
                          reads=["Osb%d" % b1, "rr"], writes=["rr"])
                    sc.op("vector", lambda e: e.tensor_tensor(out=rr[:, 1:2], in0=rr[:, 1:2], in1=nlam[:], op=ALU.mult),
                          reads=["rr", "nlam"], writes=["rr"])
                    sc.op("vector", lambda e, b0=b0, r_=r_: e.tensor_scalar(
                        out=t1[:], in0=Osb[:, b0, r_, 0:128], scalar1=rr[:, 0:1], scalar2=None, op0=ALU.mult),
                        reads=["Osb%d" % b0, "rr"], writes=["t1"])
                    sc.op("vector", lambda e, b1=b1, r1_=r1_: e.scalar_tensor_tensor(
                        out=ot[:], in0=Osb[:, b1, r1_, 0:128], scalar=rr[:, 1:2], in1=t1[:], op0=ALU.mult, op1=ALU.add),
                        reads=["Osb%d" % b1, "rr", "t1"], writes=["ot"])
                    sc.op("vector", lambda e: e.memset(rr[:, 2:3], 0.0), reads=["rr"], writes=["rr"])
                    sc.op("scalar", lambda e: e.activation(out=junk[:], in_=ot[:], func=AF.Square,
                                                           scale=float(128.0 ** -0.5), accum_out=rr[:, 2:3]),
                          reads=["ot", "rr"], writes=["junk", "rr"])
                    sc.op("vector", lambda e: e.tensor_scalar(out=rr[:, 3:4], in0=rr[:, 2:3], scalar1=EPS, scalar2=None,
                                                              op0=ALU.add), reads=["rr"], writes=["rr"])
                    sc.op("gpsimd", lambda e: e.tensor_tensor(out=rr[:, 3:4], in0=rr[:, 3:4], in1=nhalf[:, 0:1], op=ALU.pow),
                          reads=["rr", "nhalf"], writes=["rr"])
                    sc.op("vector", lambda e, j=j: e.scalar_tensor_tensor(
                        out=on4[:, j, :], in0=ot[:], scalar=rr[:, 3:4], in1=sgbc[:], op0=ALU.mult, op1=ALU.mult),
                        reads=["ot", "rr", "sgbc"], writes=["on%d" % j])

                    def tr_on(h=h, j=j):
                        pb = PB[j % 2]
                        pk = "pb%d" % (j % 2)
                        sc.op("tensor", lambda e: e.transpose(out=pb[:, 0:128], in_=on4[:, j, :], identity=ident_b[:]),
                              reads=["on%d" % j, "ident_b"], writes=[pk])
                        sc.op("scalar", lambda e: e.copy(out=aoT[:, h, j * 128:(j + 1) * 128], in_=pb[:, 0:128]),
                              reads=[pk], writes=["aoT"])
                    deferred.append(tr_on)

            while deferred:
                deferred.pop(0)()
            if s + 1 < nst:
                load_q(s + 1)
            for j in range(4):
                pb = PB[j % 2]
                pk = "pb%d" % (j % 2)

                def tr(e, j=j, pb=pb):
                    for kc in range(8):
                        ins = e.transpose(out=pb[:, kc * 128:(kc + 1) * 128], in_=xn[:, j, kc * 128:(kc + 1) * 128],
                                          identity=ident_b[:])
                    return ins
                sc.op("tensor", tr, reads=["xn%d" % j, "ident_b"], writes=[pk])
                sc.op("vector", lambda e, j=j, pb=pb: e.tensor_copy(out=hT[:, :, j * 128:(j + 1) * 128],
                                                                   in_=pb[:].rearrange("p (k t) -> p k t", k=8)),
                      reads=[pk], writes=["hT%d" % j])
            hkeys = ["hT%d" % j for j in range(4)]

            if bstop < 5:
                return
            for m in range(8):
                bA, bB, bC, bD = nb(), nb(), nb(), nb()

                def mm(e, m=m, bA=bA, bB=bB, bC=bC, bD=bD):
                    for kc in range(8):
                        e.matmul(PS[bA][:, :], lhsT=wgt[:, kc, m * 128:(m + 1) * 128], rhs=hT[:, kc, :],
                                 start=(kc == 0), stop=(kc == 7))
                    for kc in range(8):
                        e.matmul(PS[bB][:, :], lhsT=wgt[:, kc, 1024 + m * 128:1024 + (m + 1) * 128], rhs=hT[:, kc, :],
                                 start=(kc == 0), stop=(kc == 7))
                    for kc in range(4):
                        e.matmul(PS[bC][:, :], lhsT=wco[:, kc, m * 128:(m + 1) * 128], rhs=cvT[:, kc, :],
                                 start=(kc == 0), stop=(kc == 3))
                    for kc in range(4):
                        ins = e.matmul(PS[bD][:, :], lhsT=wao[:, kc, m * 128:(m + 1) * 128], rhs=aoT[:, kc, :],
                                       start=(kc == 0), stop=(kc == 3))
                    return ins
                sc.op("tensor", mm, reads=hkeys + ["wgt", "wco", "wao", "cvT", "aoT"],
                      writes=["ps%d" % b for b in (bA, bB, bC, bD)])
                sc.op("scalar", lambda e, bA=bA: e.activation(out=sgs[0][:], in_=PS[bA][:, :], func=AF.Sigmoid),
                      reads=["ps%d" % bA], writes=["sgs0"])
                sc.op("scalar", lambda e, bB=bB: e.activation(out=sgs[1][:], in_=PS[bB][:, :], func=AF.Sigmoid),
                      reads=["ps%d" % bB], writes=["sgs1"])
                sc.op("vector", lambda e, bC=bC: e.tensor_tensor(out=tu[0][:], in0=PS[bC][:, :], in1=sgs[0][:], op=ALU.mult),
                      reads=["ps%d" % bC, "sgs0"], writes=["tu0"])
                sc.op("vector", lambda e, bD=bD: e.tensor_tensor(out=tu[1][:], in0=PS[bD][:, :], in1=sgs[1][:], op=ALU.mult),
                      reads=["ps%d" % bD, "sgs1"], writes=["tu1"])
                sc.op("gpsimd", lambda e, m=m: e.tensor_tensor(out=mT[:, m, :], in0=tu[0][:], in1=tu[1][:], op=ALU.add),
                      reads=["tu0", "tu1"], writes=["mT"])
            if s + 1 < nst:
                load_cv(s + 1)
            if bstop < 6:
                return
            for j in range(4):
                t = s * 4 + j
                xj = xr[j % 2]
                xk = "xr%d" % (j % 2)
                sc.dma("sync", "b_" + xk, lambda e, t=t, xj=xj: e.dma_start(out=xj[:], in_=x_v[t]), writes=[xk])
                for half in range(2):
                    b = nb()

                    def of(e, j=j, half=half, b=b):
                        for kc in range(8):
                            ins = e.matmul(PS[b][:, :], lhsT=mT[:, kc, j * 128:(j + 1) * 128],
                                           rhs=wout[:, kc, half * 512:(half + 1) * 512], start=(kc == 0), stop=(kc == 7))
                        return ins
                    sc.op("tensor", of, reads=["mT", "wout"], writes=["ps%d" % b])
                    sc.op("vector", lambda e, xj=xj, half=half, b=b: e.tensor_tensor(
                        out=xj[:, half * 512:(half + 1) * 512], in0=PS[b][:, :], in1=xj[:, half * 512:(half + 1) * 512],
                        op=ALU.add), reads=["ps%d" % b, xk], writes=[xk])
                sc.dma("sync", "b_x1st%d" % (j % 2), lambda e, t=t, xj=xj: e.dma_start(out=x1_v[t], in_=xj[:]), reads=[xk],
                       writes=["x1_s%d" % t])
                if bstop < 7:
                    continue
                sc.op("vector", lambda e, j=j: e.memset(ss[:, 4 + j:5 + j], 0.0), writes=["ss%d" % (4 + j)])
                sc.op("scalar", lambda e, j=j, xj=xj: e.activation(out=h2b, in_=xj[:], func=AF.Square,
                                                                   scale=1.0 / 32.0, accum_out=ss[:, 4 + j:5 + j]),
                      reads=[xk, "ss%d" % (4 + j)], writes=["h2b", "E0", "E1", "ss%d" % (4 + j)])
                sc.op("vector", lambda e, j=j: e.tensor_scalar(out=rstd[:, 4 + j:5 + j], in0=ss[:, 4 + j:5 + j], scalar1=EPS,
                                                               scalar2=None, op0=ALU.add),
                      reads=["ss%d" % (4 + j)], writes=["rstd%d" % (4 + j)])
                sc.op("gpsimd", lambda e, j=j: e.tensor_tensor(out=rstd[:, 4 + j:5 + j], in0=rstd[:, 4 + j:5 + j],
                                                               in1=nhalf[:, 0:1], op=ALU.pow),
                      reads=["rstd%d" % (4 + j), "nhalf"], writes=["rstd%d" % (4 + j)])
                sc.op("vector", lambda e, j=j, xj=xj: e.scalar_tensor_tensor(
                    out=h2[:], in0=xj[:], scalar=rstd[:, 4 + j:5 + j], in1=g2bc[:], op0=ALU.mult, op1=ALU.mult),
                    reads=[xk, "rstd%d" % (4 + j), "g2bc"], writes=["h2"])
                sc.op("gpsimd", lambda e: e.tensor_copy(out=h2b, in_=h2[:]), reads=["h2"], writes=["h2b", "E0", "E1"])
                sc.dma("sync", "b_h2st", lambda e, t=t: e.dma_start(out=h2_v[t], in_=h2b), reads=["h2b", "E0", "E1"],
                       writes=["h2_s%d" % t])
                if bstop < 7.5:
                    continue
                for half in range(2):
                    b = nb()

                    def trf(e, half=half, b=b):
                        for k4 in range(4):
                            kc = half * 4 + k4
                            ins = e.transpose(out=PS[b][:, k4 * 128:(k4 + 1) * 128], in_=h2[:, kc * 128:(kc + 1) * 128],
                                              identity=cst[:, 0:128])
                        return ins
                    sc.op("tensor", trf, reads=["h2", "cst"], writes=["ps%d" % b])
                    sc.op("scalar" if half else "vector",
                          (lambda e, half=half, b=b: e.copy(out=h2T[:, half * 4:(half + 1) * 4, :],
                                                            in_=PS[b][:, :].rearrange("p (k t) -> p k t", k=4))) if half else
                          (lambda e, half=half, b=b: e.tensor_copy(out=h2T[:, half * 4:(half + 1) * 4, :],
                                                                   in_=PS[b][:, :].rearrange("p (k t) -> p k t", k=4))),
                          reads=["ps%d" % b], writes=["h2T%d" % half])
                if bstop < 7.8:
                    continue
                b = nb()

                def rf(e, b=b):
                    for kc in range(8):
                        ins = e.matmul(PS[b][:, 0:36], lhsT=h2T[:, kc, :], rhs=wr[:, kc, :], start=(kc == 0), stop=(kc == 7))
                    return ins
                sc.op("tensor", rf, reads=["h2T0", "h2T1", "wr"], writes=["ps%d" % b])
                sc.op("vector", lambda e, t=t, b=b: e.tensor_tensor(out=LG[:, t, :], in0=PS[b][:, 0:36], in1=rbbc[:],
                                                                    op=ALU.add),
                      reads=["ps%d" % b, "rbbc"], writes=["LG%d" % t])

        for s in range(nst if bstop >= 2 else 0):
            tile(s)
        for en in sc.ENGS:
            sc.wait_all(en)


def phase_c(nc, sc, G):
    names = ["LG", "dest_i", "wts", "cst", "ident_b", "PS", "PB", "eoff_d", "h2_s", "XS", "YS", "w_g_d", "w_u_d", "w_d_d"]
    LG, dest_i, wts, cst, ident_b, PS, PB, eoff_d, h2_s, XS, YS, w_g_d, w_u_d, w_d_d = (G[k] for k in names)
    ntile = G["nst"] * 4
    NR = NE * CAP
    with contextlib.ExitStack() as st:
        sb = lambda name, shape, dt=F32: st.enter_context(nc.sbuf_tensor("sb_" + name, list(shape), dt))
        xb = [sb("c_xb%d" % i, [128, D], BF16) for i in range(2)]
        U_b = sb("c_Ub", [128, 128], BF16)
        ones_b = sb("c_onesb", [128, 128], BF16)
        eoff = sb("c_eoff", [128, NE])
        with contextlib.ExitStack() as st2:
            sb2 = lambda name, shape, dt=F32: st2.enter_context(nc.sbuf_tensor("sb_" + name, list(shape), dt))
            GLd = sb2("r_GLd", [128, 32, 4])
            ohg = sb2("r_ohg", [128, 32, 4])
            pen = sb2("r_pen", [128, 32, 4])
            sm = sb2("r_sm", [128, 8, 32])
            ELm = sb2("r_ELm", [128, 32, 4, 8])
            oh1 = sb2("r_oh1", [128, 32, 32])
            EL2 = sb2("r_EL2", [128, 32, 32])
            oh2 = sb2("r_oh2", [128, 32, 32])
            Mb = sb2("r_Mb", [128, 32, 32], BF16)
            pre = sb2("r_pre", [128, 32, 32])
            tot = sb2("r_tot", [128, 32, 32])
            base = sb2("r_base", [128, 32, 32])
            prod = sb2("r_prod", [128, 32, 32])
            destf = sb2("r_destf", [128, 2, 32])
            ELm3 = ELm[:].rearrange("p t g j -> p t (g j)")
            gmax, gsum, gw, m1, m2, dm, rsel, esel = (sm[:, i, :] for i in range(8))
            bc4 = lambda a: a.unsqueeze(2).broadcast_to([128, 32, 4])
            bc32 = lambda a: a.unsqueeze(2).broadcast_to([128, 32, 32])
            LK = ["LG%d" % i for i in range(32)]
            V_ = "vector"
            sc.dma("sync", "c_eoff", lambda e: e.dma_start(out=eoff[:], in_=eoff_d.ap()), writes=["eoff"])
            sc.op(V_, lambda e: e.tensor_copy(out=U_b[:], in_=cst[:, 256:384]), reads=["cst"], writes=["U_b"])
            sc.op(V_, lambda e: e.memset(ones_b[:], 1.0), writes=["ones_b"])
            sc.op(V_, lambda e: e.reduce_max(out=gmax, in_=LG[:, :, 0:4], axis=AX.X), reads=LK, writes=["sm"])
            sc.op(V_, lambda e: e.tensor_tensor(out=GLd[:], in0=LG[:, :, 0:4], in1=bc4(gmax), op=ALU.subtract),
                  reads=LK + ["sm"], writes=["GLd"])
            sc.op(V_, lambda e: e.tensor_scalar(out=ohg[:], in0=GLd[:], scalar1=0.0, scalar2=None, op0=ALU.is_equal),
                  reads=["GLd"], writes=["ohg"])
            sc.op("scalar", lambda e: e.activation(out=GLd[:], in_=GLd[:], func=AF.Exp), reads=["GLd", "ohg"], writes=["GLd"])
            sc.op(V_, lambda e: e.reduce_sum(out=gsum, in_=GLd[:], axis=AX.X), reads=["GLd", "sm"], writes=["sm"])
            sc.op(V_, lambda e: e.reciprocal(out=gw, in_=gsum), reads=["sm"], writes=["sm"])
            sc.op(V_, lambda e: e.tensor_scalar(out=pen[:], in0=ohg[:], scalar1=-1.0, scalar2=1e30, op0=ALU.add, op1=ALU.mult),
                  reads=["ohg"], writes=["pen"])
            sc.op(V_, lambda e: e.tensor_tensor(out=ELm[:], in0=LG[:, :, 4:36].rearrange("p t (g j) -> p t g j", g=4),
                                                in1=pen[:].unsqueeze(3).broadcast_to([128, 32, 4, 8]), op=ALU.add),
                  reads=LK + ["pen"], writes=["ELm"])
            sc.op(V_, lambda e: e.reduce_max(out=m1, in_=ELm3, axis=AX.X), reads=["ELm", "sm"], writes=["sm"])
            sc.op(V_, lambda e: e.tensor_tensor(out=oh1[:], in0=ELm3, in1=bc32(m1), op=ALU.is_equal),
                  reads=["ELm", "sm"], writes=["oh1"])
            sc.op(V_, lambda e: e.scalar_tensor_tensor(out=EL2[:], in0=oh1[:], scalar=-1e30, in1=ELm3, op0=ALU.mult,
                                                       op1=ALU.add), reads=["oh1", "ELm"], writes=["EL2"])
            sc.op(V_, lambda e: e.reduce_max(out=m2, in_=EL2[:], axis=AX.X), reads=["EL2", "sm"], writes=["sm"])
            sc.op(V_, lambda e: e.tensor_tensor(out=oh2[:], in0=EL2[:], in1=bc32(m2), op=ALU.is_equal),
                  reads=["EL2", "sm"], writes=["oh2"])
            sc.op(V_, lambda e: e.tensor_tensor(out=dm, in0=m2, in1=m1, op=ALU.subtract), reads=["sm"], writes=["sm"])
            sc.op("scalar", lambda e: e.activation(out=dm, in_=dm, func=AF.Exp), reads=["sm"], writes=["sm"])
            sc.op(V_, lambda e: e.tensor_scalar(out=dm, in0=dm, scalar1=1.0, scalar2=None, op0=ALU.add), reads=["sm"], writes=["sm"])
            sc.op(V_, lambda e: e.reciprocal(out=dm, in_=dm), reads=["sm"], writes=["sm"])
            sc.op(V_, lambda e: e.tensor_tensor(out=wts[:, 0, :], in0=gw, in1=dm, op=ALU.mult), reads=["sm"], writes=["wts"])
            sc.op(V_, lambda e: e.tensor_tensor(out=wts[:, 1, :], in0=gw, in1=wts[:, 0, :], op=ALU.subtract),
                  reads=["sm", "wts"], writes=["wts"])
            sc.op(V_, lambda e: e.tensor_tensor(out=Mb[:], in0=oh1[:], in1=oh2[:], op=ALU.add), reads=["oh1", "oh2"], writes=["Mb"])
            for half in range(2):
                rhs = Mb[:, half * 16:(half + 1) * 16, :].rearrange("p t e -> p (t e)")
                sc.op("tensor", lambda e, half=half, rhs=rhs: e.matmul(PS[half][:, :], lhsT=U_b[:], rhs=rhs, start=True, stop=True),
                      reads=["Mb", "U_b"], writes=["ps%d" % half])
                sc.op("tensor", lambda e, half=half, rhs=rhs: e.matmul(PS[2 + half][:, :], lhsT=ones_b[:], rhs=rhs, start=True,
                                                                       stop=True),
                      reads=["Mb", "ones_b"], writes=["ps%d" % (2 + half)])
                sc.op(V_, lambda e, half=half: e.tensor_copy(
                    out=pre[:, half * 16:(half + 1) * 16, :].rearrange("p t e -> p (t e)"), in_=PS[half][:, :]),
                    reads=["ps%d" % half], writes=["pre"])
                sc.op("scalar", lambda e, half=half: e.copy(
                    out=tot[:, half * 16:(half + 1) * 16, :].rearrange("p t e -> p (t e)"), in_=PS[2 + half][:, :]),
                    reads=["ps%d" % (2 + half)], writes=["tot"])
            sc.op(V_, lambda e: e.memset(base[:, 0, :], 0.0), writes=["base"])
            for t in range(1, 32):
                sc.op(V_, lambda e, t=t: e.tensor_tensor(out=base[:, t, :], in0=base[:, t - 1, :], in1=tot[:, t - 1, :],
                                                         op=ALU.add), reads=["base", "tot"], writes=["base"])
            sc.op(V_, lambda e: e.tensor_tensor(out=pre[:], in0=pre[:], in1=base[:], op=ALU.add), reads=["pre", "base"],
                  writes=["pre"])
            for k, oh in enumerate([oh1, oh2]):
                ohk = "oh%d" % (k + 1)
                sc.op(V_, lambda e, oh=oh: e.tensor_tensor(out=prod[:], in0=oh[:], in1=pre[:], op=ALU.mult),
                      reads=[ohk, "pre"], writes=["prod"])
                sc.op(V_, lambda e: e.reduce_sum(out=rsel, in_=prod[:], axis=AX.X), reads=["prod", "sm"], writes=["sm"])
                sc.op(V_, lambda e, oh=oh: e.tensor_tensor(out=prod[:], in0=oh[:],
                                                           in1=eoff[:].unsqueeze(1).broadcast_to([128, 32, 32]), op=ALU.mult),
                      reads=[ohk, "eoff", "sm"], writes=["prod"])
                sc.op(V_, lambda e: e.reduce_sum(out=esel, in_=prod[:], axis=AX.X), reads=["prod", "sm"], writes=["sm"])
                sc.op(V_, lambda e, k=k: e.tensor_tensor(out=destf[:, k, :], in0=rsel, in1=esel, op=ALU.add),
                      reads=["sm"], writes=["destf"])
                sc.op(V_, lambda e: e.tensor_scalar(out=rsel, in0=rsel, scalar1=float(CAP), scalar2=1e6, op0=ALU.is_ge,
                                                    op1=ALU.mult), reads=["sm", "destf"], writes=["sm"])
                sc.op(V_, lambda e, k=k: e.tensor_tensor(out=destf[:, k, :], in0=destf[:, k, :], in1=rsel, op=ALU.add),
                      reads=["sm", "destf"], writes=["destf"])
            sc.op(V_, lambda e: e.tensor_copy(out=dest_i[:], in_=destf[:]), reads=["destf"], writes=["dest"])
            if G.get("dbg"):
                rt_o = G["rt_o"]
                sc.dma("sync", "dbg3", lambda e: e.dma_start(out=rt_o.ap()[:, 0:2, :], in_=destf[:]), reads=["destf", "dest"],
                       writes=["rt_o"])
                sc.dma("sync", "dbg4", lambda e: e.dma_start(out=rt_o.ap()[:, 2:4, :], in_=wts[:]), reads=["wts"], writes=["rt_o2"])
            for en in sc.ENGS:
                sc.wait_all(en)

        stage_raw = [sb("c_stage%d" % i, [128, 4096]) for i in range(3)]
        wg = [sb("c_wg%d" % i, [128, 8, 512], BF16) for i in range(2)]
        wu = [sb("c_wu%d" % i, [128, 8, 512], BF16) for i in range(2)]
        wd = [sb("c_wd%d" % i, [128, 4, D], BF16) for i in range(2)]
        xb4 = [sb("c_xb4_%d" % i, [128, CAP // 128, D], BF16) for i in range(2)]
        xbT4 = [sb("c_xbT4_%d" % i, [128, 8, CAP], BF16) for i in range(2)]
        sgt2 = [sb("c_sgt%d" % i, [128, 512]) for i in range(2)]
        hidT4 = [sb("c_hid4_%d" % i, [128, 4, CAP], BF16) for i in range(2)]
        yo = [sb("c_yo%d" % i, [128, D]) for i in range(2)]
        h2_v = h2_s.ap().rearrange("(t p) d -> t p d", p=128)
        for t in range(ntile):
            x_ = xb[t % 2]
            xk = "xb%d" % (t % 2)
            sc.dma("sync", "c_" + xk, lambda e, t=t, x_=x_: e.dma_start(out=x_[:], in_=h2_v[t]),
                   reads=["h2_s%d" % t], writes=[xk])
            for k in range(2):
                sc.dma("gpsimd", "c_sc%d_%d" % (t % 2, k), lambda e, t=t, k=k, x_=x_: e.indirect_dma_start(
                    out=XS.ap(), out_offset=bass.IndirectOffsetOnAxis(ap=dest_i[:, k, t:t + 1], axis=0),
                    in_=x_[:], in_offset=None, bounds_check=G["bnd"]["r"], oob_is_err=False),
                    reads=[xk, "dest", "XS"], writes=["XSk%d" % k])

        nexp = G.get("nexp", NE)
        sg_v = stage_raw[0][:].rearrange("p (k n) -> p k n", k=8)
        su_v = stage_raw[1][:].rearrange("p (k n) -> p k n", k=8)
        sd_v = stage_raw[2][:].rearrange("p (k n) -> p k n", k=4)

        def load_w(ex):
            sc.dma("sync", "c_st0", lambda e: e.dma_start(out=sg_v, in_=w_g_d.ap()[ex].rearrange("(kc p) n -> p kc n", p=128)),
                   writes=["st0"])
            sc.dma("sync", "c_st1", lambda e: e.dma_start(out=su_v, in_=w_u_d.ap()[ex].rearrange("(kc p) n -> p kc n", p=128)),
                   writes=["st1"])
            sc.dma("sync", "c_st2", lambda e: e.dma_start(out=sd_v, in_=w_d_d.ap()[ex].rearrange("(kc p) n -> p kc n", p=128)),
                   writes=["st2"])

        def cast_w(ex):
            p2 = ex % 2
            sc.op("scalar", lambda e: e.copy(out=wg[p2][:], in_=sg_v), reads=["st0"], writes=["wg%d" % p2])
            sc.op("vector", lambda e: e.tensor_copy(out=wu[p2][:], in_=su_v), reads=["st1"], writes=["wu%d" % p2])
            sc.op("vector", lambda e: e.tensor_copy(out=wd[p2][:], in_=sd_v), reads=["st2"], writes=["wd%d" % p2])

        def load_xb(ex):
            p2 = ex % 2
            sc.dma("sync", "c_xb4_%d" % p2, lambda e: e.dma_start(
                out=xb4[p2][:], in_=XS.ap()[ex * CAP:(ex + 1) * CAP, :].rearrange("(r p) d -> p r d", p=128)),
                reads=["XS", "XSk0", "XSk1"], writes=["xb4_%d" % p2])

        load_xb(0)
        load_w(0)
        for ex in range(nexp):
            p2 = ex % 2
            cast_w(ex)
            if ex + 1 < nexp:
                load_xb(ex + 1)
                load_w(ex + 1)
            NB = CAP // 128
            for r in range(NB):
                pb = PB[r % 2]
                pk = "pb%d" % (r % 2)

                def tr(e, r=r, pb=pb, p2=p2):
                    for kc in range(8):
                        ins = e.transpose(out=pb[:, kc * 128:(kc + 1) * 128], in_=xb4[p2][:, r, kc * 128:(kc + 1) * 128],
                                          identity=ident_b[:])
                    return ins
                sc.op("tensor", tr, reads=["xb4_%d" % p2, "ident_b"], writes=[pk])
                sc.op("vector" if r % 2 else "scalar",
                      (lambda e, r=r, pb=pb, p2=p2: e.tensor_copy(out=xbT4[p2][:, :, r * 128:(r + 1) * 128],
                                                                 in_=pb[:].rearrange("p (k t) -> p k t", k=8))) if r % 2 else
                      (lambda e, r=r, pb=pb, p2=p2: e.copy(out=xbT4[p2][:, :, r * 128:(r + 1) * 128],
                                                           in_=pb[:].rearrange("p (k t) -> p k t", k=8))),
                      reads=[pk], writes=["xbT4_%d_%d" % (p2, r)])
            xkeys = ["xbT4_%d_%d" % (p2, r) for r in range(NB)]
            for f in range(4):
                bg, bu = (0, 1) if f % 2 == 0 else (2, 3)

                def gu(e, p2=p2, f=f, bg=bg, bu=bu):
                    for (bb, w) in ((bg, wg[p2]), (bu, wu[p2])):
                        for kc in range(8):
                            ins = e.matmul(PS[bb][:, 0:CAP], lhsT=w[:, kc, f * 128:(f + 1) * 128], rhs=xbT4[p2][:, kc, :],
                                           start=(kc == 0), stop=(kc == 7))
                    return ins
                sc.op("tensor", gu, reads=xkeys + ["wg%d" % p2, "wu%d" % p2], writes=["ps%d" % bg, "ps%d" % bu])
                sg_ = sgt2[f % 2]
                sc.op("scalar", lambda e, bg=bg, sg_=sg_: e.activation(out=sg_[:, 0:CAP], in_=PS[bg][:, 0:CAP], func=AF.Silu),
                      reads=["ps%d" % bg], writes=["sgt%d" % (f % 2)])
                sc.op("vector", lambda e, bu=bu, sg_=sg_, f=f, p2=p2: e.tensor_tensor(
                    out=hidT4[p2][:, f, :], in0=PS[bu][:, 0:CAP], in1=sg_[:, 0:CAP], op=ALU.mult),
                    reads=["ps%d" % bu, "sgt%d" % (f % 2)], writes=["hid4_%d" % p2])
            for r in range(NB):
                row0 = ex * CAP + r * 128
                i2 = r % 2
                for half in range(2):
                    bd = 4 + half

                    def dn(e, half=half, bd=bd, r=r, p2=p2):
                        for fc in range(4):
                            ins = e.matmul(PS[bd][:, :], lhsT=hidT4[p2][:, fc, r * 128:(r + 1) * 128],
                                           rhs=wd[p2][:, fc, half * 512:(half + 1) * 512], start=(fc == 0), stop=(fc == 3))
                        return ins
                    sc.op("tensor", dn, reads=["hid4_%d" % p2, "wd%d" % p2], writes=["ps%d" % bd])
                    if half == 0:
                        sc.op("scalar", lambda e, i2=i2, bd=bd: e.copy(out=yo[i2][:, 0:512], in_=PS[bd][:, :]),
                              reads=["ps%d" % bd], writes=["yo%d" % i2])
                    else:
                        sc.op("vector", lambda e, i2=i2, bd=bd: e.tensor_copy(out=yo[i2][:, 512:1024], in_=PS[bd][:, :]),
                              reads=["ps%d" % bd], writes=["yo%d" % i2])
                sc.dma("sync", "c_yo%d" % i2, lambda e, row0=row0, i2=i2: e.dma_start(out=YS.ap()[row0:row0 + 128, :], in_=yo[i2][:]),
                       reads=["yo%d" % i2], writes=["YS"])
        for en in sc.ENGS:
            sc.wait_all(en)


def phase_d(nc, sc, G):
    names = ["dest_i", "wts", "x1_s", "YS", "out_d"]
    dest_i, wts, x1_s, YS, out_d = (G[k] for k in names)
    ntile = G["nst"] * 4
    NBUF = 4
    with contextlib.ExitStack() as st:
        sb = lambda name, shape, dt=F32: st.enter_context(nc.sbuf_tensor("sb_" + name, list(shape), dt))
        x1t = [sb("d_x1%d" % i, [128, D]) for i in range(NBUF)]
        y1 = [sb("d_y1%d" % i, [128, D]) for i in range(NBUF)]
        y2 = [sb("d_y2%d" % i, [128, D]) for i in range(NBUF)]
        x1_v = x1_s.ap().rearrange("(t p) d -> t p d", p=128)
        out_v = out_d.ap().rearrange("(t p) d -> t p d", p=128)

        def prep(t):
            i2 = t % NBUF
            sc.dma("sync", "d_x1%d" % i2, lambda e: e.dma_start(out=x1t[i2][:], in_=x1_v[t]),
                   reads=["x1_s%d" % t], writes=["dx1%d" % i2])
            for k, yt in enumerate((y1, y2)):
                yk = "dy%d_%d" % (k, i2)
                sc.op("vector", lambda e, yt=yt: e.memset(yt[i2][:], 0.0), writes=[yk])
                sc.dma("gpsimd", "d_g%d_%d" % (k, i2), lambda e, yt=yt, k=k: e.indirect_dma_start(
                    out=yt[i2][:], out_offset=None, in_=YS.ap(),
                    in_offset=bass.IndirectOffsetOnAxis(ap=dest_i[:, k, t:t + 1], axis=0),
                    bounds_check=G["bnd"]["r"], oob_is_err=False), reads=["YS", "dest"], writes=[yk])

        def finish(t):
            i2 = t % NBUF
            sc.op("vector", lambda e: e.scalar_tensor_tensor(
                out=x1t[i2][:], in0=y1[i2][:], scalar=wts[:, 0, t:t + 1], in1=x1t[i2][:], op0=ALU.mult, op1=ALU.add),
                reads=["dy0_%d" % i2, "wts", "dx1%d" % i2], writes=["dx1%d" % i2])
            sc.op("vector", lambda e: e.scalar_tensor_tensor(
                out=x1t[i2][:], in0=y2[i2][:], scalar=wts[:, 1, t:t + 1], in1=x1t[i2][:], op0=ALU.mult, op1=ALU.add),
                reads=["dy1_%d" % i2, "wts", "dx1%d" % i2], writes=["dx1%d" % i2])
            sc.dma("sync", "d_o%d" % i2, lambda e: e.dma_start(out=out_v[t], in_=x1t[i2][:]),
                   reads=["dx1%d" % i2], writes=["out%d" % t])

        AHEAD = 2
        for t in range(min(AHEAD, ntile)):
            prep(t)
        for t in range(ntile):
            if t + AHEAD < ntile:
                prep(t + AHEAD)
            finish(t)
        for en in sc.ENGS:
            sc.wait_all(en)


def _consts():
    c = np.zeros((128, 512), np.float32)
    c[:, 0:128] = np.eye(128, dtype=np.float32)
    k = np.arange(128)[:, None]
    q = np.arange(128)[None, :]
    c[:, 128:256] = (k <= q).astype(np.float32)
    c[:, 256:384] = (k < q).astype(np.float32)
    c[:, 384:512] = ((k // 64) == (q // 64)).astype(np.float32)
    return c


def make_in_maps(inputs, cores):
    f = lambda a: np.ascontiguousarray(np.asarray(a, dtype=np.float32))
    w_in = f(inputs["w_in"][0])
    g1T = f(inputs["attn_norm_g"][0].reshape(8, 128).T)
    cw = f(inputs["conv_dw_w"][0].reshape(31, 4, 128).transpose(2, 1, 0))
    cvec = f(np.stack([inputs["conv_dw_b"][0].reshape(4, 128).T, inputs["conv_ln_g"][0].reshape(4, 128).T,
                       inputs["conv_ln_b"][0].reshape(4, 128).T], axis=-1))
    qkg = f(np.stack([np.tile(inputs["q_norm_g"][0], 2), np.tile(inputs["k_norm_g"][0], 2)], axis=-1))
    cst = _consts()
    rep = lambda v: f(np.broadcast_to(np.asarray(v, np.float32)[None], (128,) + tuple(np.shape(v))))
    shared = {
        "w_in": w_in, "cst": cst, "g1T": g1T, "cw": cw, "cvec": cvec, "qkg": qkg,
        "w_co": f(inputs["w_conv_out"][0]), "w_ao": f(inputs["w_attn_out"][0]), "w_out": f(inputs["w_out"][0]),
        "g2bc": rep(inputs["ffn_norm_g"][0]),
        "lamv": rep(np.stack([inputs["lambda_q1"][0], inputs["lambda_k1"][0], inputs["lambda_q2"][0], inputs["lambda_k2"][0]])),
        "sgbc": rep(inputs["subln_g"][0]),
        "wr": f(np.concatenate([inputs["w_router_group"][0], inputs["w_router_expert"][0]], axis=1)),
        "rbbc": rep(np.concatenate([inputs["b_router_group"][0], inputs["b_router_expert"][0]])),
        "eoff": rep(np.arange(NE, dtype=np.float32) * CAP),
        "w_g": f(inputs["w_gate_e"][0]), "w_u": f(inputs["w_up_e"][0]), "w_d": f(inputs["w_down_e"][0]),
    }
    maps = []
    for c in cores:
        m = dict(shared)
        m["x"] = f(inputs["x"][c])
        maps.append(m)
    return maps


def kernel(**inputs):
    nc = build_program()
    maps = make_in_maps(inputs, list(range(8)))
    res = run_bass_kernel_spmd(nc, maps, core_ids=list(range(8)))
    return np.stack([np.asarray(r["out"]).reshape(S, D) for r in res.results], axis=0).astype(np.float32)
```

```python
import contextlib
import numpy as np
import concourse.bass as bass
import concourse.mybir as mybir
from concourse.bass_utils import run_bass_kernel_spmd

F32 = mybir.dt.float32
BF16 = mybir.dt.bfloat16
I32 = mybir.dt.int32
ALU = mybir.AluOpType
AF = mybir.ActivationFunctionType
AX = mybir.AxisListType

S = 4096
D = 1024
NST = 8
EPS = 1e-6
LAM_INIT = 0.8 - 0.6 * 1.0
CAP = 512
NE = 32


class Sched:
    ENGS = ["sync", "scalar", "vector", "gpsimd", "tensor"]

    def __init__(self, nc, stack, n_dma_sems=80):
        self.nc = nc
        self.streams = {e: [] for e in self.ENGS}
        self.sems = {}
        self.count = {}
        for e in self.ENGS:
            self.sems["e:" + e] = stack.enter_context(nc.semaphore("sem_" + e))
            self.count["e:" + e] = 0
        self.free_dma = [stack.enter_context(nc.semaphore("dsem%d" % i)) for i in range(n_dma_sems)]
        self.waited = {e: {} for e in self.ENGS}
        self.last_w = {}
        self.readers = {}

    def _dsem(self, key):
        k = "d:" + key
        if k not in self.sems:
            self.sems[k] = self.free_dma.pop()
            self.count[k] = 0
        return k

    def _deps(self, reads, writes):
        toks = []
        for k in reads:
            t = self.last_w.get(k)
            if t is not None:
                toks.append(t)
        for k in writes:
            t = self.last_w.get(k)
            if t is not None:
                toks.append(t)
            for sk, v in self.readers.get(k, {}).items():
                toks.append((sk, v))
        return toks

    def _wait(self, eng, toks):
        w = self.waited[eng]
        need = {}
        for sk, v in toks:
            if w.get(sk, 0) >= v:
                continue
            if eng == "tensor" and sk == "e:tensor":
                continue
            if need.get(sk, 0) < v:
                need[sk] = v
        for sk, v in need.items():
            w[sk] = v
            sem = self.sems[sk]
            self.streams[eng].append(lambda e, sem=sem, v=v: e.wait_ge(sem, v))

    def _commit(self, tok, reads, writes):
        sk, v = tok
        for k in reads:
            r = self.readers.setdefault(k, {})
            if r.get(sk, 0) < v:
                r[sk] = v
        for k in writes:
            self.last_w[k] = tok
            self.readers[k] = {}

    def op(self, eng, fn, reads=(), writes=()):
        self._wait(eng, self._deps(reads, writes))
        sk = "e:" + eng
        self.count[sk] += 1
        n = self.count[sk]
        sem = self.sems[sk]
        self.streams[eng].append(lambda e, fn=fn, sem=sem: fn(e).then_inc(sem, 1))
        tok = (sk, n)
        self._commit(tok, reads, writes)
        return tok

    def dma(self, q, semkey, fn, reads=(), writes=()):
        self._wait(q, self._deps(reads, writes))
        sk = self._dsem(semkey)
        self.count[sk] += 16
        n = self.count[sk]
        sem = self.sems[sk]
        self.streams[q].append(lambda e, fn=fn, sem=sem: fn(e).then_inc(sem, 16))
        tok = (sk, n)
        self._commit(tok, reads, writes)
        return tok

    def wait_all(self, eng):
        toks = []
        for k, t in self.last_w.items():
            toks.append(t)
        for k, r in self.readers.items():
            for sk, v in r.items():
                toks.append((sk, v))
        self._wait(eng, toks)

    def flush(self):
        with self.nc.Block() as block:
            for name in self.ENGS:
                fns = self.streams[name]
                if not fns:
                    continue

                def body(e, fns=fns):
                    for f in fns:
                        f(e)
                getattr(block, name)(body)
        self.streams = {e: [] for e in self.ENGS}


def build_program(dbg=False, phases=("A", "B", "C", "D"), nst=NST, stop=99, nexp=NE, bstop=99):
    nc = bass.Bass("TRN2", target_bir_lowering=False)
    okind = "ExternalOutput" if dbg else "Internal"

    def din(name, shape, dt=F32):
        return nc.dram_tensor(name, list(shape), dt, kind="ExternalInput")

    x_d = din("x", [S, D])
    w_in_d = din("w_in", [D, 4608])
    cst_d = din("cst", [128, 128 * 4])
    g1T_d = din("g1T", [128, 8])
    cw_d = din("cw", [128, 4, 31])
    cvec_d = din("cvec", [128, 4, 3])
    qkg_d = din("qkg", [128, 2])
    w_co_d = din("w_co", [512, D])
    w_ao_d = din("w_ao", [512, D])
    w_out_d = din("w_out", [D, D])
    g2bc_d = din("g2bc", [128, D])
    lamv_d = din("lamv", [128, 4, 64])
    sgbc_d = din("sgbc", [128, 128])
    wr_d = din("wr", [D, 36])
    rbbc_d = din("rbbc", [128, 36])
    eoff_d = din("eoff", [128, NE])
    w_g_d = din("w_g", [NE, D, 512])
    w_u_d = din("w_u", [NE, D, 512])
    w_d_d = din("w_d", [NE, 512, D])
    out_d = nc.dram_tensor("out", [S, D], F32, kind="ExternalOutput")
    x1_s = nc.dram_tensor("x1_s", [S, D], F32, kind=okind)
    h2_s = nc.dram_tensor("h2_s", [S, D], BF16, kind=okind)
    XS = nc.dram_tensor("XS", [NE * CAP, D], BF16, kind="Internal")
    YS = nc.dram_tensor("YS", [NE * CAP, D], F32, kind="Internal")
    if dbg:
        lg_o = nc.dram_tensor("lg_o", [128, 32, 36], F32, kind="ExternalOutput")
        rt_o = nc.dram_tensor("rt_o", [128, 4, 32], F32, kind="ExternalOutput")
    qT_s = nc.dram_tensor("qT_s", [128, 4, S], BF16, kind=okind)
    cvT_s = nc.dram_tensor("cvT_s", [128, 4, S], BF16, kind=okind)
    kT_s = nc.dram_tensor("kT_s", [128, 4, S], BF16, kind=okind)
    if dbg:
        v_o = nc.dram_tensor("v_o", [128, 32, 4, 130], BF16, kind="ExternalOutput")

    with contextlib.ExitStack() as st:
        sc = Sched(nc, st)
        sb = lambda name, shape, dt=F32: st.enter_context(nc.sbuf_tensor("sb_" + name, list(shape), dt))
        cst = sb("cst", [128, 512])
        ident_b = sb("ident_b", [128, 128], BF16)
        V = sb("V", [128, 32, 4, 130], BF16)
        PS = [st.enter_context(nc.psum_tensor("ps%d" % i, [128, 512], F32)) for i in range(6)]
        PB = [st.enter_context(nc.psum_tensor("pb%d" % i, [128, 1024], BF16)) for i in range(2)]
        nhalf = sb("nhalf", [128, 512])
        bnd = {}

        def _mk_bnd(e):
            bnd["r"] = e.alloc_register("bnd")
            e.reg_mov(bnd["r"], NE * CAP - 1)
        sc.streams["gpsimd"].append(_mk_bnd)
        LG = sb("LG", [128, 32, 36])
        dest_i = sb("dest_i", [128, 2, 32], I32)
        wts = sb("wts", [128, 2, 32])
        if dbg:
            sc.op("vector", lambda e: e.memset(LG[:], 0.0), writes=["LG%d" % i for i in range(32)])
        zeros_b = sb("zeros_b", [128, D], BF16)
        sc.op("vector", lambda e: e.memset(zeros_b[:], 0.0), writes=["zeros_b"])
        XS_v = XS.ap().rearrange("(r p) d -> r p d", p=128)
        for r in range(NE * CAP // 128):
            sc.dma("sync", "xs_zero", lambda e, r=r: e.dma_start(out=XS_v[r], in_=zeros_b[:]),
                   reads=["zeros_b"], writes=["XS"])
        sc.op("gpsimd", lambda e: e.memset(nhalf[:], -0.5), writes=["nhalf"])

        sc.dma("sync", "cst", lambda e: e.dma_start(out=cst[:], in_=cst_d.ap()), writes=["cst"])
        sc.op("vector", lambda e: e.tensor_copy(out=ident_b[:], in_=cst[:, 0:128]), reads=["cst"], writes=["ident_b"])
        sc.op("gpsimd", lambda e: e.memset(V[:], 1.0), writes=["Vones"] + ["V%d" % i for i in range(NST)])

        if "A" in phases:
            phase_a(nc, sc, locals())
        if "B" in phases:
            phase_b(nc, sc, locals())
        if dbg and "B" in phases:
            sc.dma("sync", "dbg2", lambda e: e.dma_start(out=lg_o.ap(), in_=LG[:]),
                   reads=["LG%d" % i for i in range(32)], writes=["lg_o"])
        if "C" in phases:
            phase_c(nc, sc, locals())
        if "D" in phases:
            phase_d(nc, sc, locals())

        if dbg:
            sc.dma("sync", "dbg", lambda e: e.dma_start(out=v_o.ap(), in_=V[:]),
                   reads=["V%d" % i for i in range(NST)] + ["Vones"], writes=["v_o"])
        sc.wait_all("sync")
        sc.flush()
    return nc


def phase_a(nc, sc, G):
    x_d, w_in_d, g1T_d, cw_d, cvec_d, qkg_d = (G[k] for k in ["x_d", "w_in_d", "g1T_d", "cw_d", "cvec_d", "qkg_d"])
    cst, ident_b, V, PS, PB, qT_s, cvT_s, kT_s, nhalf = (G[k] for k in ["cst", "ident_b", "V", "PS", "PB", "qT_s", "cvT_s", "kT_s", "nhalf"])
    with contextlib.ExitStack() as st:
        sb = lambda name, shape, dt=F32: st.enter_context(nc.sbuf_tensor("sb_" + name, list(shape), dt))
        NCA = 2560
        w_bf = sb("a_wbf", [128, 8, NCA], BF16)
        stage = [sb("a_stage%d" % i, [128, 8, 128]) for i in range(2)]
        g1T = sb("a_g1T", [128, 8])
        cw = sb("a_cw", [128, 4, 31])
        cvec = sb("a_cvec", [128, 4, 3])
        qkg = sb("a_qkg", [128, 2])
        diag = sb("a_diag", [128, 4, 31, 128], BF16)
        onesN = sb("a_onesN", [128, 128], BF16)
        blk = sb("a_blk", [128, 128], BF16)
        xt = [sb("a_xt0", [128, 4, D])] * 2
        ss = sb("a_ss", [128, 8])
        rstd = sb("a_rstd", [128, 8])
        xn = sb("a_xn", [128, 4, D], BF16)
        hT = sb("a_hT", [128, 8, 512], BF16)
        ybuf = [sb("a_ybuf%d" % i, [128, 4, 544], BF16) for i in range(2)]
        ybuf1 = [sb("a_ybuf1_%d" % i, [128, 4, 544], BF16) for i in range(2)]
        sg = [sb("a_sg0", [128, 512])] * 2
        ycb = sb("a_ycb", [128, 4, 512], BF16)
        ysq = sb("a_ysq", [128, 4, 512], BF16)
        mean_sb = sb("a_mean", [128, 512])
        lrstd = sb("a_lrstd", [128, 512])
        zt = [sb("a_zt0", [128, 512])] * 2
        cvT = sb("a_cvT", [128, 4, 512], BF16)
        sq = [sb("a_sq%d" % i, [128, 512], BF16) for i in range(2)]
        qr = [sb("a_qr%d" % i, [128, 512]) for i in range(2)]
        qkT = [sb("a_qT", [128, 4, 512], BF16), sb("a_kT", [128, 4, 512], BF16)]

        sc.dma("sync", "g1T", lambda e: e.dma_start(out=g1T[:], in_=g1T_d.ap()), writes=["g1T"])
        sc.dma("sync", "cw", lambda e: e.dma_start(out=cw[:], in_=cw_d.ap()), writes=["cw"])
        sc.dma("sync", "cvec", lambda e: e.dma_start(out=cvec[:], in_=cvec_d.ap()), writes=["cvec"])
        sc.dma("sync", "qkg", lambda e: e.dma_start(out=qkg[:], in_=qkg_d.ap()), writes=["qkg"])
        w_in_v = w_in_d.ap().rearrange("(kc p) n -> p kc n", p=128)
        for cg in range(NCA // 128):
            sl = stage[cg % 2]
            key = "stage%d" % (cg % 2)
            sc.dma("sync", key, lambda e, sl=sl, cg=cg: e.dma_start(out=sl[:], in_=w_in_v[:, :, cg * 128:(cg + 1) * 128]),
                   writes=[key])
            for kc in range(8):
                if kc % 2 == 0:
                    sc.op("vector", lambda e, sl=sl, cg=cg, kc=kc: e.tensor_scalar(
                        out=w_bf[:, kc, cg * 128:(cg + 1) * 128], in0=sl[:, kc, :], scalar1=g1T[:, kc:kc + 1],
                        scalar2=None, op0=ALU.mult), reads=[key, "g1T"], writes=["wbf%d_%d" % (cg // 4, kc)])
                else:
                    sc.op("scalar", lambda e, sl=sl, cg=cg, kc=kc: e.activation(
                        out=w_bf[:, kc, cg * 128:(cg + 1) * 128], in_=sl[:, kc, :], func=AF.Copy,
                        scale=g1T[:, kc:kc + 1]), reads=[key, "g1T"], writes=["wbf%d_%d" % (cg // 4, kc)])
        wkeys = lambda cg: ["wbf%d_%d" % (cg, kc) for kc in range(8)]
        for ch in range(4):
            for k in range(31):
                if k % 2 == 0:
                    sc.op("vector", lambda e, ch=ch, k=k: e.tensor_scalar(
                        out=diag[:, ch, k, :], in0=cst[:, 0:128], scalar1=cw[:, ch, k:k + 1], scalar2=None,
                        op0=ALU.mult), reads=["cst", "cw"], writes=["diag"])
                else:
                    sc.op("scalar", lambda e, ch=ch, k=k: e.activation(
                        out=diag[:, ch, k, :], in_=cst[:, 0:128], func=AF.Copy, scale=cw[:, ch, k:k + 1]),
                        reads=["cst", "cw"], writes=["diag"])
        sc.op("vector", lambda e: e.memset(onesN[:], 1.0 / 512.0), writes=["onesN"])
        sc.op("vector", lambda e: e.tensor_scalar(out=blk[:], in0=cst[:, 384:512], scalar1=1.0 / 64.0, scalar2=None,
                                                  op0=ALU.mult), reads=["cst"], writes=["blk"])
        for i in range(2):
            sc.op("gpsimd", lambda e, i=i: e.memset(ybuf[i][:], 0.0), writes=["ybuf%d" % i])
            sc.op("gpsimd", lambda e, i=i: e.memset(ybuf1[i][:], 0.0), writes=["ybuf%d" % i])

        x_v = x_d.ap().rearrange("(s j p) d -> s p j d", j=4, p=128)
        bank = [0]

        def nb():
            b = bank[0]
            bank[0] = (b + 1) % 6
            return b

        def load_x(s):
            sc.dma("sync", "xt0", lambda e, s=s: e.dma_start(out=xt[0][:], in_=x_v[s]), writes=["xt0"])

        def tile(s):
            stop = G.get("stop", 99)
            if s == 0:
                load_x(0)
            xs = xt[0]
            xk = "xt0"
            sc.op("vector", lambda e: e.memset(ss[:, 0:4], 0.0), writes=["ss"])
            for j in range(4):
                sc.op("scalar", lambda e, j=j: e.activation(out=xn[:, j, :], in_=xs[:, j, :], func=AF.Square,
                                                            scale=1.0 / 32.0, accum_out=ss[:, j:j + 1]),
                      reads=[xk, "ss"], writes=["xn%d" % j, "ss"])
            sc.op("vector", lambda e: e.tensor_scalar(out=rstd[:, 0:4], in0=ss[:, 0:4], scalar1=EPS, scalar2=None,
                                                      op0=ALU.add), reads=["ss"], writes=["rstd"])
            sc.op("gpsimd", lambda e: e.tensor_tensor(out=rstd[:, 0:4], in0=rstd[:, 0:4], in1=nhalf[:, 0:4], op=ALU.pow),
                  reads=["rstd", "nhalf"], writes=["rstd"])
            for j in range(4):
                sc.op("vector", lambda e, j=j: e.tensor_scalar(
                    out=xn[:, j, :], in0=xs[:, j, :], scalar1=rstd[:, j:j + 1], scalar2=None, op0=ALU.mult),
                    reads=[xk, "rstd"], writes=["xn%d" % j])
            if s + 1 < G["nst"]:
                load_x(s + 1)
            if stop < 2:
                return
            for j in range(4):
                pb = PB[j % 2]
                pk = "pb%d" % (j % 2)

                def tr(e, j=j, pb=pb):
                    for kc in range(8):
                        ins = e.transpose(out=pb[:, kc * 128:(kc + 1) * 128], in_=xn[:, j, kc * 128:(kc + 1) * 128],
                                          identity=ident_b[:])
                    return ins
                sc.op("tensor", tr, reads=["xn%d" % j, "ident_b"], writes=[pk])
                sc.op("vector" if j % 2 else "scalar",
                      (lambda e, j=j, pb=pb: e.tensor_copy(out=hT[:, :, j * 128:(j + 1) * 128],
                                                           in_=pb[:].rearrange("p (k t) -> p k t", k=8))) if j % 2 else
                      (lambda e, j=j, pb=pb: e.copy(out=hT[:, :, j * 128:(j + 1) * 128],
                                                    in_=pb[:].rearrange("p (k t) -> p k t", k=8))),
                      reads=[pk], writes=["hT%d" % j])
            hkeys = ["hT%d" % j for j in range(4)]

            def fm_matmul(m):
                b = nb()

                def f(e, m=m, b=b):
                    for kc in range(8):
                        ins = e.matmul(PS[b][:, :], lhsT=w_bf[:, kc, m * 128:(m + 1) * 128], rhs=hT[:, kc, :],
                                       start=(kc == 0), stop=(kc == 7))
                    return ins
                sc.op("tensor", f, reads=hkeys + wkeys(m // 4), writes=["ps%d" % b])
                return b

            if stop < 2.1:
                return
            yb = ybuf[s % 2]
            ybk = "ybuf%d" % (s % 2)
            ybo = ybuf[(s + 1) % 2]
            yb1 = ybuf1[s % 2]
            ybo1 = ybuf1[(s + 1) % 2]
            ybok = "ybuf%d" % ((s + 1) % 2)
            for ch in range(4):
                bg = fm_matmul(4 + ch)
                ba = fm_matmul(ch)
                sgt = sg[ch % 2]
                if stop < 2.3:
                    continue
                sc.op("scalar", lambda e, bg=bg, sgt=sgt: e.activation(out=sgt[:], in_=PS[bg][:, :], func=AF.Sigmoid),
                      reads=["ps%d" % bg], writes=["sg0"])
                if stop < 2.6:
                    continue
                if s > 0:
                    sc.op("gpsimd", lambda e, ch=ch: e.tensor_copy(out=yb[:, ch, 0:30], in_=ybo[:, ch, 512:542]),
                          reads=[ybok + "_%d" % ch], writes=[ybk + "_%d" % ch])
                    sc.op("gpsimd", lambda e, ch=ch: e.tensor_copy(out=yb1[:, ch, 0:29], in_=ybo1[:, ch, 512:541]),
                          reads=[ybok + "_%d" % ch], writes=[ybk + "_%d" % ch])
                sc.op("vector", lambda e, ba=ba, sgt=sgt, ch=ch: e.tensor_tensor(
                    out=yb[:, ch, 30:542], in0=PS[ba][:, :], in1=sgt[:], op=ALU.mult),
                    reads=["ps%d" % ba, "sg0", ybk], writes=[ybk + "_%d" % ch])
                sc.op("gpsimd", lambda e, ch=ch: e.tensor_copy(out=yb1[:, ch, 29:541], in_=yb[:, ch, 30:542]),
                      reads=[ybk + "_%d" % ch], writes=[ybk + "_%d" % ch])
            if stop < 3.5:
                return
            for ch in range(4):
                b = nb()

                def cf(e, ch=ch, b=b):
                    for k in (range(31) if stop != 3.6 else [0, 16, 30]):
                        rhs = yb[:, ch, k:k + 512] if k % 2 == 0 else yb1[:, ch, k - 1:k - 1 + 512]
                        ins = e.matmul(PS[b][:, :], lhsT=diag[:, ch, k, :], rhs=rhs, start=(k == 0), stop=(k == 30))
                    return ins
                sc.op("tensor", cf, reads=[ybk + "_%d" % ch, "diag", ybk], writes=["ps%d" % b])
                if stop < 3.8:
                    continue
                sc.op("vector", lambda e, ch=ch, b=b: e.tensor_scalar(
                    out=ycb[:, ch, :], in0=PS[b][:, :], scalar1=cvec[:, ch, 0:1], scalar2=None, op0=ALU.add),
                    reads=["ps%d" % b, "cvec"], writes=["ycb%d" % ch])
                if stop < 3.9:
                    continue
                sc.op("scalar", lambda e, ch=ch, b=b: e.activation(
                    out=ysq[:, ch, :], in_=ycb[:, ch, :], func=AF.Square),
                    reads=["ycb%d" % ch], writes=["ysq%d" % ch])
            if stop < 5:
                return
            bm = nb()
            bq = nb()

            def stf(e, bm=bm, bq=bq):
                for ch in range(4):
                    e.matmul(PS[bm][:, :], lhsT=onesN[:], rhs=ycb[:, ch, :], start=(ch == 0), stop=(ch == 3))
                for ch in range(4):
                    ins = e.matmul(PS[bq][:, :], lhsT=onesN[:], rhs=ysq[:, ch, :], start=(ch == 0), stop=(ch == 3))
                return ins
            sc.op("tensor", stf, reads=["ycb%d" % c for c in range(4)] + ["ysq%d" % c for c in range(4)] + ["onesN"],
                  writes=["ps%d" % bm, "ps%d" % bq])
            sc.op("scalar", lambda e, bm=bm: e.copy(out=mean_sb[:], in_=PS[bm][:, :]), reads=["ps%d" % bm], writes=["mean"])
            sc.op("vector", lambda e: e.tensor_tensor(out=lrstd[:], in0=mean_sb[:], in1=mean_sb[:], op=ALU.mult),
                  reads=["mean"], writes=["lrstd"])
            sc.op("vector", lambda e, bq=bq: e.tensor_tensor(out=lrstd[:], in0=PS[bq][:, :], in1=lrstd[:], op=ALU.subtract),
                  reads=["ps%d" % bq, "lrstd"], writes=["lrstd"])
            sc.op("vector", lambda e: e.tensor_scalar(out=lrstd[:], in0=lrstd[:], scalar1=EPS, scalar2=None,
                                                      op0=ALU.add), reads=["lrstd"], writes=["lrstd"])
            sc.op("scalar", lambda e: e.activation(out=lrstd[:], in_=lrstd[:], func=AF.Sqrt), reads=["lrstd"], writes=["lrstd"])
            sc.op("vector", lambda e: e.reciprocal(out=lrstd[:], in_=lrstd[:]), reads=["lrstd"], writes=["lrstd"])
            for ch in range(4):
                z = zt[ch % 2]
                zk = "zt0"
                sc.op("gpsimd", lambda e, ch=ch, z=z: e.tensor_tensor(out=z[:], in0=ycb[:, ch, :], in1=mean_sb[:],
                                                                      op=ALU.subtract),
                      reads=["ycb%d" % ch, "mean"], writes=[zk])
                sc.op("vector", lambda e, z=z: e.tensor_tensor(out=z[:], in0=z[:], in1=lrstd[:], op=ALU.mult),
                      reads=[zk, "lrstd"], writes=[zk])
                sc.op("vector", lambda e, ch=ch, z=z: e.tensor_scalar(out=z[:], in0=z[:], scalar1=cvec[:, ch, 1:2],
                                                                      scalar2=cvec[:, ch, 2:3], op0=ALU.mult, op1=ALU.add),
                      reads=[zk, "cvec"], writes=[zk])
                sc.op("scalar", lambda e, ch=ch, z=z: e.activation(out=cvT[:, ch, :], in_=z[:], func=AF.Silu),
                      reads=[zk], writes=["cvT"])
            sc.dma("sync", "cvT_st", lambda e, s=s: e.dma_start(out=cvT_s.ap()[:, :, s * 512:(s + 1) * 512], in_=cvT[:]),
                   reads=["cvT"], writes=["cvT_s%d" % s])
            if stop < 6:
                return
            for which in range(2):
                for h in range(4):
                    b = fm_matmul(8 + which * 4 + h)
                    i2 = h % 2
                    sc.op("scalar", lambda e, b=b, i2=i2: e.activation(out=sq[i2][:], in_=PS[b][:, :], func=AF.Square),
                          reads=["ps%d" % b], writes=["sq%d" % i2])
                    b2 = nb()
                    sc.op("tensor", lambda e, b2=b2, i2=i2: e.matmul(PS[b2][:, :], lhsT=blk[:], rhs=sq[i2][:],
                                                                    start=True, stop=True),
                          reads=["sq%d" % i2, "blk"], writes=["ps%d" % b2])
                    sc.op("vector", lambda e, b2=b2, i2=i2: e.tensor_scalar(
                        out=qr[i2][:], in0=PS[b2][:, :], scalar1=EPS, scalar2=None, op0=ALU.add),
                        reads=["ps%d" % b2], writes=["qr%d" % i2])
                    sc.op("scalar", lambda e, i2=i2: e.activation(out=qr[i2][:], in_=qr[i2][:], func=AF.Sqrt),
                          reads=["qr%d" % i2], writes=["qr%d" % i2])
                    sc.op("vector", lambda e, i2=i2: e.reciprocal(out=qr[i2][:], in_=qr[i2][:]),
                          reads=["qr%d" % i2], writes=["qr%d" % i2])
                    dst = qkT[which][:, h, :]
                    wk = ["qkT%d" % which]
                    sc.op("vector", lambda e, b=b, i2=i2, dst=dst, which=which: e.scalar_tensor_tensor(
                        out=dst, in0=PS[b][:, :], scalar=qkg[:, which:which + 1], in1=qr[i2][:],
                        op0=ALU.mult, op1=ALU.mult), reads=["ps%d" % b, "qr%d" % i2, "qkg"], writes=wk)
                dd = qT_s if which == 0 else kT_s
                sc.dma("sync", "qk_st%d" % which, lambda e, s=s, dd=dd, which=which: e.dma_start(
                    out=dd.ap()[:, :, s * 512:(s + 1) * 512], in_=qkT[which][:]),
                    reads=["qkT%d" % which], writes=["qk_s%d_%d" % (which, s)])
            if stop < 7:
                return
            for j in range(4):
                b = nb()

                def vf(e, j=j, b=b):
                    for kc in range(8):
                        ins = e.matmul(PS[b][:, :], lhsT=hT[:, kc, j * 128:(j + 1) * 128], rhs=w_bf[:, kc, 2048:2560],
                                       start=(kc == 0), stop=(kc == 7))
                    return ins
                sc.op("tensor", vf, reads=["hT%d" % j] + wkeys(4), writes=["ps%d" % b])
                sc.op("scalar" if j % 2 else "vector",
                      (lambda e, j=j, b=b: e.copy(out=V[:, s * 4 + j, :, 0:128],
                                                  in_=PS[b][:, :].rearrange("p (h e) -> p h e", h=4))) if j % 2 else
                      (lambda e, j=j, b=b: e.tensor_copy(out=V[:, s * 4 + j, :, 0:128],
                                                         in_=PS[b][:, :].rearrange("p (h e) -> p h e", h=4))),
                      reads=["ps%d" % b], writes=["V%d" % s])
        for s in range(G["nst"]):
            tile(s)
        for en in sc.ENGS:
            sc.wait_all(en)


def load_cast(sc, nc, stage, stage_key, src_view, dst, ncols, kcn, dkey, scale=None):
    for cg in range(ncols // 128):
        sl = stage[cg % 2]
        key = stage_key + str(cg % 2)
        sc.dma("sync", key, lambda e, sl=sl, cg=cg: e.dma_start(out=sl[:, 0:kcn, :], in_=src_view[:, :, cg * 128:(cg + 1) * 128]),
               writes=[key])
        if scale is None:
            eng = ["vector", "gpsimd"][cg % 2]
            sc.op(eng, lambda e, sl=sl, cg=cg: e.tensor_copy(out=dst[:, :, cg * 128:(cg + 1) * 128], in_=sl[:, 0:kcn, :]),
                  reads=[key], writes=[dkey])
        else:
            for kc in range(kcn):
                if kc % 2 == 0:
                    sc.op("vector", lambda e, sl=sl, cg=cg, kc=kc: e.tensor_scalar(
                        out=dst[:, kc, cg * 128:(cg + 1) * 128], in0=sl[:, kc, :], scalar1=scale[:, kc:kc + 1],
                        scalar2=None, op0=ALU.mult), reads=[key, "g1T"], writes=[dkey])
                else:
                    sc.op("scalar", lambda e, sl=sl, cg=cg, kc=kc: e.activation(
                        out=dst[:, kc, cg * 128:(cg + 1) * 128], in_=sl[:, kc, :], func=AF.Copy,
                        scale=scale[:, kc:kc + 1]), reads=[key, "g1T"], writes=[dkey])


def phase_b(nc, sc, G):
    names = ["x_d", "w_in_d", "g1T_d", "w_co_d", "w_ao_d", "w_out_d", "g2bc_d", "lamv_d", "sgbc_d", "wr_d", "rbbc_d",
             "cst", "ident_b", "V", "PS", "PB", "qT_s", "cvT_s", "kT_s", "nhalf", "LG", "x1_s", "h2_s"]
    (x_d, w_in_d, g1T_d, w_co_d, w_ao_d, w_out_d, g2bc_d, lamv_d, sgbc_d, wr_d, rbbc_d,
     cst, ident_b, V, PS, PB, qT_s, cvT_s, kT_s, nhalf, LG, x1_s, h2_s) = (G[k] for k in names)
    nst = G["nst"]
    with contextlib.ExitStack() as st:
        sb = lambda name, shape, dt=F32: st.enter_context(nc.sbuf_tensor("sb_" + name, list(shape), dt))
        KT = sb("b_KT", [128, 4, S], BF16)
        wgt = sb("b_wgt", [128, 8, 2048], BF16)
        wco = sb("b_wco", [128, 4, D], BF16)
        wao = sb("b_wao", [128, 4, D], BF16)
        wout = sb("b_wout", [128, 8, D], BF16)
        wr = sb("b_wr", [128, 8, 36])
        stage_raw = [sb("b_stage%d" % i, [128, 1024]) for i in range(2)]
        stage = [t[:].rearrange("p (k c) -> p k c", k=8) for t in stage_raw]
        g1T = sb("b_g1T", [128, 8])
        g2bc = sb("b_g2bc", [128, D])
        lamv = sb("b_lamv", [128, 4, 64])
        sgbc = sb("b_sgbc", [128, 128])
        rbbc = sb("b_rbbc", [128, 36])
        tri_b = sb("b_tri", [128, 128], BF16)
        lam2 = sb("b_lam2", [128, 4])
        nlam = sb("b_nlam", [128, 1])
        xr = [sb("b_xr%d" % i, [128, D]) for i in range(2)]
        xm = sb("b_xm", [128, 4 * D], BF16)
        xn = xm[:].rearrange("p (j d) -> p j d", j=4)
        mT = xm[:].rearrange("p (k t) -> p k t", k=8)
        hT = sb("b_hT", [128, 8, 512], BF16)
        ss = sb("b_ss", [128, 8])
        rstd = sb("b_rstd", [128, 8])
        qT = sb("b_qT", [128, 4, 512], BF16)
        cvT = sb("b_cvT", [128, 4, 512], BF16)
        Et_t = sb("b_Et", [128, 4, 512], BF16)
        Et = [Et_t[:, i, :] for i in range(4)]
        aoT = sb("b_aoT", [128, 4, 512], BF16)
        Osb = sb("b_Osb", [128, 3, 3, 130])
        rr = sb("b_rr", [128, 4])
        t1 = sb("b_t1", [128, 128])
        ot = sb("b_ot", [128, 128])
        on = sb("b_on", [128, 128], BF16)
        junk = sb("b_junk", [128, 128], BF16)
        sgs = [stage_raw[0][:, 0:512], stage_raw[0][:, 512:1024]]
        tu = [stage_raw[1][:, 0:512], stage_raw[1][:, 512:1024]]
        h2 = sb("b_h2", [128, D])
        h2b = Et_t[:, 0:2, :].rearrange("p a b -> p (a b)")
        h2T = Osb[:].rearrange("p a b c -> p (a b c)")[:, 0:1024].rearrange("p (k t) -> p k t", k=8)

        sc.dma("sync", "b_kt", lambda e: e.dma_start(out=KT[:, :, 0:nst * 512], in_=kT_s.ap()[:, :, 0:nst * 512]),
               reads=["qk_s1_%d" % i for i in range(nst)], writes=["KT"])
        sc.dma("sync", "b_g1T", lambda e: e.dma_start(out=g1T[:], in_=g1T_d.ap()), writes=["g1T"])
        sc.dma("sync", "b_g2bc", lambda e: e.dma_start(out=g2bc[:], in_=g2bc_d.ap()), writes=["g2bc"])
        sc.dma("sync", "b_lamv", lambda e: e.dma_start(out=lamv[:], in_=lamv_d.ap()), writes=["lamv"])
        sc.dma("sync", "b_sgbc", lambda e: e.dma_start(out=sgbc[:], in_=sgbc_d.ap()), writes=["sgbc"])
        sc.dma("sync", "b_rbbc", lambda e: e.dma_start(out=rbbc[:], in_=rbbc_d.ap()), writes=["rbbc"])
        sc.dma("sync", "b_wr", lambda e: e.dma_start(out=wr[:], in_=wr_d.ap().rearrange("(kc p) n -> p kc n", p=128)),
               writes=["wr"])
        w_in_v = w_in_d.ap().rearrange("(kc p) n -> p kc n", p=128)[:, :, 2560:4608]
        load_cast(sc, nc, stage, "b_stage", w_in_v, wgt, 2048, 8, "wgt", scale=g1T)
        load_cast(sc, nc, stage, "b_stage", w_co_d.ap().rearrange("(kc p) n -> p kc n", p=128), wco, D, 4, "wco")
        load_cast(sc, nc, stage, "b_stage", w_ao_d.ap().rearrange("(kc p) n -> p kc n", p=128), wao, D, 4, "wao")
        load_cast(sc, nc, stage, "b_stage", w_out_d.ap().rearrange("(kc p) n -> p kc n", p=128), wout, D, 8, "wout")
        sc.op("vector", lambda e: e.tensor_copy(out=tri_b[:], in_=cst[:, 128:256]), reads=["cst"], writes=["tri_b"])
        sc.op("vector", lambda e: e.tensor_scalar(out=sgbc[:], in0=sgbc[:], scalar1=1.0 - LAM_INIT, scalar2=None,
                                                  op0=ALU.mult), reads=["sgbc"], writes=["sgbc"])
        sc.op("vector", lambda e: e.tensor_tensor(out=lamv[:, 0, :], in0=lamv[:, 0, :], in1=lamv[:, 1, :], op=ALU.mult),
              reads=["lamv"], writes=["lamv"])
        sc.op("vector", lambda e: e.tensor_tensor(out=lamv[:, 2, :], in0=lamv[:, 2, :], in1=lamv[:, 3, :], op=ALU.mult),
              reads=["lamv"], writes=["lamv"])
        sc.op("vector", lambda e: e.reduce_sum(out=lam2[:, 0:1], in_=lamv[:, 0, :], axis=AX.X), reads=["lamv"], writes=["lam2"])
        sc.op("vector", lambda e: e.reduce_sum(out=lam2[:, 1:2], in_=lamv[:, 2, :], axis=AX.X), reads=["lamv", "lam2"],
              writes=["lam2"])
        sc.op("scalar", lambda e: e.activation(out=lam2[:, 2:4], in_=lam2[:, 0:2], func=AF.Exp), reads=["lam2"], writes=["lam2"])
        sc.op("vector", lambda e: e.tensor_tensor(out=nlam[:], in0=lam2[:, 3:4], in1=lam2[:, 2:3], op=ALU.subtract),
              reads=["lam2"], writes=["nlam"])
        sc.op("vector", lambda e: e.tensor_scalar(out=nlam[:], in0=nlam[:], scalar1=-LAM_INIT, scalar2=None, op0=ALU.add),
              reads=["nlam"], writes=["nlam"])

        bstop = G.get("bstop", 99)
        x_v = x_d.ap().rearrange("(t p) d -> t p d", p=128)
        x1_v = x1_s.ap().rearrange("(t p) d -> t p d", p=128)
        h2_v = h2_s.ap().rearrange("(t p) d -> t p d", p=128)
        bank = [0]

        def nb():
            b = bank[0]
            bank[0] = (b + 1) % 6
            return b

        def tile(s):
            def load_q(s2):
                sc.dma("sync", "b_qT", lambda e: e.dma_start(out=qT[:], in_=qT_s.ap()[:, :, s2 * 512:(s2 + 1) * 512]),
                       reads=["qk_s0_%d" % s2], writes=["qT"])

            def load_cv(s2):
                sc.dma("sync", "b_cvT", lambda e: e.dma_start(out=cvT[:], in_=cvT_s.ap()[:, :, s2 * 512:(s2 + 1) * 512]),
                       reads=["cvT_s%d" % s2], writes=["cvT"])
            if s == 0:
                load_q(0)
                load_cv(0)
            for j in range(4):
                xj = xr[j % 2]
                xk = "xr%d" % (j % 2)
                sc.dma("sync", "b_" + xk, lambda e, j=j, xj=xj: e.dma_start(out=xj[:], in_=x_v[s * 4 + j]), writes=[xk])
                sc.op("vector", lambda e, j=j: e.memset(ss[:, j:j + 1], 0.0), writes=["ss%d" % j])
                sc.op("scalar", lambda e, j=j, xj=xj: e.activation(out=xn[:, j, :], in_=xj[:], func=AF.Square,
                                                                   scale=1.0 / 32.0, accum_out=ss[:, j:j + 1]),
                      reads=[xk, "ss%d" % j], writes=["xn%d" % j, "ss%d" % j])
                sc.op("vector", lambda e, j=j: e.tensor_scalar(out=rstd[:, j:j + 1], in0=ss[:, j:j + 1], scalar1=EPS,
                                                               scalar2=None, op0=ALU.add), reads=["ss%d" % j], writes=["rstd%d" % j])
                sc.op("gpsimd", lambda e, j=j: e.tensor_tensor(out=rstd[:, j:j + 1], in0=rstd[:, j:j + 1], in1=nhalf[:, 0:1],
                                                               op=ALU.pow), reads=["rstd%d" % j, "nhalf"], writes=["rstd%d" % j])
                sc.op("vector", lambda e, j=j, xj=xj: e.tensor_scalar(
                    out=xn[:, j, :], in0=xj[:], scalar1=rstd[:, j:j + 1], scalar2=None, op0=ALU.mult),
                    reads=[xk, "rstd%d" % j], writes=["xn%d" % j])
            if bstop < 3:
                return
            ei = [0]
            for h in range(4):
                nkb = 4 * s + 4
                items = [(c, kb) for c in range(2) for kb in range(nkb)]

                def emit_s(idx, h=h):
                    c, kb = items[idx]
                    r0, r1 = c * 64, (c + 1) * 64
                    jmin = max(0, kb - 4 * s)
                    q0 = jmin * 128
                    sb_ = 3 + (idx % 3)
                    sc.op("tensor", lambda e, kb=kb, q0=q0, sb_=sb_, r0=r0, r1=r1, h=h: e.matmul(
                        PS[sb_][:, q0:512], lhsT=KT[r0:r1, h, kb * 128:(kb + 1) * 128], rhs=qT[r0:r1, h, q0:512],
                        start=True, stop=True), reads=["KT", "qT"], writes=["ps%d" % sb_])
                    E = Et[ei[0] % 4]
                    ek = "E%d" % (ei[0] % 4)
                    ei[0] += 1
                    sc.op("scalar", lambda e, E=E, q0=q0, sb_=sb_: e.activation(
                        out=E[:, q0:512], in_=PS[sb_][:, q0:512], func=AF.Exp, scale=0.125),
                        reads=["ps%d" % sb_], writes=[ek])
                    if kb >= 4 * s:
                        sc.op("gpsimd", lambda e, E=E, q0=q0: e.tensor_tensor(
                            out=E[:, q0:q0 + 128], in0=E[:, q0:q0 + 128], in1=tri_b[:], op=ALU.mult),
                            reads=[ek, "tri_b"], writes=[ek])
                    return (E, ek, jmin)

                def emit_av(idx, st_, h=h):
                    c, kb = items[idx]
                    E, ek, jmin = st_

                    def av(e, c=c, kb=kb, jmin=jmin, E=E, h=h, s=s):
                        for j in range(jmin, 4):
                            g = c * 4 + j
                            ob = g // 3
                            o0 = (g % 3) * 160
                            ins = e.matmul(PS[ob][:, o0:o0 + 129], lhsT=E[:, j * 128:(j + 1) * 128],
                                           rhs=V[:, kb, h, 0:129], start=(kb == 0 and g % 3 == 0),
                                           stop=(kb == 4 * s + j), skip_group_check=True)
                        return ins
                    sc.op("tensor", av, reads=[ek, "V%d" % (kb // 4), "Vones"],
                          writes=["ps0", "ps1"] if c == 0 else ["ps1", "ps2"])

                pend = [emit_s(0), emit_s(1)]
                for idx in range(len(items)):
                    if idx + 2 < len(items):
                        pend.append(emit_s(idx + 2))
                    emit_av(idx, pend.pop(0))
                if bstop < 4:
                    continue
                for b in range(3):
                    src = lambda b=b: PS[b][:, 0:480].rearrange("p (r c) -> p r c", r=3)[:, :, 0:129]
                    if b % 2 == 0:
                        sc.op("scalar", lambda e, b=b, src=src: e.copy(out=Osb[:, b, :, 0:129], in_=src()),
                              reads=["ps%d" % b], writes=["Osb%d" % b])
                    else:
                        sc.op("vector", lambda e, b=b, src=src: e.tensor_copy(out=Osb[:, b, :, 0:129], in_=src()),
                              reads=["ps%d" % b], writes=["Osb%d" % b])
                for j in range(4):
                    b0, r_ = j // 3, j % 3
                    b1, r1_ = (4 + j) // 3, (4 + j) % 3
                    sc.op("vector", lambda e, b0=b0, r_=r_: e.reciprocal(out=rr[:, 0:1], in_=Osb[:, b0, r_, 128:129]),
                          reads=["Osb%d" % b0], writes=["rr"])
                    sc.op("vector", lambda e, b1=b1, r1_=r1_: e.reciprocal(out=rr[:, 1:2], in_=Osb[:, b1, r1_, 128:129]),
                          reads=["Osb%d" % b1, "rr"], writes=["rr"])
                    sc.op("vector", lambda e: e.tensor_tensor(out=rr[:, 1:2], in0=rr[:, 1:2], in1=nlam[:], op=ALU.mult),
                          reads=["rr", "nlam"], writes=["rr"])
                    sc.op("vector", lambda e, b0=b0, r_=r_: e.tensor_scalar(
                        out=t1[:], in0=Osb[:, b0, r_, 0:128], scalar1=rr[:, 0:1], scalar2=None, op0=ALU.mult),
                        reads=["Osb%d" % b0, "rr"], writes=["t1"])
                    sc.op("vector", lambda e, b1=b1, r1_=r1_: e.scalar_tensor_tensor(
                        out=ot[:], in0=Osb[:, b1, r1_, 0:128], scalar=rr[:, 1:2], in1=t1[:], op0=ALU.mult, op1=ALU.add),
                        reads=["Osb%d" % b1, "rr", "t1"], writes=["ot"])
                    sc.op("vector", lambda e: e.memset(rr[:, 2:3], 0.0), reads=["rr"], writes=["rr"])
                    sc.op("scalar", lambda e: e.activation(out=junk[:], in_=ot[:], func=AF.Square,
                                                           scale=float(128.0 ** -0.5), accum_out=rr[:, 2:3]),
                          reads=["ot", "rr"], writes=["junk", "rr"])
                    sc.op("vector", lambda e: e.tensor_scalar(out=rr[:, 3:4], in0=rr[:, 2:3], scalar1=EPS, scalar2=None,
                                                              op0=ALU.add), reads=["rr"], writes=["rr"])
                    sc.op("gpsimd", lambda e: e.tensor_tensor(out=rr[:, 3:4], in0=rr[:, 3:4], in1=nhalf[:, 0:1], op=ALU.pow),
                          reads=["rr", "nhalf"], writes=["rr"])
                    sc.op("vector", lambda e: e.scalar_tensor_tensor(
                        out=on[:], in0=ot[:], scalar=rr[:, 3:4], in1=sgbc[:], op0=ALU.mult, op1=ALU.mult),
                        reads=["ot", "rr", "sgbc"], writes=["on"])
                    pb = PB[j % 2]
                    pk = "pb%d" % (j % 2)
                    sc.op("tensor", lambda e, pb=pb: e.transpose(out=pb[:, 0:128], in_=on[:], identity=ident_b[:]),
                          reads=["on", "ident_b"], writes=[pk])
                    sc.op("scalar", lambda e, pb=pb, h=h, j=j: e.copy(out=aoT[:, h, j * 128:(j + 1) * 128], in_=pb[:, 0:128]),
                          reads=[pk], writes=["aoT"])

            if s + 1 < nst:
                load_q(s + 1)
            for j in range(4):
                pb = PB[j % 2]
                pk = "pb%d" % (j % 2)

                def tr(e, j=j, pb=pb):
                    for kc in range(8):
                        ins = e.transpose(out=pb[:, kc * 128:(kc + 1) * 128], in_=xn[:, j, kc * 128:(kc + 1) * 128],
                                          identity=ident_b[:])
                    return ins
                sc.op("tensor", tr, reads=["xn%d" % j, "ident_b"], writes=[pk])
                sc.op("vector", lambda e, j=j, pb=pb: e.tensor_copy(out=hT[:, :, j * 128:(j + 1) * 128],
                                                                   in_=pb[:].rearrange("p (k t) -> p k t", k=8)),
                      reads=[pk], writes=["hT%d" % j])
            hkeys = ["hT%d" % j for j in range(4)]

            if bstop < 5:
                return
            for m in range(8):
                for half in range(2):
                    bG, bP = nb(), nb()
                    wsrc, asrc, akey = (wco, cvT, "cvT") if half == 0 else (wao, aoT, "aoT")

                    def mm(e, m=m, half=half, bG=bG, bP=bP, wsrc=wsrc, asrc=asrc):
                        c0 = half * 1024 + m * 128
                        for kc in range(8):
                            e.matmul(PS[bG][:, :], lhsT=wgt[:, kc, c0:c0 + 128], rhs=hT[:, kc, :],
                                     start=(kc == 0), stop=(kc == 7))
                        for kc in range(4):
                            ins = e.matmul(PS[bP][:, :], lhsT=wsrc[:, kc, m * 128:(m + 1) * 128], rhs=asrc[:, kc, :],
                                           start=(kc == 0), stop=(kc == 3))
                        return ins
                    sc.op("tensor", mm, reads=hkeys + ["wgt", "wco", "wao", akey], writes=["ps%d" % bG, "ps%d" % bP])
                    sc.op("scalar", lambda e, bG=bG, half=half: e.activation(out=sgs[half][:], in_=PS[bG][:, :], func=AF.Sigmoid),
                          reads=["ps%d" % bG], writes=["sgs%d" % half])
                    sc.op("vector", lambda e, bP=bP, half=half: e.tensor_tensor(out=tu[half][:], in0=PS[bP][:, :],
                                                                                in1=sgs[half][:], op=ALU.mult),
                          reads=["ps%d" % bP, "sgs%d" % half], writes=["tu%d" % half])
                sc.op("gpsimd", lambda e, m=m: e.tensor_tensor(out=mT[:, m, :], in0=tu[0][:], in1=tu[1][:], op=ALU.add),
                      reads=["tu0", "tu1"], writes=["mT"])
            if s + 1 < nst:
                load_cv(s + 1)
            if bstop < 6:
                return
            for j in range(4):
                t = s * 4 + j
                xj = xr[j % 2]
                xk = "xr%d" % (j % 2)
                sc.dma("sync", "b_" + xk, lambda e, t=t, xj=xj: e.dma_start(out=xj[:], in_=x_v[t]), writes=[xk])
                for half in range(2):
                    b = nb()

                    def of(e, j=j, half=half, b=b):
                        for kc in range(8):
                            ins = e.matmul(PS[b][:, :], lhsT=mT[:, kc, j * 128:(j + 1) * 128],
                                           rhs=wout[:, kc, half * 512:(half + 1) * 512], start=(kc == 0), stop=(kc == 7))
                        return ins
                    sc.op("tensor", of, reads=["mT", "wout"], writes=["ps%d" % b])
                    sc.op("vector", lambda e, xj=xj, half=half, b=b: e.tensor_tensor(
                        out=xj[:, half * 512:(half + 1) * 512], in0=PS[b][:, :], in1=xj[:, half * 512:(half + 1) * 512],
                        op=ALU.add), reads=["ps%d" % b, xk], writes=[xk])
                sc.dma("sync", "b_x1st%d" % (j % 2), lambda e, t=t, xj=xj: e.dma_start(out=x1_v[t], in_=xj[:]), reads=[xk],
                       writes=["x1_s%d" % t])
                if bstop < 7:
                    continue
                sc.op("vector", lambda e, j=j: e.memset(ss[:, 4 + j:5 + j], 0.0), writes=["ss%d" % (4 + j)])
                sc.op("scalar", lambda e, j=j, xj=xj: e.activation(out=h2b, in_=xj[:], func=AF.Square,
                                                                   scale=1.0 / 32.0, accum_out=ss[:, 4 + j:5 + j]),
                      reads=[xk, "ss%d" % (4 + j)], writes=["h2b", "E0", "E1", "ss%d" % (4 + j)])
                sc.op("vector", lambda e, j=j: e.tensor_scalar(out=rstd[:, 4 + j:5 + j], in0=ss[:, 4 + j:5 + j], scalar1=EPS,
                                                               scalar2=None, op0=ALU.add),
                      reads=["ss%d" % (4 + j)], writes=["rstd%d" % (4 + j)])
                sc.op("gpsimd", lambda e, j=j: e.tensor_tensor(out=rstd[:, 4 + j:5 + j], in0=rstd[:, 4 + j:5 + j],
                                                               in1=nhalf[:, 0:1], op=ALU.pow),
                      reads=["rstd%d" % (4 + j), "nhalf"], writes=["rstd%d" % (4 + j)])
                sc.op("vector", lambda e, j=j, xj=xj: e.scalar_tensor_tensor(
                    out=h2[:], in0=xj[:], scalar=rstd[:, 4 + j:5 + j], in1=g2bc[:], op0=ALU.mult, op1=ALU.mult),
                    reads=[xk, "rstd%d" % (4 + j), "g2bc"], writes=["h2"])
                sc.op("gpsimd", lambda e: e.tensor_copy(out=h2b, in_=h2[:]), reads=["h2"], writes=["h2b", "E0", "E1"])
                sc.dma("sync", "b_h2st", lambda e, t=t: e.dma_start(out=h2_v[t], in_=h2b), reads=["h2b", "E0", "E1"],
                       writes=["h2_s%d" % t])
                if bstop < 7.5:
                    continue
                for half in range(2):
                    b = nb()

                    def trf(e, half=half, b=b):
                        for k4 in range(4):
                            kc = half * 4 + k4
                            ins = e.transpose(out=PS[b][:, k4 * 128:(k4 + 1) * 128], in_=h2[:, kc * 128:(kc + 1) * 128],
                                              identity=cst[:, 0:128])
                        return ins
                    sc.op("tensor", trf, reads=["h2", "cst"], writes=["ps%d" % b])
                    sc.op("scalar" if half else "vector",
                          (lambda e, half=half, b=b: e.copy(out=h2T[:, half * 4:(half + 1) * 4, :],
                                                            in_=PS[b][:, :].rearrange("p (k t) -> p k t", k=4))) if half else
                          (lambda e, half=half, b=b: e.tensor_copy(out=h2T[:, half * 4:(half + 1) * 4, :],
                                                                   in_=PS[b][:, :].rearrange("p (k t) -> p k t", k=4))),
                          reads=["ps%d" % b], writes=["h2T%d" % half])
                if bstop < 7.8:
                    continue
                b = nb()

                def rf(e, b=b):
                    for kc in range(8):
                        ins = e.matmul(PS[b][:, 0:36], lhsT=h2T[:, kc, :], rhs=wr[:, kc, :], start=(kc == 0), stop=(kc == 7))
                    return ins
                sc.op("tensor", rf, reads=["h2T0", "h2T1", "wr"], writes=["ps%d" % b])
                sc.op("vector", lambda e, t=t, b=b: e.tensor_tensor(out=LG[:, t, :], in0=PS[b][:, 0:36], in1=rbbc[:],
                                                                    op=ALU.add),
                      reads=["ps%d" % b, "rbbc"], writes=["LG%d" % t])

        for s in range(nst if bstop >= 2 else 0):
            tile(s)
        for en in sc.ENGS:
            sc.wait_all(en)


def phase_c(nc, sc, G):
    names = ["LG", "dest_i", "wts", "cst", "ident_b", "PS", "PB", "eoff_d", "h2_s", "XS", "YS", "w_g_d", "w_u_d", "w_d_d"]
    LG, dest_i, wts, cst, ident_b, PS, PB, eoff_d, h2_s, XS, YS, w_g_d, w_u_d, w_d_d = (G[k] for k in names)
    ntile = G["nst"] * 4
    NR = NE * CAP
    with contextlib.ExitStack() as st:
        sb = lambda name, shape, dt=F32: st.enter_context(nc.sbuf_tensor("sb_" + name, list(shape), dt))
        xb = [sb("c_xb%d" % i, [128, D], BF16) for i in range(2)]
        U_b = sb("c_Ub", [128, 128], BF16)
        ones_b = sb("c_onesb", [128, 128], BF16)
        eoff = sb("c_eoff", [128, NE])
        with contextlib.ExitStack() as st2:
            sb2 = lambda name, shape, dt=F32: st2.enter_context(nc.sbuf_tensor("sb_" + name, list(shape), dt))
            GLd = sb2("r_GLd", [128, 32, 4])
            ohg = sb2("r_ohg", [128, 32, 4])
            pen = sb2("r_pen", [128, 32, 4])
            sm = sb2("r_sm", [128, 8, 32])
            ELm = sb2("r_ELm", [128, 32, 4, 8])
            oh1 = sb2("r_oh1", [128, 32, 32])
            EL2 = sb2("r_EL2", [128, 32, 32])
            oh2 = sb2("r_oh2", [128, 32, 32])
            Mb = sb2("r_Mb", [128, 32, 32], BF16)
            pre = sb2("r_pre", [128, 32, 32])
            tot = sb2("r_tot", [128, 32, 32])
            base = sb2("r_base", [128, 32, 32])
            prod = sb2("r_prod", [128, 32, 32])
            destf = sb2("r_destf", [128, 2, 32])
            ELm3 = ELm[:].rearrange("p t g j -> p t (g j)")
            gmax, gsum, gw, m1, m2, dm, rsel, esel = (sm[:, i, :] for i in range(8))
            bc4 = lambda a: a.unsqueeze(2).broadcast_to([128, 32, 4])
            bc32 = lambda a: a.unsqueeze(2).broadcast_to([128, 32, 32])
            LK = ["LG%d" % i for i in range(32)]
            V_ = "vector"
            sc.dma("sync", "c_eoff", lambda e: e.dma_start(out=eoff[:], in_=eoff_d.ap()), writes=["eoff"])
            sc.op(V_, lambda e: e.tensor_copy(out=U_b[:], in_=cst[:, 256:384]), reads=["cst"], writes=["U_b"])
            sc.op(V_, lambda e: e.memset(ones_b[:], 1.0), writes=["ones_b"])
            sc.op(V_, lambda e: e.reduce_max(out=gmax, in_=LG[:, :, 0:4], axis=AX.X), reads=LK, writes=["sm"])
            sc.op(V_, lambda e: e.tensor_tensor(out=GLd[:], in0=LG[:, :, 0:4], in1=bc4(gmax), op=ALU.subtract),
                  reads=LK + ["sm"], writes=["GLd"])
            sc.op(V_, lambda e: e.tensor_scalar(out=ohg[:], in0=GLd[:], scalar1=0.0, scalar2=None, op0=ALU.is_equal),
                  reads=["GLd"], writes=["ohg"])
            sc.op("scalar", lambda e: e.activation(out=GLd[:], in_=GLd[:], func=AF.Exp), reads=["GLd", "ohg"], writes=["GLd"])
            sc.op(V_, lambda e: e.reduce_sum(out=gsum, in_=GLd[:], axis=AX.X), reads=["GLd", "sm"], writes=["sm"])
            sc.op(V_, lambda e: e.reciprocal(out=gw, in_=gsum), reads=["sm"], writes=["sm"])
            sc.op(V_, lambda e: e.tensor_scalar(out=pen[:], in0=ohg[:], scalar1=-1.0, scalar2=1e30, op0=ALU.add, op1=ALU.mult),
                  reads=["ohg"], writes=["pen"])
            sc.op(V_, lambda e: e.tensor_tensor(out=ELm[:], in0=LG[:, :, 4:36].rearrange("p t (g j) -> p t g j", g=4),
                                                in1=pen[:].unsqueeze(3).broadcast_to([128, 32, 4, 8]), op=ALU.add),
                  reads=LK + ["pen"], writes=["ELm"])
            sc.op(V_, lambda e: e.reduce_max(out=m1, in_=ELm3, axis=AX.X), reads=["ELm", "sm"], writes=["sm"])
            sc.op(V_, lambda e: e.tensor_tensor(out=oh1[:], in0=ELm3, in1=bc32(m1), op=ALU.is_equal),
                  reads=["ELm", "sm"], writes=["oh1"])
            sc.op(V_, lambda e: e.scalar_tensor_tensor(out=EL2[:], in0=oh1[:], scalar=-1e30, in1=ELm3, op0=ALU.mult,
                                                       op1=ALU.add), reads=["oh1", "ELm"], writes=["EL2"])
            sc.op(V_, lambda e: e.reduce_max(out=m2, in_=EL2[:], axis=AX.X), reads=["EL2", "sm"], writes=["sm"])
            sc.op(V_, lambda e: e.tensor_tensor(out=oh2[:], in0=EL2[:], in1=bc32(m2), op=ALU.is_equal),
                  reads=["EL2", "sm"], writes=["oh2"])
            sc.op(V_, lambda e: e.tensor_tensor(out=dm, in0=m2, in1=m1, op=ALU.subtract), reads=["sm"], writes=["sm"])
            sc.op("scalar", lambda e: e.activation(out=dm, in_=dm, func=AF.Exp), reads=["sm"], writes=["sm"])
            sc.op(V_, lambda e: e.tensor_scalar(out=dm, in0=dm, scalar1=1.0, scalar2=None, op0=ALU.add), reads=["sm"], writes=["sm"])
            sc.op(V_, lambda e: e.reciprocal(out=dm, in_=dm), reads=["sm"], writes=["sm"])
            sc.op(V_, lambda e: e.tensor_tensor(out=wts[:, 0, :], in0=gw, in1=dm, op=ALU.mult), reads=["sm"], writes=["wts"])
            sc.op(V_, lambda e: e.tensor_tensor(out=wts[:, 1, :], in0=gw, in1=wts[:, 0, :], op=ALU.subtract),
                  reads=["sm", "wts"], writes=["wts"])
            sc.op(V_, lambda e: e.tensor_tensor(out=Mb[:], in0=oh1[:], in1=oh2[:], op=ALU.add), reads=["oh1", "oh2"], writes=["Mb"])
            for half in range(2):
                rhs = Mb[:, half * 16:(half + 1) * 16, :].rearrange("p t e -> p (t e)")
                sc.op("tensor", lambda e, half=half, rhs=rhs: e.matmul(PS[half][:, :], lhsT=U_b[:], rhs=rhs, start=True, stop=True),
                      reads=["Mb", "U_b"], writes=["ps%d" % half])
                sc.op("tensor", lambda e, half=half, rhs=rhs: e.matmul(PS[2 + half][:, :], lhsT=ones_b[:], rhs=rhs, start=True,
                                                                       stop=True),
                      reads=["Mb", "ones_b"], writes=["ps%d" % (2 + half)])
                sc.op(V_, lambda e, half=half: e.tensor_copy(
                    out=pre[:, half * 16:(half + 1) * 16, :].rearrange("p t e -> p (t e)"), in_=PS[half][:, :]),
                    reads=["ps%d" % half], writes=["pre"])
                sc.op("scalar", lambda e, half=half: e.copy(
                    out=tot[:, half * 16:(half + 1) * 16, :].rearrange("p t e -> p (t e)"), in_=PS[2 + half][:, :]),
                    reads=["ps%d" % (2 + half)], writes=["tot"])
            sc.op(V_, lambda e: e.memset(base[:, 0, :], 0.0), writes=["base"])
            for t in range(1, 32):
                sc.op(V_, lambda e, t=t: e.tensor_tensor(out=base[:, t, :], in0=base[:, t - 1, :], in1=tot[:, t - 1, :],
                                                         op=ALU.add), reads=["base", "tot"], writes=["base"])
            sc.op(V_, lambda e: e.tensor_tensor(out=pre[:], in0=pre[:], in1=base[:], op=ALU.add), reads=["pre", "base"],
                  writes=["pre"])
            for k, oh in enumerate([oh1, oh2]):
                ohk = "oh%d" % (k + 1)
                sc.op(V_, lambda e, oh=oh: e.tensor_tensor(out=prod[:], in0=oh[:], in1=pre[:], op=ALU.mult),
                      reads=[ohk, "pre"], writes=["prod"])
                sc.op(V_, lambda e: e.reduce_sum(out=rsel, in_=prod[:], axis=AX.X), reads=["prod", "sm"], writes=["sm"])
                sc.op(V_, lambda e, oh=oh: e.tensor_tensor(out=prod[:], in0=oh[:],
                                                           in1=eoff[:].unsqueeze(1).broadcast_to([128, 32, 32]), op=ALU.mult),
                      reads=[ohk, "eoff", "sm"], writes=["prod"])
                sc.op(V_, lambda e: e.reduce_sum(out=esel, in_=prod[:], axis=AX.X), reads=["prod", "sm"], writes=["sm"])
                sc.op(V_, lambda e, k=k: e.tensor_tensor(out=destf[:, k, :], in0=rsel, in1=esel, op=ALU.add),
                      reads=["sm"], writes=["destf"])
                sc.op(V_, lambda e: e.tensor_scalar(out=rsel, in0=rsel, scalar1=float(CAP), scalar2=1e6, op0=ALU.is_ge,
                                                    op1=ALU.mult), reads=["sm", "destf"], writes=["sm"])
                sc.op(V_, lambda e, k=k: e.tensor_tensor(out=destf[:, k, :], in0=destf[:, k, :], in1=rsel, op=ALU.add),
                      reads=["sm", "destf"], writes=["destf"])
            sc.op(V_, lambda e: e.tensor_copy(out=dest_i[:], in_=destf[:]), reads=["destf"], writes=["dest"])
            if G.get("dbg"):
                rt_o = G["rt_o"]
                sc.dma("sync", "dbg3", lambda e: e.dma_start(out=rt_o.ap()[:, 0:2, :], in_=destf[:]), reads=["destf", "dest"],
                       writes=["rt_o"])
                sc.dma("sync", "dbg4", lambda e: e.dma_start(out=rt_o.ap()[:, 2:4, :], in_=wts[:]), reads=["wts"], writes=["rt_o2"])
            for en in sc.ENGS:
                sc.wait_all(en)

        stage_raw = [sb("c_stage%d" % i, [128, 4096]) for i in range(3)]
        wg = [sb("c_wg%d" % i, [128, 8, 512], BF16) for i in range(2)]
        wu = [sb("c_wu%d" % i, [128, 8, 512], BF16) for i in range(2)]
        wd = [sb("c_wd%d" % i, [128, 4, D], BF16) for i in range(2)]
        xb4 = [sb("c_xb4_%d" % i, [128, CAP // 128, D], BF16) for i in range(2)]
        xbT4 = [sb("c_xbT4_%d" % i, [128, 8, CAP], BF16) for i in range(2)]
        sgt2 = [sb("c_sgt%d" % i, [128, 512]) for i in range(2)]
        hidT4 = [sb("c_hid4_%d" % i, [128, 4, CAP], BF16) for i in range(2)]
        yo = [sb("c_yo%d" % i, [128, D]) for i in range(2)]
        h2_v = h2_s.ap().rearrange("(t p) d -> t p d", p=128)
        for t in range(ntile):
            x_ = xb[t % 2]
            xk = "xb%d" % (t % 2)
            sc.dma("sync", "c_" + xk, lambda e, t=t, x_=x_: e.dma_start(out=x_[:], in_=h2_v[t]),
                   reads=["h2_s%d" % t], writes=[xk])
            for k in range(2):
                sc.dma("gpsimd", "c_sc%d_%d" % (t % 2, k), lambda e, t=t, k=k, x_=x_: e.indirect_dma_start(
                    out=XS.ap(), out_offset=bass.IndirectOffsetOnAxis(ap=dest_i[:, k, t:t + 1], axis=0),
                    in_=x_[:], in_offset=None, bounds_check=G["bnd"]["r"], oob_is_err=False),
                    reads=[xk, "dest", "XS"], writes=["XSk%d" % k])

        nexp = G.get("nexp", NE)
        sg_v = stage_raw[0][:].rearrange("p (k n) -> p k n", k=8)
        su_v = stage_raw[1][:].rearrange("p (k n) -> p k n", k=8)
        sd_v = stage_raw[2][:].rearrange("p (k n) -> p k n", k=4)

        def load_w(ex):
            sc.dma("sync", "c_st0", lambda e: e.dma_start(out=sg_v, in_=w_g_d.ap()[ex].rearrange("(kc p) n -> p kc n", p=128)),
                   writes=["st0"])
            sc.dma("sync", "c_st1", lambda e: e.dma_start(out=su_v, in_=w_u_d.ap()[ex].rearrange("(kc p) n -> p kc n", p=128)),
                   writes=["st1"])
            sc.dma("sync", "c_st2", lambda e: e.dma_start(out=sd_v, in_=w_d_d.ap()[ex].rearrange("(kc p) n -> p kc n", p=128)),
                   writes=["st2"])

        def cast_w(ex):
            p2 = ex % 2
            sc.op("scalar", lambda e: e.copy(out=wg[p2][:], in_=sg_v), reads=["st0"], writes=["wg%d" % p2])
            sc.op("vector", lambda e: e.tensor_copy(out=wu[p2][:], in_=su_v), reads=["st1"], writes=["wu%d" % p2])
            sc.op("vector", lambda e: e.tensor_copy(out=wd[p2][:], in_=sd_v), reads=["st2"], writes=["wd%d" % p2])

        def load_xb(ex):
            p2 = ex % 2
            sc.dma("sync", "c_xb4_%d" % p2, lambda e: e.dma_start(
                out=xb4[p2][:], in_=XS.ap()[ex * CAP:(ex + 1) * CAP, :].rearrange("(r p) d -> p r d", p=128)),
                reads=["XS", "XSk0", "XSk1"], writes=["xb4_%d" % p2])

        load_xb(0)
        load_w(0)
        for ex in range(nexp):
            p2 = ex % 2
            cast_w(ex)
            if ex + 1 < nexp:
                load_xb(ex + 1)
                load_w(ex + 1)
            NB = CAP // 128
            for r in range(NB):
                pb = PB[r % 2]
                pk = "pb%d" % (r % 2)

                def tr(e, r=r, pb=pb, p2=p2):
                    for kc in range(8):
                        ins = e.transpose(out=pb[:, kc * 128:(kc + 1) * 128], in_=xb4[p2][:, r, kc * 128:(kc + 1) * 128],
                                          identity=ident_b[:])
                    return ins
                sc.op("tensor", tr, reads=["xb4_%d" % p2, "ident_b"], writes=[pk])
                sc.op("vector" if r % 2 else "scalar",
                      (lambda e, r=r, pb=pb, p2=p2: e.tensor_copy(out=xbT4[p2][:, :, r * 128:(r + 1) * 128],
                                                                 in_=pb[:].rearrange("p (k t) -> p k t", k=8))) if r % 2 else
                      (lambda e, r=r, pb=pb, p2=p2: e.copy(out=xbT4[p2][:, :, r * 128:(r + 1) * 128],
                                                           in_=pb[:].rearrange("p (k t) -> p k t", k=8))),
                      reads=[pk], writes=["xbT4_%d_%d" % (p2, r)])
            xkeys = ["xbT4_%d_%d" % (p2, r) for r in range(NB)]
            for f in range(4):
                bg, bu = (0, 1) if f % 2 == 0 else (2, 3)

                def gu(e, p2=p2, f=f, bg=bg, bu=bu):
                    for (bb, w) in ((bg, wg[p2]), (bu, wu[p2])):
                        for kc in range(8):
                            ins = e.matmul(PS[bb][:, 0:CAP], lhsT=w[:, kc, f * 128:(f + 1) * 128], rhs=xbT4[p2][:, kc, :],
                                           start=(kc == 0), stop=(kc == 7))
                    return ins
                sc.op("tensor", gu, reads=xkeys + ["wg%d" % p2, "wu%d" % p2], writes=["ps%d" % bg, "ps%d" % bu])
                sg_ = sgt2[f % 2]
                sc.op("scalar", lambda e, bg=bg, sg_=sg_: e.activation(out=sg_[:, 0:CAP], in_=PS[bg][:, 0:CAP], func=AF.Silu),
                      reads=["ps%d" % bg], writes=["sgt%d" % (f % 2)])
                sc.op("vector", lambda e, bu=bu, sg_=sg_, f=f, p2=p2: e.tensor_tensor(
                    out=hidT4[p2][:, f, :], in0=PS[bu][:, 0:CAP], in1=sg_[:, 0:CAP], op=ALU.mult),
                    reads=["ps%d" % bu, "sgt%d" % (f % 2)], writes=["hid4_%d" % p2])
            for r in range(NB):
                row0 = ex * CAP + r * 128
                i2 = r % 2
                for half in range(2):
                    bd = 4 + half

                    def dn(e, half=half, bd=bd, r=r, p2=p2):
                        for fc in range(4):
                            ins = e.matmul(PS[bd][:, :], lhsT=hidT4[p2][:, fc, r * 128:(r + 1) * 128],
                                           rhs=wd[p2][:, fc, half * 512:(half + 1) * 512], start=(fc == 0), stop=(fc == 3))
                        return ins
                    sc.op("tensor", dn, reads=["hid4_%d" % p2, "wd%d" % p2], writes=["ps%d" % bd])
                    if half == 0:
                        sc.op("scalar", lambda e, i2=i2, bd=bd: e.copy(out=yo[i2][:, 0:512], in_=PS[bd][:, :]),
                              reads=["ps%d" % bd], writes=["yo%d" % i2])
                    else:
                        sc.op("vector", lambda e, i2=i2, bd=bd: e.tensor_copy(out=yo[i2][:, 512:1024], in_=PS[bd][:, :]),
                              reads=["ps%d" % bd], writes=["yo%d" % i2])
                sc.dma("sync", "c_yo%d" % i2, lambda e, row0=row0, i2=i2: e.dma_start(out=YS.ap()[row0:row0 + 128, :], in_=yo[i2][:]),
                       reads=["yo%d" % i2], writes=["YS"])
        for en in sc.ENGS:
            sc.wait_all(en)


def phase_d(nc, sc, G):
    names = ["dest_i", "wts", "x1_s", "YS", "out_d"]
    dest_i, wts, x1_s, YS, out_d = (G[k] for k in names)
    ntile = G["nst"] * 4
    NBUF = 4
    with contextlib.ExitStack() as st:
        sb = lambda name, shape, dt=F32: st.enter_context(nc.sbuf_tensor("sb_" + name, list(shape), dt))
        x1t = [sb("d_x1%d" % i, [128, D]) for i in range(NBUF)]
        y1 = [sb("d_y1%d" % i, [128, D]) for i in range(NBUF)]
        y2 = [sb("d_y2%d" % i, [128, D]) for i in range(NBUF)]
        x1_v = x1_s.ap().rearrange("(t p) d -> t p d", p=128)
        out_v = out_d.ap().rearrange("(t p) d -> t p d", p=128)

        def prep(t):
            i2 = t % NBUF
            sc.dma("sync", "d_x1%d" % i2, lambda e: e.dma_start(out=x1t[i2][:], in_=x1_v[t]),
                   reads=["x1_s%d" % t], writes=["dx1%d" % i2])
            for k, yt in enumerate((y1, y2)):
                yk = "dy%d_%d" % (k, i2)
                sc.op("vector", lambda e, yt=yt: e.memset(yt[i2][:], 0.0), writes=[yk])
                sc.dma("gpsimd", "d_g%d_%d" % (k, i2), lambda e, yt=yt, k=k: e.indirect_dma_start(
                    out=yt[i2][:], out_offset=None, in_=YS.ap(),
                    in_offset=bass.IndirectOffsetOnAxis(ap=dest_i[:, k, t:t + 1], axis=0),
                    bounds_check=G["bnd"]["r"], oob_is_err=False), reads=["YS", "dest"], writes=[yk])

        def finish(t):
            i2 = t % NBUF
            sc.op("vector", lambda e: e.scalar_tensor_tensor(
                out=x1t[i2][:], in0=y1[i2][:], scalar=wts[:, 0, t:t + 1], in1=x1t[i2][:], op0=ALU.mult, op1=ALU.add),
                reads=["dy0_%d" % i2, "wts", "dx1%d" % i2], writes=["dx1%d" % i2])
            sc.op("vector", lambda e: e.scalar_tensor_tensor(
                out=x1t[i2][:], in0=y2[i2][:], scalar=wts[:, 1, t:t + 1], in1=x1t[i2][:], op0=ALU.mult, op1=ALU.add),
                reads=["dy1_%d" % i2, "wts", "dx1%d" % i2], writes=["dx1%d" % i2])
            sc.dma("sync", "d_o%d" % i2, lambda e: e.dma_start(out=out_v[t], in_=x1t[i2][:]),
                   reads=["dx1%d" % i2], writes=["out%d" % t])

        AHEAD = 2
        for t in range(min(AHEAD, ntile)):
            prep(t)
        for t in range(ntile):
            if t + AHEAD < ntile:
                prep(t + AHEAD)
            finish(t)
        for en in sc.ENGS:
            sc.wait_all(en)


def _consts():
    c = np.zeros((128, 512), np.float32)
    c[:, 0:128] = np.eye(128, dtype=np.float32)
    k = np.arange(128)[:, None]
    q = np.arange(128)[None, :]
    c[:, 128:256] = (k <= q).astype(np.float32)
    c[:, 256:384] = (k < q).astype(np.float32)
    c[:, 384:512] = ((k // 64) == (q // 64)).astype(np.float32)
    return c


def make_in_maps(inputs, cores):
    f = lambda a: np.ascontiguousarray(np.asarray(a, dtype=np.float32))
    w_in = f(inputs["w_in"][0])
    g1T = f(inputs["attn_norm_g"][0].reshape(8, 128).T)
    cw = f(inputs["conv_dw_w"][0].reshape(31, 4, 128).transpose(2, 1, 0))
    cvec = f(np.stack([inputs["conv_dw_b"][0].reshape(4, 128).T, inputs["conv_ln_g"][0].reshape(4, 128).T,
                       inputs["conv_ln_b"][0].reshape(4, 128).T], axis=-1))
    qkg = f(np.stack([np.tile(inputs["q_norm_g"][0], 2), np.tile(inputs["k_norm_g"][0], 2)], axis=-1))
    cst = _consts()
    rep = lambda v: f(np.broadcast_to(np.asarray(v, np.float32)[None], (128,) + tuple(np.shape(v))))
    shared = {
        "w_in": w_in, "cst": cst, "g1T": g1T, "cw": cw, "cvec": cvec, "qkg": qkg,
        "w_co": f(inputs["w_conv_out"][0]), "w_ao": f(inputs["w_attn_out"][0]), "w_out": f(inputs["w_out"][0]),
        "g2bc": rep(inputs["ffn_norm_g"][0]),
        "lamv": rep(np.stack([inputs["lambda_q1"][0], inputs["lambda_k1"][0], inputs["lambda_q2"][0], inputs["lambda_k2"][0]])),
        "sgbc": rep(inputs["subln_g"][0]),
        "wr": f(np.concatenate([inputs["w_router_group"][0], inputs["w_router_expert"][0]], axis=1)),
        "rbbc": rep(np.concatenate([inputs["b_router_group"][0], inputs["b_router_expert"][0]])),
        "eoff": rep(np.arange(NE, dtype=np.float32) * CAP),
        "w_g": f(inputs["w_gate_e"][0]), "w_u": f(inputs["w_up_e"][0]), "w_d": f(inputs["w_down_e"][0]),
    }
    maps = []
    for c in cores:
        m = dict(shared)
        m["x"] = f(inputs["x"][c])
        maps.append(m)
    return maps


def kernel(**inputs):
    nc = build_program()
    maps = make_in_maps(inputs, list(range(8)))
    res = run_bass_kernel_spmd(nc, maps, core_ids=list(range(8)))
    return np.stack([np.asarray(r["out"]).reshape(S, D) for r in res.results], axis=0).astype(np.float32)
```

```python
import contextlib
import numpy as np
import concourse.bass as bass
import concourse.mybir as mybir
from concourse.bass_utils import run_bass_kernel_spmd

F32 = mybir.dt.float32
BF16 = mybir.dt.bfloat16
I32 = mybir.dt.int32
ALU = mybir.AluOpType
AF = mybir.ActivationFunctionType
AX = mybir.AxisListType

S = 4096
D = 1024
NST = 8
EPS = 1e-6
LAM_INIT = 0.8 - 0.6 * 1.0
CAP = 512
NE = 32


class Sched:
    ENGS = ["sync", "scalar", "vector", "gpsimd", "tensor"]

    def __init__(self, nc, stack, n_dma_sems=80):
        self.nc = nc
        self.streams = {e: [] for e in self.ENGS}
        self.sems = {}
        self.count = {}
        for e in self.ENGS:
            self.sems["e:" + e] = stack.enter_context(nc.semaphore("sem_" + e))
            self.count["e:" + e] = 0
        self.free_dma = [stack.enter_context(nc.semaphore("dsem%d" % i)) for i in range(n_dma_sems)]
        self.waited = {e: {} for e in self.ENGS}
        self.last_w = {}
        self.readers = {}

    def _dsem(self, key):
        k = "d:" + key
        if k not in self.sems:
            self.sems[k] = self.free_dma.pop()
            self.count[k] = 0
        return k

    def _deps(self, reads, writes):
        toks = []
        for k in reads:
            t = self.last_w.get(k)
            if t is not None:
                toks.append(t)
        for k in writes:
            t = self.last_w.get(k)
            if t is not None:
                toks.append(t)
            for sk, v in self.readers.get(k, {}).items():
                toks.append((sk, v))
        return toks

    def _wait(self, eng, toks):
        w = self.waited[eng]
        need = {}
        for sk, v in toks:
            if w.get(sk, 0) >= v:
                continue
            if eng == "tensor" and sk == "e:tensor":
                continue
            if need.get(sk, 0) < v:
                need[sk] = v
        for sk, v in need.items():
            w[sk] = v
            sem = self.sems[sk]
            self.streams[eng].append(lambda e, sem=sem, v=v: e.wait_ge(sem, v))

    def _commit(self, tok, reads, writes):
        sk, v = tok
        for k in reads:
            r = self.readers.setdefault(k, {})
            if r.get(sk, 0) < v:
                r[sk] = v
        for k in writes:
            self.last_w[k] = tok
            self.readers[k] = {}

    def op(self, eng, fn, reads=(), writes=()):
        self._wait(eng, self._deps(reads, writes))
        sk = "e:" + eng
        self.count[sk] += 1
        n = self.count[sk]
        sem = self.sems[sk]
        self.streams[eng].append(lambda e, fn=fn, sem=sem: fn(e).then_inc(sem, 1))
        tok = (sk, n)
        self._commit(tok, reads, writes)
        return tok

    def dma(self, q, semkey, fn, reads=(), writes=()):
        self._wait(q, self._deps(reads, writes))
        sk = self._dsem(semkey)
        self.count[sk] += 16
        n = self.count[sk]
        sem = self.sems[sk]
        self.streams[q].append(lambda e, fn=fn, sem=sem: fn(e).then_inc(sem, 16))
        tok = (sk, n)
        self._commit(tok, reads, writes)
        return tok

    def wait_all(self, eng):
        toks = []
        for k, t in self.last_w.items():
            toks.append(t)
        for k, r in self.readers.items():
            for sk, v in r.items():
                toks.append((sk, v))
        self._wait(eng, toks)

    def flush(self):
        with self.nc.Block() as block:
            for name in self.ENGS:
                fns = self.streams[name]
                if not fns:
                    continue

                def body(e, fns=fns):
                    for f in fns:
                        f(e)
                getattr(block, name)(body)
        self.streams = {e: [] for e in self.ENGS}


def build_program(dbg=False, phases=("A", "B", "C", "D"), nst=NST, stop=99, nexp=NE, bstop=99):
    nc = bass.Bass("TRN2", target_bir_lowering=False)
    okind = "ExternalOutput" if dbg else "Internal"

    def din(name, shape, dt=F32):
        return nc.dram_tensor(name, list(shape), dt, kind="ExternalInput")

    x_d = din("x", [S, D])
    w_in_d = din("w_in", [D, 4608])
    cst_d = din("cst", [128, 128 * 4])
    g1T_d = din("g1T", [128, 8])
    cw_d = din("cw", [128, 4, 31])
    cvec_d = din("cvec", [128, 4, 3])
    qkg_d = din("qkg", [128, 2])
    w_co_d = din("w_co", [512, D])
    w_ao_d = din("w_ao", [512, D])
    w_out_d = din("w_out", [D, D])
    g2bc_d = din("g2bc", [128, D])
    lamv_d = din("lamv", [128, 4, 64])
    sgbc_d = din("sgbc", [128, 128])
    wr_d = din("wr", [D, 36])
    rbbc_d = din("rbbc", [128, 36])
    eoff_d = din("eoff", [128, NE])
    w_g_d = din("w_g", [NE, D, 512])
    w_u_d = din("w_u", [NE, D, 512])
    w_d_d = din("w_d", [NE, 512, D])
    out_d = nc.dram_tensor("out", [S, D], F32, kind="ExternalOutput")
    x1_s = nc.dram_tensor("x1_s", [S, D], F32, kind=okind)
    h2_s = nc.dram_tensor("h2_s", [S, D], BF16, kind=okind)
    XS = nc.dram_tensor("XS", [NE * CAP, D], BF16, kind="Internal")
    YS = nc.dram_tensor("YS", [NE * CAP, D], F32, kind="Internal")
    if dbg:
        lg_o = nc.dram_tensor("lg_o", [128, 32, 36], F32, kind="ExternalOutput")
        rt_o = nc.dram_tensor("rt_o", [128, 4, 32], F32, kind="ExternalOutput")
    qT_s = nc.dram_tensor("qT_s", [128, 4, S], BF16, kind=okind)
    cvT_s = nc.dram_tensor("cvT_s", [128, 4, S], BF16, kind=okind)
    kT_s = nc.dram_tensor("kT_s", [128, 4, S], BF16, kind=okind)
    if dbg:
        v_o = nc.dram_tensor("v_o", [128, 32, 4, 130], BF16, kind="ExternalOutput")

    with contextlib.ExitStack() as st:
        sc = Sched(nc, st)
        sb = lambda name, shape, dt=F32: st.enter_context(nc.sbuf_tensor("sb_" + name, list(shape), dt))
        cst = sb("cst", [128, 512])
        ident_b = sb("ident_b", [128, 128], BF16)
        V = sb("V", [128, 32, 4, 130], BF16)
        PS = [st.enter_context(nc.psum_tensor("ps%d" % i, [128, 512], F32)) for i in range(6)]
        PB = [st.enter_context(nc.psum_tensor("pb%d" % i, [128, 1024], BF16)) for i in range(2)]
        nhalf = sb("nhalf", [128, 512])
        bnd = {}

        def _mk_bnd(e):
            bnd["r"] = e.alloc_register("bnd")
            e.reg_mov(bnd["r"], NE * CAP - 1)
        sc.streams["gpsimd"].append(_mk_bnd)
        LG = sb("LG", [128, 32, 36])
        dest_i = sb("dest_i", [128, 2, 32], I32)
        wts = sb("wts", [128, 2, 32])
        if dbg:
            sc.op("vector", lambda e: e.memset(LG[:], 0.0), writes=["LG%d" % i for i in range(32)])
        zeros_b = sb("zeros_b", [128, D], BF16)
        sc.op("vector", lambda e: e.memset(zeros_b[:], 0.0), writes=["zeros_b"])
        XS_v = XS.ap().rearrange("(r p) d -> r p d", p=128)
        for r in range(NE * CAP // 128):
            sc.dma("sync", "xs_zero", lambda e, r=r: e.dma_start(out=XS_v[r], in_=zeros_b[:]),
                   reads=["zeros_b"], writes=["XS"])
        sc.op("gpsimd", lambda e: e.memset(nhalf[:], -0.5), writes=["nhalf"])

        sc.dma("sync", "cst", lambda e: e.dma_start(out=cst[:], in_=cst_d.ap()), writes=["cst"])
        sc.op("vector", lambda e: e.tensor_copy(out=ident_b[:], in_=cst[:, 0:128]), reads=["cst"], writes=["ident_b"])
        sc.op("gpsimd", lambda e: e.memset(V[:], 1.0), writes=["Vones"] + ["V%d" % i for i in range(NST)])

        if "A" in phases:
            phase_a(nc, sc, locals())
        if "B" in phases:
            phase_b(nc, sc, locals())
        if dbg and "B" in phases:
            sc.dma("sync", "dbg2", lambda e: e.dma_start(out=lg_o.ap(), in_=LG[:]),
                   reads=["LG%d" % i for i in range(32)], writes=["lg_o"])
        if "C" in phases:
            phase_c(nc, sc, locals())
        if "D" in phases:
            phase_d(nc, sc, locals())

        if dbg:
            sc.dma("sync", "dbg", lambda e: e.dma_start(out=v_o.ap(), in_=V[:]),
                   reads=["V%d" % i for i in range(NST)] + ["Vones"], writes=["v_o"])
        sc.wait_all("sync")
        sc.flush()
    return nc


def phase_a(nc, sc, G):
    x_d, w_in_d, g1T_d, cw_d, cvec_d, qkg_d = (G[k] for k in ["x_d", "w_in_d", "g1T_d", "cw_d", "cvec_d", "qkg_d"])
    cst, ident_b, V, PS, PB, qT_s, cvT_s, kT_s, nhalf = (G[k] for k in ["cst", "ident_b", "V", "PS", "PB", "qT_s", "cvT_s", "kT_s", "nhalf"])
    with contextlib.ExitStack() as st:
        sb = lambda name, shape, dt=F32: st.enter_context(nc.sbuf_tensor("sb_" + name, list(shape), dt))
        NCA = 2560
        w_bf = sb("a_wbf", [128, 8, NCA], BF16)
        stage = [sb("a_stage%d" % i, [128, 8, 128]) for i in range(2)]
        g1T = sb("a_g1T", [128, 8])
        cw = sb("a_cw", [128, 4, 31])
        cvec = sb("a_cvec", [128, 4, 3])
        qkg = sb("a_qkg", [128, 2])
        diag = sb("a_diag", [128, 4, 31, 128], BF16)
        onesN = sb("a_onesN", [128, 128], BF16)
        blk = sb("a_blk", [128, 128], BF16)
        xt = [sb("a_xt0", [128, 4, D])] * 2
        ss = sb("a_ss", [128, 8])
        rstd = sb("a_rstd", [128, 8])
        xn = sb("a_xn", [128, 4, D], BF16)
        hT = sb("a_hT", [128, 8, 512], BF16)
        ybuf = [sb("a_ybuf%d" % i, [128, 4, 544], BF16) for i in range(2)]
        ybuf1 = [sb("a_ybuf1_%d" % i, [128, 4, 544], BF16) for i in range(2)]
        sg = [sb("a_sg0", [128, 512])] * 2
        ycb = sb("a_ycb", [128, 4, 512], BF16)
        ysq = sb("a_ysq", [128, 4, 512], BF16)
        mean_sb = sb("a_mean", [128, 512])
        lrstd = sb("a_lrstd", [128, 512])
        zt = [sb("a_zt0", [128, 512])] * 2
        cvT = sb("a_cvT", [128, 4, 512], BF16)
        sq = [sb("a_sq%d" % i, [128, 512], BF16) for i in range(2)]
        qr = [sb("a_qr%d" % i, [128, 512]) for i in range(2)]
        qkT = [sb("a_qT", [128, 4, 512], BF16), sb("a_kT", [128, 4, 512], BF16)]

        sc.dma("sync", "g1T", lambda e: e.dma_start(out=g1T[:], in_=g1T_d.ap()), writes=["g1T"])
        sc.dma("sync", "cw", lambda e: e.dma_start(out=cw[:], in_=cw_d.ap()), writes=["cw"])
        sc.dma("sync", "cvec", lambda e: e.dma_start(out=cvec[:], in_=cvec_d.ap()), writes=["cvec"])
        sc.dma("sync", "qkg", lambda e: e.dma_start(out=qkg[:], in_=qkg_d.ap()), writes=["qkg"])
        w_in_v = w_in_d.ap().rearrange("(kc p) n -> p kc n", p=128)
        for cg in range(NCA // 128):
            sl = stage[cg % 2]
            key = "stage%d" % (cg % 2)
            sc.dma("sync", key, lambda e, sl=sl, cg=cg: e.dma_start(out=sl[:], in_=w_in_v[:, :, cg * 128:(cg + 1) * 128]),
                   writes=[key])
            for kc in range(8):
                if kc % 2 == 0:
                    sc.op("vector", lambda e, sl=sl, cg=cg, kc=kc: e.tensor_scalar(
                        out=w_bf[:, kc, cg * 128:(cg + 1) * 128], in0=sl[:, kc, :], scalar1=g1T[:, kc:kc + 1],
                        scalar2=None, op0=ALU.mult), reads=[key, "g1T"], writes=["wbf%d_%d" % (cg // 4, kc)])
                else:
                    sc.op("scalar", lambda e, sl=sl, cg=cg, kc=kc: e.activation(
                        out=w_bf[:, kc, cg * 128:(cg + 1) * 128], in_=sl[:, kc, :], func=AF.Copy,
                        scale=g1T[:, kc:kc + 1]), reads=[key, "g1T"], writes=["wbf%d_%d" % (cg // 4, kc)])
        wkeys = lambda cg: ["wbf%d_%d" % (cg, kc) for kc in range(8)]
        for ch in range(4):
            for k in range(31):
                if k % 2 == 0:
                    sc.op("vector", lambda e, ch=ch, k=k: e.tensor_scalar(
                        out=diag[:, ch, k, :], in0=cst[:, 0:128], scalar1=cw[:, ch, k:k + 1], scalar2=None,
                        op0=ALU.mult), reads=["cst", "cw"], writes=["diag"])
                else:
                    sc.op("scalar", lambda e, ch=ch, k=k: e.activation(
                        out=diag[:, ch, k, :], in_=cst[:, 0:128], func=AF.Copy, scale=cw[:, ch, k:k + 1]),
                        reads=["cst", "cw"], writes=["diag"])
        sc.op("vector", lambda e: e.memset(onesN[:], 1.0 / 512.0), writes=["onesN"])
        sc.op("vector", lambda e: e.tensor_scalar(out=blk[:], in0=cst[:, 384:512], scalar1=1.0 / 64.0, scalar2=None,
                                                  op0=ALU.mult), reads=["cst"], writes=["blk"])
        for i in range(2):
            sc.op("gpsimd", lambda e, i=i: e.memset(ybuf[i][:], 0.0), writes=["ybuf%d" % i])
            sc.op("gpsimd", lambda e, i=i: e.memset(ybuf1[i][:], 0.0), writes=["ybuf%d" % i])

        x_v = x_d.ap().rearrange("(s j p) d -> s p j d", j=4, p=128)
        bank = [0]

        def nb():
            b = bank[0]
            bank[0] = (b + 1) % 6
            return b

        def load_x(s):
            sc.dma("sync", "xt0", lambda e, s=s: e.dma_start(out=xt[0][:], in_=x_v[s]), writes=["xt0"])

        def tile(s):
            stop = G.get("stop", 99)
            if s == 0:
                load_x(0)
            xs = xt[0]
            xk = "xt0"
            sc.op("vector", lambda e: e.memset(ss[:, 0:4], 0.0), writes=["ss"])
            for j in range(4):
                sc.op("scalar", lambda e, j=j: e.activation(out=xn[:, j, :], in_=xs[:, j, :], func=AF.Square,
                                                            scale=1.0 / 32.0, accum_out=ss[:, j:j + 1]),
                      reads=[xk, "ss"], writes=["xn%d" % j, "ss"])
            sc.op("vector", lambda e: e.tensor_scalar(out=rstd[:, 0:4], in0=ss[:, 0:4], scalar1=EPS, scalar2=None,
                                                      op0=ALU.add), reads=["ss"], writes=["rstd"])
            sc.op("gpsimd", lambda e: e.tensor_tensor(out=rstd[:, 0:4], in0=rstd[:, 0:4], in1=nhalf[:, 0:4], op=ALU.pow),
                  reads=["rstd", "nhalf"], writes=["rstd"])
            for j in range(4):
                sc.op("vector", lambda e, j=j: e.tensor_scalar(
                    out=xn[:, j, :], in0=xs[:, j, :], scalar1=rstd[:, j:j + 1], scalar2=None, op0=ALU.mult),
                    reads=[xk, "rstd"], writes=["xn%d" % j])
            if s + 1 < G["nst"]:
                load_x(s + 1)
            if stop < 2:
                return
            for j in range(4):
                pb = PB[j % 2]
                pk = "pb%d" % (j % 2)

                def tr(e, j=j, pb=pb):
                    for kc in range(8):
                        ins = e.transpose(out=pb[:, kc * 128:(kc + 1) * 128], in_=xn[:, j, kc * 128:(kc + 1) * 128],
                                          identity=ident_b[:])
                    return ins
                sc.op("tensor", tr, reads=["xn%d" % j, "ident_b"], writes=[pk])
                sc.op("vector" if j % 2 else "scalar",
                      (lambda e, j=j, pb=pb: e.tensor_copy(out=hT[:, :, j * 128:(j + 1) * 128],
                                                           in_=pb[:].rearrange("p (k t) -> p k t", k=8))) if j % 2 else
                      (lambda e, j=j, pb=pb: e.copy(out=hT[:, :, j * 128:(j + 1) * 128],
                                                    in_=pb[:].rearrange("p (k t) -> p k t", k=8))),
                      reads=[pk], writes=["hT%d" % j])
            hkeys = ["hT%d" % j for j in range(4)]

            def fm_matmul(m):
                b = nb()

                def f(e, m=m, b=b):
                    for kc in range(8):
                        ins = e.matmul(PS[b][:, :], lhsT=w_bf[:, kc, m * 128:(m + 1) * 128], rhs=hT[:, kc, :],
                                       start=(kc == 0), stop=(kc == 7))
                    return ins
                sc.op("tensor", f, reads=hkeys + wkeys(m // 4), writes=["ps%d" % b])
                return b

            if stop < 2.1:
                return
            yb = ybuf[s % 2]
            ybk = "ybuf%d" % (s % 2)
            ybo = ybuf[(s + 1) % 2]
            yb1 = ybuf1[s % 2]
            ybo1 = ybuf1[(s + 1) % 2]
            ybok = "ybuf%d" % ((s + 1) % 2)
            for ch in range(4):
                bg = fm_matmul(4 + ch)
                ba = fm_matmul(ch)
                sgt = sg[ch % 2]
                if stop < 2.3:
                    continue
                sc.op("scalar", lambda e, bg=bg, sgt=sgt: e.activation(out=sgt[:], in_=PS[bg][:, :], func=AF.Sigmoid),
                      reads=["ps%d" % bg], writes=["sg0"])
                if stop < 2.6:
                    continue
                if s > 0:
                    sc.op("gpsimd", lambda e, ch=ch: e.tensor_copy(out=yb[:, ch, 0:30], in_=ybo[:, ch, 512:542]),
                          reads=[ybok + "_%d" % ch], writes=[ybk + "_%d" % ch])
                    sc.op("gpsimd", lambda e, ch=ch: e.tensor_copy(out=yb1[:, ch, 0:29], in_=ybo1[:, ch, 512:541]),
                          reads=[ybok + "_%d" % ch], writes=[ybk + "_%d" % ch])
                sc.op("vector", lambda e, ba=ba, sgt=sgt, ch=ch: e.tensor_tensor(
                    out=yb[:, ch, 30:542], in0=PS[ba][:, :], in1=sgt[:], op=ALU.mult),
                    reads=["ps%d" % ba, "sg0", ybk], writes=[ybk + "_%d" % ch])
                sc.op("gpsimd", lambda e, ch=ch: e.tensor_copy(out=yb1[:, ch, 29:541], in_=yb[:, ch, 30:542]),
                      reads=[ybk + "_%d" % ch], writes=[ybk + "_%d" % ch])
            if stop < 3.5:
                return
            for ch in range(4):
                b = nb()

                def cf(e, ch=ch, b=b):
                    for k in (range(31) if stop != 3.6 else [0, 16, 30]):
                        rhs = yb[:, ch, k:k + 512] if k % 2 == 0 else yb1[:, ch, k - 1:k - 1 + 512]
                        ins = e.matmul(PS[b][:, :], lhsT=diag[:, ch, k, :], rhs=rhs, start=(k == 0), stop=(k == 30))
                    return ins
                sc.op("tensor", cf, reads=[ybk + "_%d" % ch, "diag", ybk], writes=["ps%d" % b])
                if stop < 3.8:
                    continue
                sc.op("vector", lambda e, ch=ch, b=b: e.tensor_scalar(
                    out=ycb[:, ch, :], in0=PS[b][:, :], scalar1=cvec[:, ch, 0:1], scalar2=None, op0=ALU.add),
                    reads=["ps%d" % b, "cvec"], writes=["ycb%d" % ch])
                if stop < 3.9:
                    continue
                sc.op("scalar", lambda e, ch=ch, b=b: e.activation(
                    out=ysq[:, ch, :], in_=ycb[:, ch, :], func=AF.Square),
                    reads=["ycb%d" % ch], writes=["ysq%d" % ch])
            if stop < 5:
                return
            bm = nb()
            bq = nb()

            def stf(e, bm=bm, bq=bq):
                for ch in range(4):
                    e.matmul(PS[bm][:, :], lhsT=onesN[:], rhs=ycb[:, ch, :], start=(ch == 0), stop=(ch == 3))
                for ch in range(4):
                    ins = e.matmul(PS[bq][:, :], lhsT=onesN[:], rhs=ysq[:, ch, :], start=(ch == 0), stop=(ch == 3))
                return ins
            sc.op("tensor", stf, reads=["ycb%d" % c for c in range(4)] + ["ysq%d" % c for c in range(4)] + ["onesN"],
                  writes=["ps%d" % bm, "ps%d" % bq])
            sc.op("scalar", lambda e, bm=bm: e.copy(out=mean_sb[:], in_=PS[bm][:, :]), reads=["ps%d" % bm], writes=["mean"])
            sc.op("vector", lambda e: e.tensor_tensor(out=lrstd[:], in0=mean_sb[:], in1=mean_sb[:], op=ALU.mult),
                  reads=["mean"], writes=["lrstd"])
            sc.op("vector", lambda e, bq=bq: e.tensor_tensor(out=lrstd[:], in0=PS[bq][:, :], in1=lrstd[:], op=ALU.subtract),
                  reads=["ps%d" % bq, "lrstd"], writes=["lrstd"])
            sc.op("vector", lambda e: e.tensor_scalar(out=lrstd[:], in0=lrstd[:], scalar1=EPS, scalar2=None,
                                                      op0=ALU.add), reads=["lrstd"], writes=["lrstd"])
            sc.op("scalar", lambda e: e.activation(out=lrstd[:], in_=lrstd[:], func=AF.Sqrt), reads=["lrstd"], writes=["lrstd"])
            sc.op("vector", lambda e: e.reciprocal(out=lrstd[:], in_=lrstd[:]), reads=["lrstd"], writes=["lrstd"])
            for ch in range(4):
                z = zt[ch % 2]
                zk = "zt0"
                sc.op("gpsimd", lambda e, ch=ch, z=z: e.tensor_tensor(out=z[:], in0=ycb[:, ch, :], in1=mean_sb[:],
                                                                      op=ALU.subtract),
                      reads=["ycb%d" % ch, "mean"], writes=[zk])
                sc.op("vector", lambda e, z=z: e.tensor_tensor(out=z[:], in0=z[:], in1=lrstd[:], op=ALU.mult),
                      reads=[zk, "lrstd"], writes=[zk])
                sc.op("vector", lambda e, ch=ch, z=z: e.tensor_scalar(out=z[:], in0=z[:], scalar1=cvec[:, ch, 1:2],
                                                                      scalar2=cvec[:, ch, 2:3], op0=ALU.mult, op1=ALU.add),
                      reads=[zk, "cvec"], writes=[zk])
                sc.op("scalar", lambda e, ch=ch, z=z: e.activation(out=cvT[:, ch, :], in_=z[:], func=AF.Silu),
                      reads=[zk], writes=["cvT"])
            sc.dma("sync", "cvT_st", lambda e, s=s: e.dma_start(out=cvT_s.ap()[:, :, s * 512:(s + 1) * 512], in_=cvT[:]),
                   reads=["cvT"], writes=["cvT_s%d" % s])
            if stop < 6:
                return
            def qk_stage1(ci):
                which, h = divmod(ci, 4)
                b = fm_matmul(8 + which * 4 + h)
                i2 = ci % 2
                sc.op("scalar", lambda e: e.activation(out=sq[i2][:], in_=PS[b][:, :], func=AF.Square),
                      reads=["ps%d" % b], writes=["sq%d" % i2])
                return b

            def qk_stage2(ci, b):
                which, h = divmod(ci, 4)
                i2 = ci % 2
                b2 = nb()
                sc.op("tensor", lambda e: e.matmul(PS[b2][:, :], lhsT=blk[:], rhs=sq[i2][:], start=True, stop=True),
                      reads=["sq%d" % i2, "blk"], writes=["ps%d" % b2])
                sc.op("vector", lambda e: e.tensor_scalar(out=qr[i2][:], in0=PS[b2][:, :], scalar1=EPS, scalar2=None,
                                                          op0=ALU.add), reads=["ps%d" % b2], writes=["qr%d" % i2])
                sc.op("scalar", lambda e: e.activation(out=qr[i2][:], in_=qr[i2][:], func=AF.Sqrt),
                      reads=["qr%d" % i2], writes=["qr%d" % i2])
                sc.op("vector", lambda e: e.reciprocal(out=qr[i2][:], in_=qr[i2][:]), reads=["qr%d" % i2], writes=["qr%d" % i2])
                sc.op("vector", lambda e: e.scalar_tensor_tensor(
                    out=qkT[which][:, h, :], in0=PS[b][:, :], scalar=qkg[:, which:which + 1], in1=qr[i2][:],
                    op0=ALU.mult, op1=ALU.mult), reads=["ps%d" % b, "qr%d" % i2, "qkg"], writes=["qkT%d" % which])
                if h == 3:
                    dd = qT_s if which == 0 else kT_s
                    sc.dma("sync", "qk_st%d" % which, lambda e: e.dma_start(
                        out=dd.ap()[:, :, s * 512:(s + 1) * 512], in_=qkT[which][:]),
                        reads=["qkT%d" % which], writes=["qk_s%d_%d" % (which, s)])

            pb_ = qk_stage1(0)
            for ci in range(8):
                nb_ = qk_stage1(ci + 1) if ci + 1 < 8 else None
                qk_stage2(ci, pb_)
                pb_ = nb_
            if stop < 7:
                return
            for j in range(4):
                b = nb()

                def vf(e, j=j, b=b):
                    for kc in range(8):
                        ins = e.matmul(PS[b][:, :], lhsT=hT[:, kc, j * 128:(j + 1) * 128], rhs=w_bf[:, kc, 2048:2560],
                                       start=(kc == 0), stop=(kc == 7))
                    return ins
                sc.op("tensor", vf, reads=["hT%d" % j] + wkeys(4), writes=["ps%d" % b])
                sc.op("scalar" if j % 2 else "vector",
                      (lambda e, j=j, b=b: e.copy(out=V[:, s * 4 + j, :, 0:128],
                                                  in_=PS[b][:, :].rearrange("p (h e) -> p h e", h=4))) if j % 2 else
                      (lambda e, j=j, b=b: e.tensor_copy(out=V[:, s * 4 + j, :, 0:128],
                                                         in_=PS[b][:, :].rearrange("p (h e) -> p h e", h=4))),
                      reads=["ps%d" % b], writes=["V%d" % s])
        for s in range(G["nst"]):
            tile(s)
        for en in sc.ENGS:
            sc.wait_all(en)


def load_cast(sc, nc, stage, stage_key, src_view, dst, ncols, kcn, dkey, scale=None):
    for cg in range(ncols // 128):
        sl = stage[cg % 2]
        key = stage_key + str(cg % 2)
        sc.dma("sync", key, lambda e, sl=sl, cg=cg: e.dma_start(out=sl[:, 0:kcn, :], in_=src_view[:, :, cg * 128:(cg + 1) * 128]),
               writes=[key])
        if scale is None:
            eng = ["vector", "gpsimd"][cg % 2]
            sc.op(eng, lambda e, sl=sl, cg=cg: e.tensor_copy(out=dst[:, :, cg * 128:(cg + 1) * 128], in_=sl[:, 0:kcn, :]),
                  reads=[key], writes=[dkey])
        else:
            for kc in range(kcn):
                if kc % 2 == 0:
                    sc.op("vector", lambda e, sl=sl, cg=cg, kc=kc: e.tensor_scalar(
                        out=dst[:, kc, cg * 128:(cg + 1) * 128], in0=sl[:, kc, :], scalar1=scale[:, kc:kc + 1],
                        scalar2=None, op0=ALU.mult), reads=[key, "g1T"], writes=[dkey])
                else:
                    sc.op("scalar", lambda e, sl=sl, cg=cg, kc=kc: e.activation(
                        out=dst[:, kc, cg * 128:(cg + 1) * 128], in_=sl[:, kc, :], func=AF.Copy,
                        scale=scale[:, kc:kc + 1]), reads=[key, "g1T"], writes=[dkey])


def phase_b(nc, sc, G):
    names = ["x_d", "w_in_d", "g1T_d", "w_co_d", "w_ao_d", "w_out_d", "g2bc_d", "lamv_d", "sgbc_d", "wr_d", "rbbc_d",
             "cst", "ident_b", "V", "PS", "PB", "qT_s", "cvT_s", "kT_s", "nhalf", "LG", "x1_s", "h2_s"]
    (x_d, w_in_d, g1T_d, w_co_d, w_ao_d, w_out_d, g2bc_d, lamv_d, sgbc_d, wr_d, rbbc_d,
     cst, ident_b, V, PS, PB, qT_s, cvT_s, kT_s, nhalf, LG, x1_s, h2_s) = (G[k] for k in names)
    nst = G["nst"]
    with contextlib.ExitStack() as st:
        sb = lambda name, shape, dt=F32: st.enter_context(nc.sbuf_tensor("sb_" + name, list(shape), dt))
        KT = sb("b_KT", [128, 4, S], BF16)
        wgt = sb("b_wgt", [128, 8, 2048], BF16)
        wco = sb("b_wco", [128, 4, D], BF16)
        wao = sb("b_wao", [128, 4, D], BF16)
        wout = sb("b_wout", [128, 8, D], BF16)
        wr = sb("b_wr", [128, 8, 36])
        stage_raw = [sb("b_stage%d" % i, [128, 1024]) for i in range(2)]
        stage = [t[:].rearrange("p (k c) -> p k c", k=8) for t in stage_raw]
        g1T = sb("b_g1T", [128, 8])
        g2bc = sb("b_g2bc", [128, D])
        lamv = sb("b_lamv", [128, 4, 64])
        sgbc = sb("b_sgbc", [128, 128])
        rbbc = sb("b_rbbc", [128, 36])
        tri_b = sb("b_tri", [128, 128], BF16)
        lam2 = sb("b_lam2", [128, 4])
        nlam = sb("b_nlam", [128, 1])
        xr = [sb("b_xr%d" % i, [128, D]) for i in range(2)]
        xm = sb("b_xm", [128, 4 * D], BF16)
        xn = xm[:].rearrange("p (j d) -> p j d", j=4)
        mT = xm[:].rearrange("p (k t) -> p k t", k=8)
        hT = sb("b_hT", [128, 8, 512], BF16)
        ss = sb("b_ss", [128, 8])
        rstd = sb("b_rstd", [128, 8])
        qT = sb("b_qT", [128, 4, 512], BF16)
        cvT = sb("b_cvT", [128, 4, 512], BF16)
        Et_t = sb("b_Et", [128, 4, 512], BF16)
        Et = [Et_t[:, i, :] for i in range(4)]
        aoT = sb("b_aoT", [128, 4, 512], BF16)
        Osb = sb("b_Osb", [128, 3, 3, 130])
        rr = sb("b_rr", [128, 4])
        t1 = sb("b_t1", [128, 128])
        ot = sb("b_ot", [128, 128])
        on = sb("b_on", [128, 128], BF16)
        junk = sb("b_junk", [128, 128], BF16)
        sgs = [stage_raw[0][:, 0:512], stage_raw[0][:, 512:1024]]
        tu = [stage_raw[1][:, 0:512], stage_raw[1][:, 512:1024]]
        h2 = sb("b_h2", [128, D])
        h2b = Et_t[:, 0:2, :].rearrange("p a b -> p (a b)")
        h2T = Osb[:].rearrange("p a b c -> p (a b c)")[:, 0:1024].rearrange("p (k t) -> p k t", k=8)

        sc.dma("sync", "b_kt", lambda e: e.dma_start(out=KT[:, :, 0:nst * 512], in_=kT_s.ap()[:, :, 0:nst * 512]),
               reads=["qk_s1_%d" % i for i in range(nst)], writes=["KT"])
        sc.dma("sync", "b_g1T", lambda e: e.dma_start(out=g1T[:], in_=g1T_d.ap()), writes=["g1T"])
        sc.dma("sync", "b_g2bc", lambda e: e.dma_start(out=g2bc[:], in_=g2bc_d.ap()), writes=["g2bc"])
        sc.dma("sync", "b_lamv", lambda e: e.dma_start(out=lamv[:], in_=lamv_d.ap()), writes=["lamv"])
        sc.dma("sync", "b_sgbc", lambda e: e.dma_start(out=sgbc[:], in_=sgbc_d.ap()), writes=["sgbc"])
        sc.dma("sync", "b_rbbc", lambda e: e.dma_start(out=rbbc[:], in_=rbbc_d.ap()), writes=["rbbc"])
        sc.dma("sync", "b_wr", lambda e: e.dma_start(out=wr[:], in_=wr_d.ap().rearrange("(kc p) n -> p kc n", p=128)),
               writes=["wr"])
        w_in_v = w_in_d.ap().rearrange("(kc p) n -> p kc n", p=128)[:, :, 2560:4608]
        load_cast(sc, nc, stage, "b_stage", w_in_v, wgt, 2048, 8, "wgt", scale=g1T)
        load_cast(sc, nc, stage, "b_stage", w_co_d.ap().rearrange("(kc p) n -> p kc n", p=128), wco, D, 4, "wco")
        load_cast(sc, nc, stage, "b_stage", w_ao_d.ap().rearrange("(kc p) n -> p kc n", p=128), wao, D, 4, "wao")
        load_cast(sc, nc, stage, "b_stage", w_out_d.ap().rearrange("(kc p) n -> p kc n", p=128), wout, D, 8, "wout")
        sc.op("vector", lambda e: e.tensor_copy(out=tri_b[:], in_=cst[:, 128:256]), reads=["cst"], writes=["tri_b"])
        sc.op("vector", lambda e: e.tensor_scalar(out=sgbc[:], in0=sgbc[:], scalar1=1.0 - LAM_INIT, scalar2=None,
                                                  op0=ALU.mult), reads=["sgbc"], writes=["sgbc"])
        sc.op("vector", lambda e: e.tensor_tensor(out=lamv[:, 0, :], in0=lamv[:, 0, :], in1=lamv[:, 1, :], op=ALU.mult),
              reads=["lamv"], writes=["lamv"])
        sc.op("vector", lambda e: e.tensor_tensor(out=lamv[:, 2, :], in0=lamv[:, 2, :], in1=lamv[:, 3, :], op=ALU.mult),
              reads=["lamv"], writes=["lamv"])
        sc.op("vector", lambda e: e.reduce_sum(out=lam2[:, 0:1], in_=lamv[:, 0, :], axis=AX.X), reads=["lamv"], writes=["lam2"])
        sc.op("vector", lambda e: e.reduce_sum(out=lam2[:, 1:2], in_=lamv[:, 2, :], axis=AX.X), reads=["lamv", "lam2"],
              writes=["lam2"])
        sc.op("scalar", lambda e: e.activation(out=lam2[:, 2:4], in_=lam2[:, 0:2], func=AF.Exp), reads=["lam2"], writes=["lam2"])
        sc.op("vector", lambda e: e.tensor_tensor(out=nlam[:], in0=lam2[:, 3:4], in1=lam2[:, 2:3], op=ALU.subtract),
              reads=["lam2"], writes=["nlam"])
        sc.op("vector", lambda e: e.tensor_scalar(out=nlam[:], in0=nlam[:], scalar1=-LAM_INIT, scalar2=None, op0=ALU.add),
              reads=["nlam"], writes=["nlam"])

        bstop = G.get("bstop", 99)
        x_v = x_d.ap().rearrange("(t p) d -> t p d", p=128)
        x1_v = x1_s.ap().rearrange("(t p) d -> t p d", p=128)
        h2_v = h2_s.ap().rearrange("(t p) d -> t p d", p=128)
        bank = [0]

        def nb():
            b = bank[0]
            bank[0] = (b + 1) % 6
            return b

        def tile(s):
            def load_q(s2):
                sc.dma("sync", "b_qT", lambda e: e.dma_start(out=qT[:], in_=qT_s.ap()[:, :, s2 * 512:(s2 + 1) * 512]),
                       reads=["qk_s0_%d" % s2], writes=["qT"])

            def load_cv(s2):
                sc.dma("sync", "b_cvT", lambda e: e.dma_start(out=cvT[:], in_=cvT_s.ap()[:, :, s2 * 512:(s2 + 1) * 512]),
                       reads=["cvT_s%d" % s2], writes=["cvT"])
            if s == 0:
                load_q(0)
                load_cv(0)
            for j in range(4):
                xj = xr[j % 2]
                xk = "xr%d" % (j % 2)
                sc.dma("sync", "b_" + xk, lambda e, j=j, xj=xj: e.dma_start(out=xj[:], in_=x_v[s * 4 + j]), writes=[xk])
                sc.op("vector", lambda e, j=j: e.memset(ss[:, j:j + 1], 0.0), writes=["ss%d" % j])
                sc.op("scalar", lambda e, j=j, xj=xj: e.activation(out=xn[:, j, :], in_=xj[:], func=AF.Square,
                                                                   scale=1.0 / 32.0, accum_out=ss[:, j:j + 1]),
                      reads=[xk, "ss%d" % j], writes=["xn%d" % j, "ss%d" % j])
                sc.op("vector", lambda e, j=j: e.tensor_scalar(out=rstd[:, j:j + 1], in0=ss[:, j:j + 1], scalar1=EPS,
                                                               scalar2=None, op0=ALU.add), reads=["ss%d" % j], writes=["rstd%d" % j])
                sc.op("gpsimd", lambda e, j=j: e.tensor_tensor(out=rstd[:, j:j + 1], in0=rstd[:, j:j + 1], in1=nhalf[:, 0:1],
                                                               op=ALU.pow), reads=["rstd%d" % j, "nhalf"], writes=["rstd%d" % j])
                sc.op("vector", lambda e, j=j, xj=xj: e.tensor_scalar(
                    out=xn[:, j, :], in0=xj[:], scalar1=rstd[:, j:j + 1], scalar2=None, op0=ALU.mult),
                    reads=[xk, "rstd%d" % j], writes=["xn%d" % j])
            if bstop < 3:
                return
            ei = [0]
            for h in range(4):
                nkb = 4 * s + 4
                items = [(c, kb) for c in range(2) for kb in range(nkb)]

                def emit_s(idx, h=h):
                    c, kb = items[idx]
                    r0, r1 = c * 64, (c + 1) * 64
                    jmin = max(0, kb - 4 * s)
                    q0 = jmin * 128
                    sb_ = 3 + (idx % 3)
                    sc.op("tensor", lambda e, kb=kb, q0=q0, sb_=sb_, r0=r0, r1=r1, h=h: e.matmul(
                        PS[sb_][:, q0:512], lhsT=KT[r0:r1, h, kb * 128:(kb + 1) * 128], rhs=qT[r0:r1, h, q0:512],
                        start=True, stop=True), reads=["KT", "qT"], writes=["ps%d" % sb_])
                    E = Et[ei[0] % 4]
                    ek = "E%d" % (ei[0] % 4)
                    ei[0] += 1
                    sc.op("scalar", lambda e, E=E, q0=q0, sb_=sb_: e.activation(
                        out=E[:, q0:512], in_=PS[sb_][:, q0:512], func=AF.Exp, scale=0.125),
                        reads=["ps%d" % sb_], writes=[ek])
                    if kb >= 4 * s:
                        sc.op("gpsimd", lambda e, E=E, q0=q0: e.tensor_tensor(
                            out=E[:, q0:q0 + 128], in0=E[:, q0:q0 + 128], in1=tri_b[:], op=ALU.mult),
                            reads=[ek, "tri_b"], writes=[ek])
                    return (E, ek, jmin)

                def emit_av(idx, st_, h=h):
                    c, kb = items[idx]
                    E, ek, jmin = st_

                    def av(e, c=c, kb=kb, jmin=jmin, E=E, h=h, s=s):
                        for j in range(jmin, 4):
                            g = c * 4 + j
                            ob = g // 3
                            o0 = (g % 3) * 160
                            ins = e.matmul(PS[ob][:, o0:o0 + 129], lhsT=E[:, j * 128:(j + 1) * 128],
                                           rhs=V[:, kb, h, 0:129], start=(kb == 0 and g % 3 == 0),
                                           stop=(kb == 4 * s + j), skip_group_check=True)
                        return ins
                    sc.op("tensor", av, reads=[ek, "V%d" % (kb // 4), "Vones"],
                          writes=["ps0", "ps1"] if c == 0 else ["ps1", "ps2"])

                pend = [emit_s(0), emit_s(1)]
                for idx in range(len(items)):
                    if idx + 2 < len(items):
                        pend.append(emit_s(idx + 2))
                    emit_av(idx, pend.pop(0))
                if bstop < 4:
                    continue
                for b in range(3):
                    src = lambda b=b: PS[b][:, 0:480].rearrange("p (r c) -> p r c", r=3)[:, :, 0:129]
                    if b % 2 == 0:
                        sc.op("scalar", lambda e, b=b, src=src: e.copy(out=Osb[:, b, :, 0:129], in_=src()),
                              reads=["ps%d" % b], writes=["Osb%d" % b])
                    else:
                        sc.op("vector", lambda e, b=b, src=src: e.tensor_copy(out=Osb[:, b, :, 0:129], in_=src()),
                              reads=["ps%d" % b], writes=["Osb%d" % b])
                for j in range(4):
                    b0, r_ = j // 3, j % 3
                    b1, r1_ = (4 + j) // 3, (4 + j) % 3
                    sc.op("vector", lambda e, b0=b0, r_=r_: e.reciprocal(out=rr[:, 0:1], in_=Osb[:, b0, r_, 128:129]),
                          reads=["Osb%d" % b0], writes=["rr"])
                    sc.op("vector", lambda e, b1=b1, r1_=r1_: e.reciprocal(out=rr[:, 1:2], in_=Osb[:, b1, r1_, 128:129]),
                          reads=["Osb%d" % b1, "rr"], writes=["rr"])
                    sc.op("vector", lambda e: e.tensor_tensor(out=rr[:, 1:2], in0=rr[:, 1:2], in1=nlam[:], op=ALU.mult),
                          reads=["rr", "nlam"], writes=["rr"])
                    sc.op("vector", lambda e, b0=b0, r_=r_: e.tensor_scalar(
                        out=t1[:], in0=Osb[:, b0, r_, 0:128], scalar1=rr[:, 0:1], scalar2=None, op0=ALU.mult),
                        reads=["Osb%d" % b0, "rr"], writes=["t1"])
                    sc.op("vector", lambda e, b1=b1, r1_=r1_: e.scalar_tensor_tensor(
                        out=ot[:], in0=Osb[:, b1, r1_, 0:128], scalar=rr[:, 1:2], in1=t1[:], op0=ALU.mult, op1=ALU.add),
                        reads=["Osb%d" % b1, "rr", "t1"], writes=["ot"])
                    sc.op("vector", lambda e: e.memset(rr[:, 2:3], 0.0), reads=["rr"], writes=["rr"])
                    sc.op("scalar", lambda e: e.activation(out=junk[:], in_=ot[:], func=AF.Square,
                                                           scale=float(128.0 ** -0.5), accum_out=rr[:, 2:3]),
                          reads=["ot", "rr"], writes=["junk", "rr"])
                    sc.op("vector", lambda e: e.tensor_scalar(out=rr[:, 3:4], in0=rr[:, 2:3], scalar1=EPS, scalar2=None,
                                                              op0=ALU.add), reads=["rr"], writes=["rr"])
                    sc.op("gpsimd", lambda e: e.tensor_tensor(out=rr[:, 3:4], in0=rr[:, 3:4], in1=nhalf[:, 0:1], op=ALU.pow),
                          reads=["rr", "nhalf"], writes=["rr"])
                    sc.op("vector", lambda e: e.scalar_tensor_tensor(
                        out=on[:], in0=ot[:], scalar=rr[:, 3:4], in1=sgbc[:], op0=ALU.mult, op1=ALU.mult),
                        reads=["ot", "rr", "sgbc"], writes=["on"])
                    pb = PB[j % 2]
                    pk = "pb%d" % (j % 2)
                    sc.op("tensor", lambda e, pb=pb: e.transpose(out=pb[:, 0:128], in_=on[:], identity=ident_b[:]),
                          reads=["on", "ident_b"], writes=[pk])
                    sc.op("scalar", lambda e, pb=pb, h=h, j=j: e.copy(out=aoT[:, h, j * 128:(j + 1) * 128], in_=pb[:, 0:128]),
                          reads=[pk], writes=["aoT"])

            if s + 1 < nst:
                load_q(s + 1)
            for j in range(4):
                pb = PB[j % 2]
                pk = "pb%d" % (j % 2)

                def tr(e, j=j, pb=pb):
                    for kc in range(8):
                        ins = e.transpose(out=pb[:, kc * 128:(kc + 1) * 128], in_=xn[:, j, kc * 128:(kc + 1) * 128],
                                          identity=ident_b[:])
                    return ins
                sc.op("tensor", tr, reads=["xn%d" % j, "ident_b"], writes=[pk])
                sc.op("vector", lambda e, j=j, pb=pb: e.tensor_copy(out=hT[:, :, j * 128:(j + 1) * 128],
                                                                   in_=pb[:].rearrange("p (k t) -> p k t", k=8)),
                      reads=[pk], writes=["hT%d" % j])
            hkeys = ["hT%d" % j for j in range(4)]

            if bstop < 5:
                return
            for m in range(8):
                for half in range(2):
                    bG, bP = nb(), nb()
                    wsrc, asrc, akey = (wco, cvT, "cvT") if half == 0 else (wao, aoT, "aoT")

                    def mm(e, m=m, half=half, bG=bG, bP=bP, wsrc=wsrc, asrc=asrc):
                        c0 = half * 1024 + m * 128
                        for kc in range(8):
                            e.matmul(PS[bG][:, :], lhsT=wgt[:, kc, c0:c0 + 128], rhs=hT[:, kc, :],
                                     start=(kc == 0), stop=(kc == 7))
                        for kc in range(4):
                            ins = e.matmul(PS[bP][:, :], lhsT=wsrc[:, kc, m * 128:(m + 1) * 128], rhs=asrc[:, kc, :],
                                           start=(kc == 0), stop=(kc == 3))
                        return ins
                    sc.op("tensor", mm, reads=hkeys + ["wgt", "wco", "wao", akey], writes=["ps%d" % bG, "ps%d" % bP])
                    sc.op("scalar", lambda e, bG=bG, half=half: e.activation(out=sgs[half][:], in_=PS[bG][:, :], func=AF.Sigmoid),
                          reads=["ps%d" % bG], writes=["sgs%d" % half])
                    sc.op("vector", lambda e, bP=bP, half=half: e.tensor_tensor(out=tu[half][:], in0=PS[bP][:, :],
                                                                                in1=sgs[half][:], op=ALU.mult),
                          reads=["ps%d" % bP, "sgs%d" % half], writes=["tu%d" % half])
                sc.op("gpsimd", lambda e, m=m: e.tensor_tensor(out=mT[:, m, :], in0=tu[0][:], in1=tu[1][:], op=ALU.add),
                      reads=["tu0", "tu1"], writes=["mT"])
            if s + 1 < nst:
                load_cv(s + 1)
            if bstop < 6:
                return
            for j in range(4):
                t = s * 4 + j
                xj = xr[j % 2]
                xk = "xr%d" % (j % 2)
                sc.dma("sync", "b_" + xk, lambda e, t=t, xj=xj: e.dma_start(out=xj[:], in_=x_v[t]), writes=[xk])
                for half in range(2):
                    b = nb()

                    def of(e, j=j, half=half, b=b):
                        for kc in range(8):
                            ins = e.matmul(PS[b][:, :], lhsT=mT[:, kc, j * 128:(j + 1) * 128],
                                           rhs=wout[:, kc, half * 512:(half + 1) * 512], start=(kc == 0), stop=(kc == 7))
                        return ins
                    sc.op("tensor", of, reads=["mT", "wout"], writes=["ps%d" % b])
                    sc.op("vector", lambda e, xj=xj, half=half, b=b: e.tensor_tensor(
                        out=xj[:, half * 512:(half + 1) * 512], in0=PS[b][:, :], in1=xj[:, half * 512:(half + 1) * 512],
                        op=ALU.add), reads=["ps%d" % b, xk], writes=[xk])
                sc.dma("sync", "b_x1st%d" % (j % 2), lambda e, t=t, xj=xj: e.dma_start(out=x1_v[t], in_=xj[:]), reads=[xk],
                       writes=["x1_s%d" % t])
                if bstop < 7:
                    continue
                sc.op("vector", lambda e, j=j: e.memset(ss[:, 4 + j:5 + j], 0.0), writes=["ss%d" % (4 + j)])
                sc.op("scalar", lambda e, j=j, xj=xj: e.activation(out=h2b, in_=xj[:], func=AF.Square,
                                                                   scale=1.0 / 32.0, accum_out=ss[:, 4 + j:5 + j]),
                      reads=[xk, "ss%d" % (4 + j)], writes=["h2b", "E0", "E1", "ss%d" % (4 + j)])
                sc.op("vector", lambda e, j=j: e.tensor_scalar(out=rstd[:, 4 + j:5 + j], in0=ss[:, 4 + j:5 + j], scalar1=EPS,
                                                               scalar2=None, op0=ALU.add),
                      reads=["ss%d" % (4 + j)], writes=["rstd%d" % (4 + j)])
                sc.op("gpsimd", lambda e, j=j: e.tensor_tensor(out=rstd[:, 4 + j:5 + j], in0=rstd[:, 4 + j:5 + j],
                                                               in1=nhalf[:, 0:1], op=ALU.pow),
                      reads=["rstd%d" % (4 + j), "nhalf"], writes=["rstd%d" % (4 + j)])
                sc.op("vector", lambda e, j=j, xj=xj: e.scalar_tensor_tensor(
                    out=h2[:], in0=xj[:], scalar=rstd[:, 4 + j:5 + j], in1=g2bc[:], op0=ALU.mult, op1=ALU.mult),
                    reads=[xk, "rstd%d" % (4 + j), "g2bc"], writes=["h2"])
                sc.op("gpsimd", lambda e: e.tensor_copy(out=h2b, in_=h2[:]), reads=["h2"], writes=["h2b", "E0", "E1"])
                sc.dma("sync", "b_h2st", lambda e, t=t: e.dma_start(out=h2_v[t], in_=h2b), reads=["h2b", "E0", "E1"],
                       writes=["h2_s%d" % t])
                if bstop < 7.5:
                    continue
                for half in range(2):
                    b = nb()

                    def trf(e, half=half, b=b):
                        for k4 in range(4):
                            kc = half * 4 + k4
                            ins = e.transpose(out=PS[b][:, k4 * 128:(k4 + 1) * 128], in_=h2[:, kc * 128:(kc + 1) * 128],
                                              identity=cst[:, 0:128])
                        return ins
                    sc.op("tensor", trf, reads=["h2", "cst"], writes=["ps%d" % b])
                    sc.op("scalar" if half else "vector",
                          (lambda e, half=half, b=b: e.copy(out=h2T[:, half * 4:(half + 1) * 4, :],
                                                            in_=PS[b][:, :].rearrange("p (k t) -> p k t", k=4))) if half else
                          (lambda e, half=half, b=b: e.tensor_copy(out=h2T[:, half * 4:(half + 1) * 4, :],
                                                                   in_=PS[b][:, :].rearrange("p (k t) -> p k t", k=4))),
                          reads=["ps%d" % b], writes=["h2T%d" % half])
                if bstop < 7.8:
                    continue
                b = nb()

                def rf(e, b=b):
                    for kc in range(8):
                        ins = e.matmul(PS[b][:, 0:36], lhsT=h2T[:, kc, :], rhs=wr[:, kc, :], start=(kc == 0), stop=(kc == 7))
                    return ins
                sc.op("tensor", rf, reads=["h2T0", "h2T1", "wr"], writes=["ps%d" % b])
                sc.op("vector", lambda e, t=t, b=b: e.tensor_tensor(out=LG[:, t, :], in0=PS[b][:, 0:36], in1=rbbc[:],
                                                                    op=ALU.add),
                      reads=["ps%d" % b, "rbbc"], writes=["LG%d" % t])

        for s in range(nst if bstop >= 2 else 0):
            tile(s)
        for en in sc.ENGS:
            sc.wait_all(en)


def phase_c(nc, sc, G):
    names = ["LG", "dest_i", "wts", "cst", "ident_b", "PS", "PB", "eoff_d", "h2_s", "XS", "YS", "w_g_d", "w_u_d", "w_d_d"]
    LG, dest_i, wts, cst, ident_b, PS, PB, eoff_d, h2_s, XS, YS, w_g_d, w_u_d, w_d_d = (G[k] for k in names)
    ntile = G["nst"] * 4
    NR = NE * CAP
    with contextlib.ExitStack() as st:
        sb = lambda name, shape, dt=F32: st.enter_context(nc.sbuf_tensor("sb_" + name, list(shape), dt))
        xb = [sb("c_xb%d" % i, [128, D], BF16) for i in range(2)]
        U_b = sb("c_Ub", [128, 128], BF16)
        ones_b = sb("c_onesb", [128, 128], BF16)
        eoff = sb("c_eoff", [128, NE])
        with contextlib.ExitStack() as st2:
            sb2 = lambda name, shape, dt=F32: st2.enter_context(nc.sbuf_tensor("sb_" + name, list(shape), dt))
            GLd = sb2("r_GLd", [128, 32, 4])
            ohg = sb2("r_ohg", [128, 32, 4])
            pen = sb2("r_pen", [128, 32, 4])
            sm = sb2("r_sm", [128, 8, 32])
            ELm = sb2("r_ELm", [128, 32, 4, 8])
            oh1 = sb2("r_oh1", [128, 32, 32])
            EL2 = sb2("r_EL2", [128, 32, 32])
            oh2 = sb2("r_oh2", [128, 32, 32])
            Mb = sb2("r_Mb", [128, 32, 32], BF16)
            pre = sb2("r_pre", [128, 32, 32])
            tot = sb2("r_tot", [128, 32, 32])
            base = sb2("r_base", [128, 32, 32])
            prod = sb2("r_prod", [128, 32, 32])
            destf = sb2("r_destf", [128, 2, 32])
            ELm3 = ELm[:].rearrange("p t g j -> p t (g j)")
            gmax, gsum, gw, m1, m2, dm, rsel, esel = (sm[:, i, :] for i in range(8))
            bc4 = lambda a: a.unsqueeze(2).broadcast_to([128, 32, 4])
            bc32 = lambda a: a.unsqueeze(2).broadcast_to([128, 32, 32])
            LK = ["LG%d" % i for i in range(32)]
            V_ = "vector"
            sc.dma("sync", "c_eoff", lambda e: e.dma_start(out=eoff[:], in_=eoff_d.ap()), writes=["eoff"])
            sc.op(V_, lambda e: e.tensor_copy(out=U_b[:], in_=cst[:, 256:384]), reads=["cst"], writes=["U_b"])
            sc.op(V_, lambda e: e.memset(ones_b[:], 1.0), writes=["ones_b"])
            sc.op(V_, lambda e: e.reduce_max(out=gmax, in_=LG[:, :, 0:4], axis=AX.X), reads=LK, writes=["sm"])
            sc.op(V_, lambda e: e.tensor_tensor(out=GLd[:], in0=LG[:, :, 0:4], in1=bc4(gmax), op=ALU.subtract),
                  reads=LK + ["sm"], writes=["GLd"])
            sc.op(V_, lambda e: e.tensor_scalar(out=ohg[:], in0=GLd[:], scalar1=0.0, scalar2=None, op0=ALU.is_equal),
                  reads=["GLd"], writes=["ohg"])
            sc.op("scalar", lambda e: e.activation(out=GLd[:], in_=GLd[:], func=AF.Exp), reads=["GLd", "ohg"], writes=["GLd"])
            sc.op(V_, lambda e: e.reduce_sum(out=gsum, in_=GLd[:], axis=AX.X), reads=["GLd", "sm"], writes=["sm"])
            sc.op(V_, lambda e: e.reciprocal(out=gw, in_=gsum), reads=["sm"], writes=["sm"])
            sc.op(V_, lambda e: e.tensor_scalar(out=pen[:], in0=ohg[:], scalar1=-1.0, scalar2=1e30, op0=ALU.add, op1=ALU.mult),
                  reads=["ohg"], writes=["pen"])
            sc.op(V_, lambda e: e.tensor_tensor(out=ELm[:], in0=LG[:, :, 4:36].rearrange("p t (g j) -> p t g j", g=4),
                                                in1=pen[:].unsqueeze(3).broadcast_to([128, 32, 4, 8]), op=ALU.add),
                  reads=LK + ["pen"], writes=["ELm"])
            sc.op(V_, lambda e: e.reduce_max(out=m1, in_=ELm3, axis=AX.X), reads=["ELm", "sm"], writes=["sm"])
            sc.op(V_, lambda e: e.tensor_tensor(out=oh1[:], in0=ELm3, in1=bc32(m1), op=ALU.is_equal),
                  reads=["ELm", "sm"], writes=["oh1"])
            sc.op(V_, lambda e: e.scalar_tensor_tensor(out=EL2[:], in0=oh1[:], scalar=-1e30, in1=ELm3, op0=ALU.mult,
                                                       op1=ALU.add), reads=["oh1", "ELm"], writes=["EL2"])
            sc.op(V_, lambda e: e.reduce_max(out=m2, in_=EL2[:], axis=AX.X), reads=["EL2", "sm"], writes=["sm"])
            sc.op(V_, lambda e: e.tensor_tensor(out=oh2[:], in0=EL2[:], in1=bc32(m2), op=ALU.is_equal),
                  reads=["EL2", "sm"], writes=["oh2"])
            sc.op(V_, lambda e: e.tensor_tensor(out=dm, in0=m2, in1=m1, op=ALU.subtract), reads=["sm"], writes=["sm"])
            sc.op("scalar", lambda e: e.activation(out=dm, in_=dm, func=AF.Exp), reads=["sm"], writes=["sm"])
            sc.op(V_, lambda e: e.tensor_scalar(out=dm, in0=dm, scalar1=1.0, scalar2=None, op0=ALU.add), reads=["sm"], writes=["sm"])
            sc.op(V_, lambda e: e.reciprocal(out=dm, in_=dm), reads=["sm"], writes=["sm"])
            sc.op(V_, lambda e: e.tensor_tensor(out=wts[:, 0, :], in0=gw, in1=dm, op=ALU.mult), reads=["sm"], writes=["wts"])
            sc.op(V_, lambda e: e.tensor_tensor(out=wts[:, 1, :], in0=gw, in1=wts[:, 0, :], op=ALU.subtract),
                  reads=["sm", "wts"], writes=["wts"])
            sc.op(V_, lambda e: e.tensor_tensor(out=Mb[:], in0=oh1[:], in1=oh2[:], op=ALU.add), reads=["oh1", "oh2"], writes=["Mb"])
            for half in range(2):
                rhs = Mb[:, half * 16:(half + 1) * 16, :].rearrange("p t e -> p (t e)")
                sc.op("tensor", lambda e, half=half, rhs=rhs: e.matmul(PS[half][:, :], lhsT=U_b[:], rhs=rhs, start=True, stop=True),
                      reads=["Mb", "U_b"], writes=["ps%d" % half])
                sc.op("tensor", lambda e, half=half, rhs=rhs: e.matmul(PS[2 + half][:, :], lhsT=ones_b[:], rhs=rhs, start=True,
                                                                       stop=True),
                      reads=["Mb", "ones_b"], writes=["ps%d" % (2 + half)])
                sc.op(V_, lambda e, half=half: e.tensor_copy(
                    out=pre[:, half * 16:(half + 1) * 16, :].rearrange("p t e -> p (t e)"), in_=PS[half][:, :]),
                    reads=["ps%d" % half], writes=["pre"])
                sc.op("scalar", lambda e, half=half: e.copy(
                    out=tot[:, half * 16:(half + 1) * 16, :].rearrange("p t e -> p (t e)"), in_=PS[2 + half][:, :]),
                    reads=["ps%d" % (2 + half)], writes=["tot"])
            sc.op(V_, lambda e: e.memset(base[:, 0, :], 0.0), writes=["base"])
            for t in range(1, 32):
                sc.op(V_, lambda e, t=t: e.tensor_tensor(out=base[:, t, :], in0=base[:, t - 1, :], in1=tot[:, t - 1, :],
                                                         op=ALU.add), reads=["base", "tot"], writes=["base"])
            sc.op(V_, lambda e: e.tensor_tensor(out=pre[:], in0=pre[:], in1=base[:], op=ALU.add), reads=["pre", "base"],
                  writes=["pre"])
            for k, oh in enumerate([oh1, oh2]):
                ohk = "oh%d" % (k + 1)
                sc.op(V_, lambda e, oh=oh: e.tensor_tensor(out=prod[:], in0=oh[:], in1=pre[:], op=ALU.mult),
                      reads=[ohk, "pre"], writes=["prod"])
                sc.op(V_, lambda e: e.reduce_sum(out=rsel, in_=prod[:], axis=AX.X), reads=["prod", "sm"], writes=["sm"])
                sc.op(V_, lambda e, oh=oh: e.tensor_tensor(out=prod[:], in0=oh[:],
                                                           in1=eoff[:].unsqueeze(1).broadcast_to([128, 32, 32]), op=ALU.mult),
                      reads=[ohk, "eoff", "sm"], writes=["prod"])
                sc.op(V_, lambda e: e.reduce_sum(out=esel, in_=prod[:], axis=AX.X), reads=["prod", "sm"], writes=["sm"])
                sc.op(V_, lambda e, k=k: e.tensor_tensor(out=destf[:, k, :], in0=rsel, in1=esel, op=ALU.add),
                      reads=["sm"], writes=["destf"])
                sc.op(V_, lambda e: e.tensor_scalar(out=rsel, in0=rsel, scalar1=float(CAP), scalar2=1e6, op0=ALU.is_ge,
                                                    op1=ALU.mult), reads=["sm", "destf"], writes=["sm"])
                sc.op(V_, lambda e, k=k: e.tensor_tensor(out=destf[:, k, :], in0=destf[:, k, :], in1=rsel, op=ALU.add),
                      reads=["sm", "destf"], writes=["destf"])
            sc.op(V_, lambda e: e.tensor_copy(out=dest_i[:], in_=destf[:]), reads=["destf"], writes=["dest"])
            if G.get("dbg"):
                rt_o = G["rt_o"]
                sc.dma("sync", "dbg3", lambda e: e.dma_start(out=rt_o.ap()[:, 0:2, :], in_=destf[:]), reads=["destf", "dest"],
                       writes=["rt_o"])
                sc.dma("sync", "dbg4", lambda e: e.dma_start(out=rt_o.ap()[:, 2:4, :], in_=wts[:]), reads=["wts"], writes=["rt_o2"])
            for en in sc.ENGS:
                sc.wait_all(en)

        stage_raw = [sb("c_stage%d" % i, [128, 4096]) for i in range(3)]
        wg = [sb("c_wg%d" % i, [128, 8, 512], BF16) for i in range(2)]
        wu = [sb("c_wu%d" % i, [128, 8, 512], BF16) for i in range(2)]
        wd = [sb("c_wd%d" % i, [128, 4, D], BF16) for i in range(2)]
        xb4 = [sb("c_xb4_%d" % i, [128, CAP // 128, D], BF16) for i in range(2)]
        xbT4 = [sb("c_xbT4_%d" % i, [128, 8, CAP], BF16) for i in range(2)]
        sgt2 = [sb("c_sgt%d" % i, [128, 512]) for i in range(2)]
        hidT4 = [sb("c_hid4_%d" % i, [128, 4, CAP], BF16) for i in range(2)]
        yo = [sb("c_yo%d" % i, [128, D]) for i in range(2)]
        h2_v = h2_s.ap().rearrange("(t p) d -> t p d", p=128)
        for t in range(ntile):
            x_ = xb[t % 2]
            xk = "xb%d" % (t % 2)
            sc.dma("sync", "c_" + xk, lambda e, t=t, x_=x_: e.dma_start(out=x_[:], in_=h2_v[t]),
                   reads=["h2_s%d" % t], writes=[xk])
            for k in range(2):
                sc.dma("gpsimd", "c_sc%d_%d" % (t % 2, k), lambda e, t=t, k=k, x_=x_: e.indirect_dma_start(
                    out=XS.ap(), out_offset=bass.IndirectOffsetOnAxis(ap=dest_i[:, k, t:t + 1], axis=0),
                    in_=x_[:], in_offset=None, bounds_check=G["bnd"]["r"], oob_is_err=False),
                    reads=[xk, "dest", "XS"], writes=["XSk%d" % k])

        nexp = G.get("nexp", NE)
        sg_v = stage_raw[0][:].rearrange("p (k n) -> p k n", k=8)
        su_v = stage_raw[1][:].rearrange("p (k n) -> p k n", k=8)
        sd_v = stage_raw[2][:].rearrange("p (k n) -> p k n", k=4)

        def load_w(ex):
            sc.dma("sync", "c_st0", lambda e: e.dma_start(out=sg_v, in_=w_g_d.ap()[ex].rearrange("(kc p) n -> p kc n", p=128)),
                   writes=["st0"])
            sc.dma("sync", "c_st1", lambda e: e.dma_start(out=su_v, in_=w_u_d.ap()[ex].rearrange("(kc p) n -> p kc n", p=128)),
                   writes=["st1"])
            sc.dma("sync", "c_st2", lambda e: e.dma_start(out=sd_v, in_=w_d_d.ap()[ex].rearrange("(kc p) n -> p kc n", p=128)),
                   writes=["st2"])

        def cast_w(ex):
            p2 = ex % 2
            sc.op("scalar", lambda e: e.copy(out=wg[p2][:], in_=sg_v), reads=["st0"], writes=["wg%d" % p2])
            sc.op("vector", lambda e: e.tensor_copy(out=wu[p2][:], in_=su_v), reads=["st1"], writes=["wu%d" % p2])
            sc.op("vector", lambda e: e.tensor_copy(out=wd[p2][:], in_=sd_v), reads=["st2"], writes=["wd%d" % p2])

        def load_xb(ex):
            p2 = ex % 2
            sc.dma("sync", "c_xb4_%d" % p2, lambda e: e.dma_start(
                out=xb4[p2][:], in_=XS.ap()[ex * CAP:(ex + 1) * CAP, :].rearrange("(r p) d -> p r d", p=128)),
                reads=["XS", "XSk0", "XSk1"], writes=["xb4_%d" % p2])

        load_xb(0)
        load_w(0)
        for ex in range(nexp):
            p2 = ex % 2
            cast_w(ex)
            if ex + 1 < nexp:
                load_xb(ex + 1)
                load_w(ex + 1)
            NB = CAP // 128
            for r in range(NB):
                pb = PB[r % 2]
                pk = "pb%d" % (r % 2)

                def tr(e, r=r, pb=pb, p2=p2):
                    for kc in range(8):
                        ins = e.transpose(out=pb[:, kc * 128:(kc + 1) * 128], in_=xb4[p2][:, r, kc * 128:(kc + 1) * 128],
                                          identity=ident_b[:])
                    return ins
                sc.op("tensor", tr, reads=["xb4_%d" % p2, "ident_b"], writes=[pk])
                sc.op("vector" if r % 2 else "scalar",
                      (lambda e, r=r, pb=pb, p2=p2: e.tensor_copy(out=xbT4[p2][:, :, r * 128:(r + 1) * 128],
                                                                 in_=pb[:].rearrange("p (k t) -> p k t", k=8))) if r % 2 else
                      (lambda e, r=r, pb=pb, p2=p2: e.copy(out=xbT4[p2][:, :, r * 128:(r + 1) * 128],
                                                           in_=pb[:].rearrange("p (k t) -> p k t", k=8))),
                      reads=[pk], writes=["xbT4_%d_%d" % (p2, r)])
            xkeys = ["xbT4_%d_%d" % (p2, r) for r in range(NB)]
            for f in range(4):
                bg, bu = (0, 1) if f % 2 == 0 else (2, 3)

                def gu(e, p2=p2, f=f, bg=bg, bu=bu):
                    for (bb, w) in ((bg, wg[p2]), (bu, wu[p2])):
                        for kc in range(8):
                            ins = e.matmul(PS[bb][:, 0:CAP], lhsT=w[:, kc, f * 128:(f + 1) * 128], rhs=xbT4[p2][:, kc, :],
                                           start=(kc == 0), stop=(kc == 7))
                    return ins
                sc.op("tensor", gu, reads=xkeys + ["wg%d" % p2, "wu%d" % p2], writes=["ps%d" % bg, "ps%d" % bu])
                sg_ = sgt2[f % 2]
                sc.op("scalar", lambda e, bg=bg, sg_=sg_: e.activation(out=sg_[:, 0:CAP], in_=PS[bg][:, 0:CAP], func=AF.Silu),
                      reads=["ps%d" % bg], writes=["sgt%d" % (f % 2)])
                sc.op("vector", lambda e, bu=bu, sg_=sg_, f=f, p2=p2: e.tensor_tensor(
                    out=hidT4[p2][:, f, :], in0=PS[bu][:, 0:CAP], in1=sg_[:, 0:CAP], op=ALU.mult),
                    reads=["ps%d" % bu, "sgt%d" % (f % 2)], writes=["hid4_%d" % p2])
            for r in range(NB):
                row0 = ex * CAP + r * 128
                i2 = r % 2
                for half in range(2):
                    bd = 4 + half

                    def dn(e, half=half, bd=bd, r=r, p2=p2):
                        for fc in range(4):
                            ins = e.matmul(PS[bd][:, :], lhsT=hidT4[p2][:, fc, r * 128:(r + 1) * 128],
                                           rhs=wd[p2][:, fc, half * 512:(half + 1) * 512], start=(fc == 0), stop=(fc == 3))
                        return ins
                    sc.op("tensor", dn, reads=["hid4_%d" % p2, "wd%d" % p2], writes=["ps%d" % bd])
                    if half == 0:
                        sc.op("scalar", lambda e, i2=i2, bd=bd: e.copy(out=yo[i2][:, 0:512], in_=PS[bd][:, :]),
                              reads=["ps%d" % bd], writes=["yo%d" % i2])
                    else:
                        sc.op("vector", lambda e, i2=i2, bd=bd: e.tensor_copy(out=yo[i2][:, 512:1024], in_=PS[bd][:, :]),
                              reads=["ps%d" % bd], writes=["yo%d" % i2])
                sc.dma("sync", "c_yo%d" % i2, lambda e, row0=row0, i2=i2: e.dma_start(out=YS.ap()[row0:row0 + 128, :], in_=yo[i2][:]),
                       reads=["yo%d" % i2], writes=["YS"])
        for en in sc.ENGS:
            sc.wait_all(en)


def phase_d(nc, sc, G):
    names = ["dest_i", "wts", "x1_s", "YS", "out_d"]
    dest_i, wts, x1_s, YS, out_d = (G[k] for k in names)
    ntile = G["nst"] * 4
    NBUF = 4
    with contextlib.ExitStack() as st:
        sb = lambda name, shape, dt=F32: st.enter_context(nc.sbuf_tensor("sb_" + name, list(shape), dt))
        x1t = [sb("d_x1%d" % i, [128, D]) for i in range(NBUF)]
        y1 = [sb("d_y1%d" % i, [128, D]) for i in range(NBUF)]
        y2 = [sb("d_y2%d" % i, [128, D]) for i in range(NBUF)]
        x1_v = x1_s.ap().rearrange("(t p) d -> t p d", p=128)
        out_v = out_d.ap().rearrange("(t p) d -> t p d", p=128)

        def prep(t):
            i2 = t % NBUF
            sc.dma("sync", "d_x1%d" % i2, lambda e: e.dma_start(out=x1t[i2][:], in_=x1_v[t]),
                   reads=["x1_s%d" % t], writes=["dx1%d" % i2])
            for k, yt in enumerate((y1, y2)):
                yk = "dy%d_%d" % (k, i2)
                sc.op("vector", lambda e, yt=yt: e.memset(yt[i2][:], 0.0), writes=[yk])
                sc.dma("gpsimd", "d_g%d_%d" % (k, i2), lambda e, yt=yt, k=k: e.indirect_dma_start(
                    out=yt[i2][:], out_offset=None, in_=YS.ap(),
                    in_offset=bass.IndirectOffsetOnAxis(ap=dest_i[:, k, t:t + 1], axis=0),
                    bounds_check=G["bnd"]["r"], oob_is_err=False), reads=["YS", "dest"], writes=[yk])

        def finish(t):
            i2 = t % NBUF
            sc.op("vector", lambda e: e.scalar_tensor_tensor(
                out=x1t[i2][:], in0=y1[i2][:], scalar=wts[:, 0, t:t + 1], in1=x1t[i2][:], op0=ALU.mult, op1=ALU.add),
                reads=["dy0_%d" % i2, "wts", "dx1%d" % i2], writes=["dx1%d" % i2])
            sc.op("vector", lambda e: e.scalar_tensor_tensor(
                out=x1t[i2][:], in0=y2[i2][:], scalar=wts[:, 1, t:t + 1], in1=x1t[i2][:], op0=ALU.mult, op1=ALU.add),
                reads=["dy1_%d" % i2, "wts", "dx1%d" % i2], writes=["dx1%d" % i2])
            sc.dma("sync", "d_o%d" % i2, lambda e: e.dma_start(out=out_v[t], in_=x1t[i2][:]),
                   reads=["dx1%d" % i2], writes=["out%d" % t])

        AHEAD = 2
        for t in range(min(AHEAD, ntile)):
            prep(t)
        for t in range(ntile):
            if t + AHEAD < ntile:
                prep(t + AHEAD)
            finish(t)
        for en in sc.ENGS:
            sc.wait_all(en)


def _consts():
    c = np.zeros((128, 512), np.float32)
    c[:, 0:128] = np.eye(128, dtype=np.float32)
    k = np.arange(128)[:, None]
    q = np.arange(128)[None, :]
    c[:, 128:256] = (k <= q).astype(np.float32)
    c[:, 256:384] = (k < q).astype(np.float32)
    c[:, 384:512] = ((k // 64) == (q // 64)).astype(np.float32)
    return c


def make_in_maps(inputs, cores):
    f = lambda a: np.ascontiguousarray(np.asarray(a, dtype=np.float32))
    w_in = f(inputs["w_in"][0])
    g1T = f(inputs["attn_norm_g"][0].reshape(8, 128).T)
    cw = f(inputs["conv_dw_w"][0].reshape(31, 4, 128).transpose(2, 1, 0))
    cvec = f(np.stack([inputs["conv_dw_b"][0].reshape(4, 128).T, inputs["conv_ln_g"][0].reshape(4, 128).T,
                       inputs["conv_ln_b"][0].reshape(4, 128).T], axis=-1))
    qkg = f(np.stack([np.tile(inputs["q_norm_g"][0], 2), np.tile(inputs["k_norm_g"][0], 2)], axis=-1))
    cst = _consts()
    rep = lambda v: f(np.broadcast_to(np.asarray(v, np.float32)[None], (128,) + tuple(np.shape(v))))
    shared = {
        "w_in": w_in, "cst": cst, "g1T": g1T, "cw": cw, "cvec": cvec, "qkg": qkg,
        "w_co": f(inputs["w_conv_out"][0]), "w_ao": f(inputs["w_attn_out"][0]), "w_out": f(inputs["w_out"][0]),
        "g2bc": rep(inputs["ffn_norm_g"][0]),
        "lamv": rep(np.stack([inputs["lambda_q1"][0], inputs["lambda_k1"][0], inputs["lambda_q2"][0], inputs["lambda_k2"][0]])),
        "sgbc": rep(inputs["subln_g"][0]),
        "wr": f(np.concatenate([inputs["w_router_group"][0], inputs["w_router_expert"][0]], axis=1)),
        "rbbc": rep(np.concatenate([inputs["b_router_group"][0], inputs["b_router_expert"][0]])),
        "eoff": rep(np.arange(NE, dtype=np.float32) * CAP),
        "w_g": f(inputs["w_gate_e"][0]), "w_u": f(inputs["w_up_e"][0]), "w_d": f(inputs["w_down_e"][0]),
    }
    maps = []
    for c in cores:
        m = dict(shared)
        m["x"] = f(inputs["x"][c])
        maps.append(m)
    return maps


def kernel(**inputs):
    nc = build_program()
    maps = make_in_maps(inputs, list(range(8)))
    res = run_bass_kernel_spmd(nc, maps, core_ids=list(range(8)))
    return np.stack([np.asarray(r["out"]).reshape(S, D) for r in res.results], axis=0).astype(np.float32)
```

```python
import contextlib
import numpy as np
import concourse.bass as bass
import concourse.mybir as mybir
from concourse.bass_utils import run_bass_kernel_spmd

F32 = mybir.dt.float32
BF16 = mybir.dt.bfloat16
I32 = mybir.dt.int32
ALU = mybir.AluOpType
AF = mybir.ActivationFunctionType
AX = mybir.AxisListType

S = 4096
D = 1024
NST = 8
EPS = 1e-6
LAM_INIT = 0.8 - 0.6 * 1.0
CAP = 512
NE = 32


class Sched:
    ENGS = ["sync", "scalar", "vector", "gpsimd", "tensor"]

    def __init__(self, nc, stack, n_dma_sems=80):
        self.nc = nc
        self.streams = {e: [] for e in self.ENGS}
        self.sems = {}
        self.count = {}
        for e in self.ENGS:
            self.sems["e:" + e] = stack.enter_context(nc.semaphore("sem_" + e))
            self.count["e:" + e] = 0
        self.free_dma = [stack.enter_context(nc.semaphore("dsem%d" % i)) for i in range(n_dma_sems)]
        self.waited = {e: {} for e in self.ENGS}
        self.last_w = {}
        self.readers = {}

    def _dsem(self, key):
        k = "d:" + key
        if k not in self.sems:
            self.sems[k] = self.free_dma.pop()
            self.count[k] = 0
        return k

    def _deps(self, reads, writes):
        toks = []
        for k in reads:
            t = self.last_w.get(k)
            if t is not None:
                toks.append(t)
        for k in writes:
            t = self.last_w.get(k)
            if t is not None:
                toks.append(t)
            for sk, v in self.readers.get(k, {}).items():
                toks.append((sk, v))
        return toks

    def _wait(self, eng, toks):
        w = self.waited[eng]
        need = {}
        for sk, v in toks:
            if w.get(sk, 0) >= v:
                continue
            if eng == "tensor" and sk == "e:tensor":
                continue
            if need.get(sk, 0) < v:
                need[sk] = v
        for sk, v in need.items():
            w[sk] = v
            sem = self.sems[sk]
            self.streams[eng].append(lambda e, sem=sem, v=v: e.wait_ge(sem, v))

    def _commit(self, tok, reads, writes):
        sk, v = tok
        for k in reads:
            r = self.readers.setdefault(k, {})
            if r.get(sk, 0) < v:
                r[sk] = v
        for k in writes:
            self.last_w[k] = tok
            self.readers[k] = {}

    def op(self, eng, fn, reads=(), writes=()):
        self._wait(eng, self._deps(reads, writes))
        sk = "e:" + eng
        self.count[sk] += 1
        n = self.count[sk]
        sem = self.sems[sk]
        self.streams[eng].append(lambda e, fn=fn, sem=sem: fn(e).then_inc(sem, 1))
        tok = (sk, n)
        self._commit(tok, reads, writes)
        return tok

    def dma(self, q, semkey, fn, reads=(), writes=()):
        self._wait(q, self._deps(reads, writes))
        sk = self._dsem(semkey)
        self.count[sk] += 16
        n = self.count[sk]
        sem = self.sems[sk]
        self.streams[q].append(lambda e, fn=fn, sem=sem: fn(e).then_inc(sem, 16))
        tok = (sk, n)
        self._commit(tok, reads, writes)
        return tok

    def wait_all(self, eng):
        toks = []
        for k, t in self.last_w.items():
            toks.append(t)
        for k, r in self.readers.items():
            for sk, v in r.items():
                toks.append((sk, v))
        self._wait(eng, toks)

    def flush(self):
        with self.nc.Block() as block:
            for name in self.ENGS:
                fns = self.streams[name]
                if not fns:
                    continue

                def body(e, fns=fns):
                    for f in fns:
                        f(e)
                getattr(block, name)(body)
        self.streams = {e: [] for e in self.ENGS}


def build_program(dbg=False, phases=("A", "B", "C", "D"), nst=NST, stop=99, nexp=NE, bstop=99):
    nc = bass.Bass("TRN2", target_bir_lowering=False)
    okind = "ExternalOutput" if dbg else "Internal"

    def din(name, shape, dt=F32):
        return nc.dram_tensor(name, list(shape), dt, kind="ExternalInput")

    x_d = din("x", [S, D])
    w_in_d = din("w_in", [D, 4608])
    cst_d = din("cst", [128, 128 * 4])
    g1T_d = din("g1T", [128, 8])
    cw_d = din("cw", [128, 4, 31])
    cvec_d = din("cvec", [128, 4, 3])
    qkg_d = din("qkg", [128, 2])
    w_co_d = din("w_co", [512, D])
    w_ao_d = din("w_ao", [512, D])
    w_out_d = din("w_out", [D, D])
    g2bc_d = din("g2bc", [128, D])
    lamv_d = din("lamv", [128, 4, 64])
    sgbc_d = din("sgbc", [128, 128])
    wr_d = din("wr", [D, 36])
    rbbc_d = din("rbbc", [128, 36])
    eoff_d = din("eoff", [128, NE])
    w_g_d = din("w_g", [NE, D, 512])
    w_u_d = din("w_u", [NE, D, 512])
    w_d_d = din("w_d", [NE, 512, D])
    out_d = nc.dram_tensor("out", [S, D], F32, kind="ExternalOutput")
    x1_s = nc.dram_tensor("x1_s", [S, D], F32, kind=okind)
    h2_s = nc.dram_tensor("h2_s", [S, D], BF16, kind=okind)
    XS = nc.dram_tensor("XS", [NE * CAP, D], BF16, kind="Internal")
    YS = nc.dram_tensor("YS", [NE * CAP, D], F32, kind="Internal")
    if dbg:
        lg_o = nc.dram_tensor("lg_o", [128, 32, 36], F32, kind="ExternalOutput")
        rt_o = nc.dram_tensor("rt_o", [128, 4, 32], F32, kind="ExternalOutput")
    qT_s = nc.dram_tensor("qT_s", [128, 4, S], BF16, kind=okind)
    cvT_s = nc.dram_tensor("cvT_s", [128, 4, S], BF16, kind=okind)
    kT_s = nc.dram_tensor("kT_s", [128, 4, S], BF16, kind=okind)
    if dbg:
        v_o = nc.dram_tensor("v_o", [128, 32, 4, 130], BF16, kind="ExternalOutput")

    with contextlib.ExitStack() as st:
        sc = Sched(nc, st)
        sb = lambda name, shape, dt=F32: st.enter_context(nc.sbuf_tensor("sb_" + name, list(shape), dt))
        cst = sb("cst", [128, 512])
        ident_b = sb("ident_b", [128, 128], BF16)
        V = sb("V", [128, 32, 4, 130], BF16)
        PS = [st.enter_context(nc.psum_tensor("ps%d" % i, [128, 512], F32)) for i in range(6)]
        PB = [st.enter_context(nc.psum_tensor("pb%d" % i, [128, 1024], BF16)) for i in range(2)]
        nhalf = sb("nhalf", [128, 512])
        bnd = {}

        def _mk_bnd(e):
            bnd["r"] = e.alloc_register("bnd")
            e.reg_mov(bnd["r"], NE * CAP - 1)
        sc.streams["gpsimd"].append(_mk_bnd)
        LG = sb("LG", [128, 32, 36])
        dest_i = sb("dest_i", [128, 2, 32], I32)
        wts = sb("wts", [128, 2, 32])
        if dbg:
            sc.op("vector", lambda e: e.memset(LG[:], 0.0), writes=["LG%d" % i for i in range(32)])
        zeros_b = sb("zeros_b", [128, D], BF16)
        sc.op("vector", lambda e: e.memset(zeros_b[:], 0.0), writes=["zeros_b"])
        XS_v = XS.ap().rearrange("(r p) d -> r p d", p=128)
        xs_next = [0]

        def xs_zero(n):
            for _ in range(n):
                r = xs_next[0]
                if r >= NE * CAP // 128:
                    return
                xs_next[0] += 1
                sc.dma("sync", "xs_zero", lambda e, r=r: e.dma_start(out=XS_v[r], in_=zeros_b[:]),
                       reads=["zeros_b"], writes=["XS"])
        sc.op("gpsimd", lambda e: e.memset(nhalf[:], -0.5), writes=["nhalf"])

        sc.dma("sync", "cst", lambda e: e.dma_start(out=cst[:], in_=cst_d.ap()), writes=["cst"])
        sc.op("vector", lambda e: e.tensor_copy(out=ident_b[:], in_=cst[:, 0:128]), reads=["cst"], writes=["ident_b"])
        sc.op("gpsimd", lambda e: e.memset(V[:], 1.0), writes=["Vones"] + ["V%d" % i for i in range(NST)])

        if "A" in phases:
            phase_a(nc, sc, locals())
        if "B" in phases:
            phase_b(nc, sc, locals())
        if dbg and "B" in phases:
            sc.dma("sync", "dbg2", lambda e: e.dma_start(out=lg_o.ap(), in_=LG[:]),
                   reads=["LG%d" % i for i in range(32)], writes=["lg_o"])
        if "C" in phases:
            phase_c(nc, sc, locals())
        if "D" in phases:
            phase_d(nc, sc, locals())

        if dbg:
            sc.dma("sync", "dbg", lambda e: e.dma_start(out=v_o.ap(), in_=V[:]),
                   reads=["V%d" % i for i in range(NST)] + ["Vones"], writes=["v_o"])
        sc.wait_all("sync")
        sc.flush()
    return nc


def phase_a(nc, sc, G):
    x_d, w_in_d, g1T_d, cw_d, cvec_d, qkg_d = (G[k] for k in ["x_d", "w_in_d", "g1T_d", "cw_d", "cvec_d", "qkg_d"])
    cst, ident_b, V, PS, PB, qT_s, cvT_s, kT_s, nhalf = (G[k] for k in ["cst", "ident_b", "V", "PS", "PB", "qT_s", "cvT_s", "kT_s", "nhalf"])
    with contextlib.ExitStack() as st:
        sb = lambda name, shape, dt=F32: st.enter_context(nc.sbuf_tensor("sb_" + name, list(shape), dt))
        NCA = 2560
        w_bf = sb("a_wbf", [128, 8, NCA], BF16)
        stage = [sb("a_stage%d" % i, [128, 8, 128]) for i in range(2)]
        g1T = sb("a_g1T", [128, 8])
        cw = sb("a_cw", [128, 4, 31])
        cvec = sb("a_cvec", [128, 4, 3])
        qkg = sb("a_qkg", [128, 2])
        diag = sb("a_diag", [128, 4, 31, 128], BF16)
        onesN = sb("a_onesN", [128, 128], BF16)
        blk = sb("a_blk", [128, 128], BF16)
        xt = [sb("a_xt0", [128, 4, D])] * 2
        ss = sb("a_ss", [128, 8])
        rstd = sb("a_rstd", [128, 8])
        xn = sb("a_xn", [128, 4, D], BF16)
        hT = sb("a_hT", [128, 8, 512], BF16)
        ybuf = [sb("a_ybuf%d" % i, [128, 4, 544], BF16) for i in range(2)]
        ybuf1 = [sb("a_ybuf1_%d" % i, [128, 4, 544], BF16) for i in range(2)]
        sg = [sb("a_sg0", [128, 512])] * 2
        ycb = sb("a_ycb", [128, 4, 512], BF16)
        ysq = sb("a_ysq", [128, 4, 512], BF16)
        mean_sb = sb("a_mean", [128, 512])
        lrstd = sb("a_lrstd", [128, 512])
        zt = [sb("a_zt0", [128, 512])] * 2
        cvT = sb("a_cvT", [128, 4, 512], BF16)
        sq = [sb("a_sq%d" % i, [128, 512], BF16) for i in range(2)]
        qr = [sb("a_qr%d" % i, [128, 512]) for i in range(2)]
        qkT = [sb("a_qT", [128, 4, 512], BF16), sb("a_kT", [128, 4, 512], BF16)]

        sc.dma("sync", "g1T", lambda e: e.dma_start(out=g1T[:], in_=g1T_d.ap()), writes=["g1T"])
        sc.dma("sync", "cw", lambda e: e.dma_start(out=cw[:], in_=cw_d.ap()), writes=["cw"])
        sc.dma("sync", "cvec", lambda e: e.dma_start(out=cvec[:], in_=cvec_d.ap()), writes=["cvec"])
        sc.dma("sync", "qkg", lambda e: e.dma_start(out=qkg[:], in_=qkg_d.ap()), writes=["qkg"])
        w_in_v = w_in_d.ap().rearrange("(kc p) n -> p kc n", p=128)
        for cg in range(NCA // 128):
            sl = stage[cg % 2]
            key = "stage%d" % (cg % 2)
            sc.dma("sync", key, lambda e, sl=sl, cg=cg: e.dma_start(out=sl[:], in_=w_in_v[:, :, cg * 128:(cg + 1) * 128]),
                   writes=[key])
            for kc in range(8):
                if kc % 2 == 0:
                    sc.op("vector", lambda e, sl=sl, cg=cg, kc=kc: e.tensor_scalar(
                        out=w_bf[:, kc, cg * 128:(cg + 1) * 128], in0=sl[:, kc, :], scalar1=g1T[:, kc:kc + 1],
                        scalar2=None, op0=ALU.mult), reads=[key, "g1T"], writes=["wbf%d_%d" % (cg // 4, kc)])
                else:
                    sc.op("scalar", lambda e, sl=sl, cg=cg, kc=kc: e.activation(
                        out=w_bf[:, kc, cg * 128:(cg + 1) * 128], in_=sl[:, kc, :], func=AF.Copy,
                        scale=g1T[:, kc:kc + 1]), reads=[key, "g1T"], writes=["wbf%d_%d" % (cg // 4, kc)])
        wkeys = lambda cg: ["wbf%d_%d" % (cg, kc) for kc in range(8)]
        for ch in range(4):
            for k in range(31):
                if k % 2 == 0:
                    sc.op("vector", lambda e, ch=ch, k=k: e.tensor_scalar(
                        out=diag[:, ch, k, :], in0=cst[:, 0:128], scalar1=cw[:, ch, k:k + 1], scalar2=None,
                        op0=ALU.mult), reads=["cst", "cw"], writes=["diag"])
                else:
                    sc.op("scalar", lambda e, ch=ch, k=k: e.activation(
                        out=diag[:, ch, k, :], in_=cst[:, 0:128], func=AF.Copy, scale=cw[:, ch, k:k + 1]),
                        reads=["cst", "cw"], writes=["diag"])
        sc.op("vector", lambda e: e.memset(onesN[:], 1.0 / 512.0), writes=["onesN"])
        sc.op("vector", lambda e: e.tensor_scalar(out=blk[:], in0=cst[:, 384:512], scalar1=1.0 / 64.0, scalar2=None,
                                                  op0=ALU.mult), reads=["cst"], writes=["blk"])
        for i in range(2):
            sc.op("gpsimd", lambda e, i=i: e.memset(ybuf[i][:], 0.0), writes=["ybuf%d" % i])
            sc.op("gpsimd", lambda e, i=i: e.memset(ybuf1[i][:], 0.0), writes=["ybuf%d" % i])

        x_v = x_d.ap().rearrange("(s j p) d -> s p j d", j=4, p=128)
        bank = [0]

        def nb():
            b = bank[0]
            bank[0] = (b + 1) % 6
            return b

        def load_x(s):
            sc.dma("sync", "xt0", lambda e, s=s: e.dma_start(out=xt[0][:], in_=x_v[s]), writes=["xt0"])

        def tile(s):
            stop = G.get("stop", 99)
            if s == 0:
                load_x(0)
            xs = xt[0]
            xk = "xt0"
            sc.op("vector", lambda e: e.memset(ss[:, 0:4], 0.0), writes=["ss"])
            for j in range(4):
                sc.op("scalar", lambda e, j=j: e.activation(out=xn[:, j, :], in_=xs[:, j, :], func=AF.Square,
                                                            scale=1.0 / 32.0, accum_out=ss[:, j:j + 1]),
                      reads=[xk, "ss"], writes=["xn%d" % j, "ss"])
            sc.op("vector", lambda e: e.tensor_scalar(out=rstd[:, 0:4], in0=ss[:, 0:4], scalar1=EPS, scalar2=None,
                                                      op0=ALU.add), reads=["ss"], writes=["rstd"])
            sc.op("gpsimd", lambda e: e.tensor_tensor(out=rstd[:, 0:4], in0=rstd[:, 0:4], in1=nhalf[:, 0:4], op=ALU.pow),
                  reads=["rstd", "nhalf"], writes=["rstd"])
            for j in range(4):
                sc.op("vector", lambda e, j=j: e.tensor_scalar(
                    out=xn[:, j, :], in0=xs[:, j, :], scalar1=rstd[:, j:j + 1], scalar2=None, op0=ALU.mult),
                    reads=[xk, "rstd"], writes=["xn%d" % j])
            if s + 1 < G["nst"]:
                load_x(s + 1)
            G["xs_zero"](16)
            if stop < 2:
                return
            for j in range(4):
                pb = PB[j % 2]
                pk = "pb%d" % (j % 2)

                def tr(e, j=j, pb=pb):
                    for kc in range(8):
                        ins = e.transpose(out=pb[:, kc * 128:(kc + 1) * 128], in_=xn[:, j, kc * 128:(kc + 1) * 128],
                                          identity=ident_b[:])
                    return ins
                sc.op("tensor", tr, reads=["xn%d" % j, "ident_b"], writes=[pk])
                sc.op("vector" if j % 2 else "scalar",
                      (lambda e, j=j, pb=pb: e.tensor_copy(out=hT[:, :, j * 128:(j + 1) * 128],
                                                           in_=pb[:].rearrange("p (k t) -> p k t", k=8))) if j % 2 else
                      (lambda e, j=j, pb=pb: e.copy(out=hT[:, :, j * 128:(j + 1) * 128],
                                                    in_=pb[:].rearrange("p (k t) -> p k t", k=8))),
                      reads=[pk], writes=["hT%d" % j])
            hkeys = ["hT%d" % j for j in range(4)]

            def fm_matmul(m):
                b = nb()

                def f(e, m=m, b=b):
                    for kc in range(8):
                        ins = e.matmul(PS[b][:, :], lhsT=w_bf[:, kc, m * 128:(m + 1) * 128], rhs=hT[:, kc, :],
                                       start=(kc == 0), stop=(kc == 7))
                    return ins
                sc.op("tensor", f, reads=hkeys + wkeys(m // 4), writes=["ps%d" % b])
                return b

            if stop < 2.1:
                return
            yb = ybuf[s % 2]
            ybk = "ybuf%d" % (s % 2)
            ybo = ybuf[(s + 1) % 2]
            yb1 = ybuf1[s % 2]
            ybo1 = ybuf1[(s + 1) % 2]
            ybok = "ybuf%d" % ((s + 1) % 2)
            for ch in range(4):
                bg = fm_matmul(4 + ch)
                ba = fm_matmul(ch)
                sgt = sg[ch % 2]
                if stop < 2.3:
                    continue
                sc.op("scalar", lambda e, bg=bg, sgt=sgt: e.activation(out=sgt[:], in_=PS[bg][:, :], func=AF.Sigmoid),
                      reads=["ps%d" % bg], writes=["sg0"])
                if stop < 2.6:
                    continue
                if s > 0:
                    sc.op("gpsimd", lambda e, ch=ch: e.tensor_copy(out=yb[:, ch, 0:30], in_=ybo[:, ch, 512:542]),
                          reads=[ybok + "_%d" % ch], writes=[ybk + "_%d" % ch])
                    sc.op("gpsimd", lambda e, ch=ch: e.tensor_copy(out=yb1[:, ch, 0:29], in_=ybo1[:, ch, 512:541]),
                          reads=[ybok + "_%d" % ch], writes=[ybk + "_%d" % ch])
                sc.op("vector", lambda e, ba=ba, sgt=sgt, ch=ch: e.tensor_tensor(
                    out=yb[:, ch, 30:542], in0=PS[ba][:, :], in1=sgt[:], op=ALU.mult),
                    reads=["ps%d" % ba, "sg0", ybk], writes=[ybk + "_%d" % ch])
                sc.op("gpsimd", lambda e, ch=ch: e.tensor_copy(out=yb1[:, ch, 29:541], in_=yb[:, ch, 30:542]),
                      reads=[ybk + "_%d" % ch], writes=[ybk + "_%d" % ch])
            if stop < 3.5:
                return
            for ch in range(4):
                b = nb()

                def cf(e, ch=ch, b=b):
                    for k in (range(31) if stop != 3.6 else [0, 16, 30]):
                        rhs = yb[:, ch, k:k + 512] if k % 2 == 0 else yb1[:, ch, k - 1:k - 1 + 512]
                        ins = e.matmul(PS[b][:, :], lhsT=diag[:, ch, k, :], rhs=rhs, start=(k == 0), stop=(k == 30))
                    return ins
                sc.op("tensor", cf, reads=[ybk + "_%d" % ch, "diag", ybk], writes=["ps%d" % b])
                if stop < 3.8:
                    continue
                sc.op("vector", lambda e, ch=ch, b=b: e.tensor_scalar(
                    out=ycb[:, ch, :], in0=PS[b][:, :], scalar1=cvec[:, ch, 0:1], scalar2=None, op0=ALU.add),
                    reads=["ps%d" % b, "cvec"], writes=["ycb%d" % ch])
                if stop < 3.9:
                    continue
                sc.op("scalar", lambda e, ch=ch, b=b: e.activation(
                    out=ysq[:, ch, :], in_=ycb[:, ch, :], func=AF.Square),
                    reads=["ycb%d" % ch], writes=["ysq%d" % ch])
            if stop < 5:
                return
            bm = nb()
            bq = nb()

            def stf(e, bm=bm, bq=bq):
                for ch in range(4):
                    e.matmul(PS[bm][:, :], lhsT=onesN[:], rhs=ycb[:, ch, :], start=(ch == 0), stop=(ch == 3))
                for ch in range(4):
                    ins = e.matmul(PS[bq][:, :], lhsT=onesN[:], rhs=ysq[:, ch, :], start=(ch == 0), stop=(ch == 3))
                return ins
            sc.op("tensor", stf, reads=["ycb%d" % c for c in range(4)] + ["ysq%d" % c for c in range(4)] + ["onesN"],
                  writes=["ps%d" % bm, "ps%d" % bq])
            sc.op("scalar", lambda e, bm=bm: e.copy(out=mean_sb[:], in_=PS[bm][:, :]), reads=["ps%d" % bm], writes=["mean"])
            sc.op("vector", lambda e: e.tensor_tensor(out=lrstd[:], in0=mean_sb[:], in1=mean_sb[:], op=ALU.mult),
                  reads=["mean"], writes=["lrstd"])
            sc.op("vector", lambda e, bq=bq: e.tensor_tensor(out=lrstd[:], in0=PS[bq][:, :], in1=lrstd[:], op=ALU.subtract),
                  reads=["ps%d" % bq, "lrstd"], writes=["lrstd"])
            sc.op("vector", lambda e: e.tensor_scalar(out=lrstd[:], in0=lrstd[:], scalar1=EPS, scalar2=None,
                                                      op0=ALU.add), reads=["lrstd"], writes=["lrstd"])
            sc.op("scalar", lambda e: e.activation(out=lrstd[:], in_=lrstd[:], func=AF.Sqrt), reads=["lrstd"], writes=["lrstd"])
            sc.op("vector", lambda e: e.reciprocal(out=lrstd[:], in_=lrstd[:]), reads=["lrstd"], writes=["lrstd"])
            for ch in range(4):
                z = zt[ch % 2]
                zk = "zt0"
                sc.op("gpsimd", lambda e, ch=ch, z=z: e.tensor_tensor(out=z[:], in0=ycb[:, ch, :], in1=mean_sb[:],
                                                                      op=ALU.subtract),
                      reads=["ycb%d" % ch, "mean"], writes=[zk])
                sc.op("vector", lambda e, z=z: e.tensor_tensor(out=z[:], in0=z[:], in1=lrstd[:], op=ALU.mult),
                      reads=[zk, "lrstd"], writes=[zk])
                sc.op("vector", lambda e, ch=ch, z=z: e.tensor_scalar(out=z[:], in0=z[:], scalar1=cvec[:, ch, 1:2],
                                                                      scalar2=cvec[:, ch, 2:3], op0=ALU.mult, op1=ALU.add),
                      reads=[zk, "cvec"], writes=[zk])
                sc.op("scalar", lambda e, ch=ch, z=z: e.activation(out=cvT[:, ch, :], in_=z[:], func=AF.Silu),
                      reads=[zk], writes=["cvT"])
            sc.dma("sync", "cvT_st", lambda e, s=s: e.dma_start(out=cvT_s.ap()[:, :, s * 512:(s + 1) * 512], in_=cvT[:]),
                   reads=["cvT"], writes=["cvT_s%d" % s])
            if stop < 6:
                return
            def qk_stage1(ci):
                which, h = divmod(ci, 4)
                b = fm_matmul(8 + which * 4 + h)
                i2 = ci % 2
                sc.op("scalar", lambda e: e.activation(out=sq[i2][:], in_=PS[b][:, :], func=AF.Square),
                      reads=["ps%d" % b], writes=["sq%d" % i2])
                return b

            def qk_stage2(ci, b):
                which, h = divmod(ci, 4)
                i2 = ci % 2
                b2 = nb()
                sc.op("tensor", lambda e: e.matmul(PS[b2][:, :], lhsT=blk[:], rhs=sq[i2][:], start=True, stop=True),
                      reads=["sq%d" % i2, "blk"], writes=["ps%d" % b2])
                sc.op("vector", lambda e: e.tensor_scalar(out=qr[i2][:], in0=PS[b2][:, :], scalar1=EPS, scalar2=None,
                                                          op0=ALU.add), reads=["ps%d" % b2], writes=["qr%d" % i2])
                sc.op("scalar", lambda e: e.activation(out=qr[i2][:], in_=qr[i2][:], func=AF.Sqrt),
                      reads=["qr%d" % i2], writes=["qr%d" % i2])
                sc.op("vector", lambda e: e.reciprocal(out=qr[i2][:], in_=qr[i2][:]), reads=["qr%d" % i2], writes=["qr%d" % i2])
                sc.op("vector", lambda e: e.scalar_tensor_tensor(
                    out=qkT[which][:, h, :], in0=PS[b][:, :], scalar=qkg[:, which:which + 1], in1=qr[i2][:],
                    op0=ALU.mult, op1=ALU.mult), reads=["ps%d" % b, "qr%d" % i2, "qkg"], writes=["qkT%d" % which])
                if h == 3:
                    dd = qT_s if which == 0 else kT_s
                    sc.dma("sync", "qk_st%d" % which, lambda e: e.dma_start(
                        out=dd.ap()[:, :, s * 512:(s + 1) * 512], in_=qkT[which][:]),
                        reads=["qkT%d" % which], writes=["qk_s%d_%d" % (which, s)])

            pb_ = qk_stage1(0)
            for ci in range(8):
                nb_ = qk_stage1(ci + 1) if ci + 1 < 8 else None
                qk_stage2(ci, pb_)
                pb_ = nb_
            if stop < 7:
                return
            for j in range(4):
                b = nb()

                def vf(e, j=j, b=b):
                    for kc in range(8):
                        ins = e.matmul(PS[b][:, :], lhsT=hT[:, kc, j * 128:(j + 1) * 128], rhs=w_bf[:, kc, 2048:2560],
                                       start=(kc == 0), stop=(kc == 7))
                    return ins
                sc.op("tensor", vf, reads=["hT%d" % j] + wkeys(4), writes=["ps%d" % b])
                sc.op("scalar" if j % 2 else "vector",
                      (lambda e, j=j, b=b: e.copy(out=V[:, s * 4 + j, :, 0:128],
                                                  in_=PS[b][:, :].rearrange("p (h e) -> p h e", h=4))) if j % 2 else
                      (lambda e, j=j, b=b: e.tensor_copy(out=V[:, s * 4 + j, :, 0:128],
                                                         in_=PS[b][:, :].rearrange("p (h e) -> p h e", h=4))),
                      reads=["ps%d" % b], writes=["V%d" % s])
        for s in range(G["nst"]):
            tile(s)
        G["xs_zero"](NE * CAP // 128)
        for en in sc.ENGS:
            sc.wait_all(en)


def load_cast(sc, nc, stage, stage_key, src_view, dst, ncols, kcn, dkey, scale=None):
    for cg in range(ncols // 128):
        sl = stage[cg % 2]
        key = stage_key + str(cg % 2)
        sc.dma("sync", key, lambda e, sl=sl, cg=cg: e.dma_start(out=sl[:, 0:kcn, :], in_=src_view[:, :, cg * 128:(cg + 1) * 128]),
               writes=[key])
        if scale is None:
            eng = ["vector", "gpsimd"][cg % 2]
            sc.op(eng, lambda e, sl=sl, cg=cg: e.tensor_copy(out=dst[:, :, cg * 128:(cg + 1) * 128], in_=sl[:, 0:kcn, :]),
                  reads=[key], writes=[dkey])
        else:
            for kc in range(kcn):
                if kc % 2 == 0:
                    sc.op("vector", lambda e, sl=sl, cg=cg, kc=kc: e.tensor_scalar(
                        out=dst[:, kc, cg * 128:(cg + 1) * 128], in0=sl[:, kc, :], scalar1=scale[:, kc:kc + 1],
                        scalar2=None, op0=ALU.mult), reads=[key, "g1T"], writes=[dkey])
                else:
                    sc.op("scalar", lambda e, sl=sl, cg=cg, kc=kc: e.activation(
                        out=dst[:, kc, cg * 128:(cg + 1) * 128], in_=sl[:, kc, :], func=AF.Copy,
                        scale=scale[:, kc:kc + 1]), reads=[key, "g1T"], writes=[dkey])


def phase_b(nc, sc, G):
    names = ["x_d", "w_in_d", "g1T_d", "w_co_d", "w_ao_d", "w_out_d", "g2bc_d", "lamv_d", "sgbc_d", "wr_d", "rbbc_d",
             "cst", "ident_b", "V", "PS", "PB", "qT_s", "cvT_s", "kT_s", "nhalf", "LG", "x1_s", "h2_s"]
    (x_d, w_in_d, g1T_d, w_co_d, w_ao_d, w_out_d, g2bc_d, lamv_d, sgbc_d, wr_d, rbbc_d,
     cst, ident_b, V, PS, PB, qT_s, cvT_s, kT_s, nhalf, LG, x1_s, h2_s) = (G[k] for k in names)
    nst = G["nst"]
    with contextlib.ExitStack() as st:
        sb = lambda name, shape, dt=F32: st.enter_context(nc.sbuf_tensor("sb_" + name, list(shape), dt))
        KT = sb("b_KT", [128, 4, S], BF16)
        wgt = sb("b_wgt", [128, 8, 2048], BF16)
        wco = sb("b_wco", [128, 4, D], BF16)
        wao = sb("b_wao", [128, 4, D], BF16)
        wout = sb("b_wout", [128, 8, D], BF16)
        wr = sb("b_wr", [128, 8, 36])
        stage_raw = [sb("b_stage%d" % i, [128, 1024]) for i in range(2)]
        stage = [t[:].rearrange("p (k c) -> p k c", k=8) for t in stage_raw]
        g1T = sb("b_g1T", [128, 8])
        g2bc = sb("b_g2bc", [128, D])
        lamv = sb("b_lamv", [128, 4, 64])
        sgbc = sb("b_sgbc", [128, 128])
        rbbc = sb("b_rbbc", [128, 36])
        tri_b = sb("b_tri", [128, 128], BF16)
        lam2 = sb("b_lam2", [128, 4])
        nlam = sb("b_nlam", [128, 1])
        xr = [sb("b_xr%d" % i, [128, D]) for i in range(2)]
        xm = sb("b_xm", [128, 4 * D], BF16)
        xn = xm[:].rearrange("p (j d) -> p j d", j=4)
        mT = xm[:].rearrange("p (k t) -> p k t", k=8)
        hT = sb("b_hT", [128, 8, 512], BF16)
        ss = sb("b_ss", [128, 8])
        rstd = sb("b_rstd", [128, 8])
        qT = sb("b_qT", [128, 4, 512], BF16)
        cvT = sb("b_cvT", [128, 4, 512], BF16)
        Et_t = sb("b_Et", [128, 4, 512], BF16)
        Et = [Et_t[:, i, :] for i in range(4)]
        aoT = sb("b_aoT", [128, 4, 512], BF16)
        Osb = sb("b_Osb", [128, 3, 3, 130])
        rr = sb("b_rr", [128, 4])
        t1 = sb("b_t1", [128, 128])
        ot = sb("b_ot", [128, 128])
        on = sb("b_on", [128, 128], BF16)
        junk = sb("b_junk", [128, 128], BF16)
        sgs = [stage_raw[0][:, 0:512], stage_raw[0][:, 512:1024]]
        tu = [stage_raw[1][:, 0:512], stage_raw[1][:, 512:1024]]
        h2 = sb("b_h2", [128, D])
        h2b = Et_t[:, 0:2, :].rearrange("p a b -> p (a b)")
        h2T = Osb[:].rearrange("p a b c -> p (a b c)")[:, 0:1024].rearrange("p (k t) -> p k t", k=8)

        sc.dma("sync", "b_kt", lambda e: e.dma_start(out=KT[:, :, 0:nst * 512], in_=kT_s.ap()[:, :, 0:nst * 512]),
               reads=["qk_s1_%d" % i for i in range(nst)], writes=["KT"])
        sc.dma("sync", "b_g1T", lambda e: e.dma_start(out=g1T[:], in_=g1T_d.ap()), writes=["g1T"])
        sc.dma("sync", "b_g2bc", lambda e: e.dma_start(out=g2bc[:], in_=g2bc_d.ap()), writes=["g2bc"])
        sc.dma("sync", "b_lamv", lambda e: e.dma_start(out=lamv[:], in_=lamv_d.ap()), writes=["lamv"])
        sc.dma("sync", "b_sgbc", lambda e: e.dma_start(out=sgbc[:], in_=sgbc_d.ap()), writes=["sgbc"])
        sc.dma("sync", "b_rbbc", lambda e: e.dma_start(out=rbbc[:], in_=rbbc_d.ap()), writes=["rbbc"])
        sc.dma("sync", "b_wr", lambda e: e.dma_start(out=wr[:], in_=wr_d.ap().rearrange("(kc p) n -> p kc n", p=128)),
               writes=["wr"])
        w_in_v = w_in_d.ap().rearrange("(kc p) n -> p kc n", p=128)[:, :, 2560:4608]
        load_cast(sc, nc, stage, "b_stage", w_in_v, wgt, 2048, 8, "wgt", scale=g1T)
        load_cast(sc, nc, stage, "b_stage", w_co_d.ap().rearrange("(kc p) n -> p kc n", p=128), wco, D, 4, "wco")
        load_cast(sc, nc, stage, "b_stage", w_ao_d.ap().rearrange("(kc p) n -> p kc n", p=128), wao, D, 4, "wao")
        load_cast(sc, nc, stage, "b_stage", w_out_d.ap().rearrange("(kc p) n -> p kc n", p=128), wout, D, 8, "wout")
        sc.op("vector", lambda e: e.tensor_copy(out=tri_b[:], in_=cst[:, 128:256]), reads=["cst"], writes=["tri_b"])
        sc.op("vector", lambda e: e.tensor_scalar(out=sgbc[:], in0=sgbc[:], scalar1=1.0 - LAM_INIT, scalar2=None,
                                                  op0=ALU.mult), reads=["sgbc"], writes=["sgbc"])
        sc.op("vector", lambda e: e.tensor_tensor(out=lamv[:, 0, :], in0=lamv[:, 0, :], in1=lamv[:, 1, :], op=ALU.mult),
              reads=["lamv"], writes=["lamv"])
        sc.op("vector", lambda e: e.tensor_tensor(out=lamv[:, 2, :], in0=lamv[:, 2, :], in1=lamv[:, 3, :], op=ALU.mult),
              reads=["lamv"], writes=["lamv"])
        sc.op("vector", lambda e: e.reduce_sum(out=lam2[:, 0:1], in_=lamv[:, 0, :], axis=AX.X), reads=["lamv"], writes=["lam2"])
        sc.op("vector", lambda e: e.reduce_sum(out=lam2[:, 1:2], in_=lamv[:, 2, :], axis=AX.X), reads=["lamv", "lam2"],
              writes=["lam2"])
        sc.op("scalar", lambda e: e.activation(out=lam2[:, 2:4], in_=lam2[:, 0:2], func=AF.Exp), reads=["lam2"], writes=["lam2"])
        sc.op("vector", lambda e: e.tensor_tensor(out=nlam[:], in0=lam2[:, 3:4], in1=lam2[:, 2:3], op=ALU.subtract),
              reads=["lam2"], writes=["nlam"])
        sc.op("vector", lambda e: e.tensor_scalar(out=nlam[:], in0=nlam[:], scalar1=-LAM_INIT, scalar2=None, op0=ALU.add),
              reads=["nlam"], writes=["nlam"])

        bstop = G.get("bstop", 99)
        x_v = x_d.ap().rearrange("(t p) d -> t p d", p=128)
        x1_v = x1_s.ap().rearrange("(t p) d -> t p d", p=128)
        h2_v = h2_s.ap().rearrange("(t p) d -> t p d", p=128)
        bank = [0]

        def nb():
            b = bank[0]
            bank[0] = (b + 1) % 6
            return b

        def tile(s):
            def load_q(s2):
                sc.dma("sync", "b_qT", lambda e: e.dma_start(out=qT[:], in_=qT_s.ap()[:, :, s2 * 512:(s2 + 1) * 512]),
                       reads=["qk_s0_%d" % s2], writes=["qT"])

            def load_cv(s2):
                sc.dma("sync", "b_cvT", lambda e: e.dma_start(out=cvT[:], in_=cvT_s.ap()[:, :, s2 * 512:(s2 + 1) * 512]),
                       reads=["cvT_s%d" % s2], writes=["cvT"])
            if s == 0:
                load_q(0)
                load_cv(0)
            for j in range(4):
                xj = xr[j % 2]
                xk = "xr%d" % (j % 2)
                sc.dma("sync", "b_" + xk, lambda e, j=j, xj=xj: e.dma_start(out=xj[:], in_=x_v[s * 4 + j]), writes=[xk])
                sc.op("vector", lambda e, j=j: e.memset(ss[:, j:j + 1], 0.0), writes=["ss%d" % j])
                sc.op("scalar", lambda e, j=j, xj=xj: e.activation(out=xn[:, j, :], in_=xj[:], func=AF.Square,
                                                                   scale=1.0 / 32.0, accum_out=ss[:, j:j + 1]),
                      reads=[xk, "ss%d" % j], writes=["xn%d" % j, "ss%d" % j])
                sc.op("vector", lambda e, j=j: e.tensor_scalar(out=rstd[:, j:j + 1], in0=ss[:, j:j + 1], scalar1=EPS,
                                                               scalar2=None, op0=ALU.add), reads=["ss%d" % j], writes=["rstd%d" % j])
                sc.op("gpsimd", lambda e, j=j: e.tensor_tensor(out=rstd[:, j:j + 1], in0=rstd[:, j:j + 1], in1=nhalf[:, 0:1],
                                                               op=ALU.pow), reads=["rstd%d" % j, "nhalf"], writes=["rstd%d" % j])
                sc.op("vector", lambda e, j=j, xj=xj: e.tensor_scalar(
                    out=xn[:, j, :], in0=xj[:], scalar1=rstd[:, j:j + 1], scalar2=None, op0=ALU.mult),
                    reads=[xk, "rstd%d" % j], writes=["xn%d" % j])
            if bstop < 3:
                return
            ei = [0]
            for h in range(4):
                nkb = 4 * s + 4
                items = [(c, kb) for c in range(2) for kb in range(nkb)]

                def emit_s(idx, h=h):
                    c, kb = items[idx]
                    r0, r1 = c * 64, (c + 1) * 64
                    jmin = max(0, kb - 4 * s)
                    q0 = jmin * 128
                    sb_ = 3 + (idx % 3)
                    sc.op("tensor", lambda e, kb=kb, q0=q0, sb_=sb_, r0=r0, r1=r1, h=h: e.matmul(
                        PS[sb_][:, q0:512], lhsT=KT[r0:r1, h, kb * 128:(kb + 1) * 128], rhs=qT[r0:r1, h, q0:512],
                        start=True, stop=True), reads=["KT", "qT"], writes=["ps%d" % sb_])
                    E = Et[ei[0] % 4]
                    ek = "E%d" % (ei[0] % 4)
                    ei[0] += 1
                    sc.op("scalar", lambda e, E=E, q0=q0, sb_=sb_: e.activation(
                        out=E[:, q0:512], in_=PS[sb_][:, q0:512], func=AF.Exp, scale=0.125),
                        reads=["ps%d" % sb_], writes=[ek])
                    if kb >= 4 * s:
                        sc.op("gpsimd", lambda e, E=E, q0=q0: e.tensor_tensor(
                            out=E[:, q0:q0 + 128], in0=E[:, q0:q0 + 128], in1=tri_b[:], op=ALU.mult),
                            reads=[ek, "tri_b"], writes=[ek])
                    return (E, ek, jmin)

                def emit_av(idx, st_, h=h):
                    c, kb = items[idx]
                    E, ek, jmin = st_

                    def av(e, c=c, kb=kb, jmin=jmin, E=E, h=h, s=s):
                        for j in range(jmin, 4):
                            g = c * 4 + j
                            ob = g // 3
                            o0 = (g % 3) * 160
                            ins = e.matmul(PS[ob][:, o0:o0 + 129], lhsT=E[:, j * 128:(j + 1) * 128],
                                           rhs=V[:, kb, h, 0:129], start=(kb == 0 and g % 3 == 0),
                                           stop=(kb == 4 * s + j), skip_group_check=True)
                        return ins
                    sc.op("tensor", av, reads=[ek, "V%d" % (kb // 4), "Vones"],
                          writes=["ps0", "ps1"] if c == 0 else ["ps1", "ps2"])

                pend = [emit_s(0), emit_s(1)]
                for idx in range(len(items)):
                    if idx + 2 < len(items):
                        pend.append(emit_s(idx + 2))
                    emit_av(idx, pend.pop(0))
                if bstop < 4:
                    continue
                for b in range(3):
                    src = lambda b=b: PS[b][:, 0:480].rearrange("p (r c) -> p r c", r=3)[:, :, 0:129]
                    if b % 2 == 0:
                        sc.op("scalar", lambda e, b=b, src=src: e.copy(out=Osb[:, b, :, 0:129], in_=src()),
                              reads=["ps%d" % b], writes=["Osb%d" % b])
                    else:
                        sc.op("vector", lambda e, b=b, src=src: e.tensor_copy(out=Osb[:, b, :, 0:129], in_=src()),
                              reads=["ps%d" % b], writes=["Osb%d" % b])
                for j in range(4):
                    b0, r_ = j // 3, j % 3
                    b1, r1_ = (4 + j) // 3, (4 + j) % 3
                    sc.op("vector", lambda e, b0=b0, r_=r_: e.reciprocal(out=rr[:, 0:1], in_=Osb[:, b0, r_, 128:129]),
                          reads=["Osb%d" % b0], writes=["rr"])
                    sc.op("vector", lambda e, b1=b1, r1_=r1_: e.reciprocal(out=rr[:, 1:2], in_=Osb[:, b1, r1_, 128:129]),
                          reads=["Osb%d" % b1, "rr"], writes=["rr"])
                    sc.op("vector", lambda e: e.tensor_tensor(out=rr[:, 1:2], in0=rr[:, 1:2], in1=nlam[:], op=ALU.mult),
                          reads=["rr", "nlam"], writes=["rr"])
                    sc.op("vector", lambda e, b0=b0, r_=r_: e.tensor_scalar(
                        out=t1[:], in0=Osb[:, b0, r_, 0:128], scalar1=rr[:, 0:1], scalar2=None, op0=ALU.mult),
                        reads=["Osb%d" % b0, "rr"], writes=["t1"])
                    sc.op("vector", lambda e, b1=b1, r1_=r1_: e.scalar_tensor_tensor(
                        out=ot[:], in0=Osb[:, b1, r1_, 0:128], scalar=rr[:, 1:2], in1=t1[:], op0=ALU.mult, op1=ALU.add),
                        reads=["Osb%d" % b1, "rr", "t1"], writes=["ot"])
                    sc.op("vector", lambda e: e.memset(rr[:, 2:3], 0.0), reads=["rr"], writes=["rr"])
                    sc.op("scalar", lambda e: e.activation(out=junk[:], in_=ot[:], func=AF.Square,
                                                           scale=float(128.0 ** -0.5), accum_out=rr[:, 2:3]),
                          reads=["ot", "rr"], writes=["junk", "rr"])
                    sc.op("vector", lambda e: e.tensor_scalar(out=rr[:, 3:4], in0=rr[:, 2:3], scalar1=EPS, scalar2=None,
                                                              op0=ALU.add), reads=["rr"], writes=["rr"])
                    sc.op("gpsimd", lambda e: e.tensor_tensor(out=rr[:, 3:4], in0=rr[:, 3:4], in1=nhalf[:, 0:1], op=ALU.pow),
                          reads=["rr", "nhalf"], writes=["rr"])
                    sc.op("vector", lambda e: e.scalar_tensor_tensor(
                        out=on[:], in0=ot[:], scalar=rr[:, 3:4], in1=sgbc[:], op0=ALU.mult, op1=ALU.mult),
                        reads=["ot", "rr", "sgbc"], writes=["on"])
                    pb = PB[j % 2]
                    pk = "pb%d" % (j % 2)
                    sc.op("tensor", lambda e, pb=pb: e.transpose(out=pb[:, 0:128], in_=on[:], identity=ident_b[:]),
                          reads=["on", "ident_b"], writes=[pk])
                    sc.op("scalar", lambda e, pb=pb, h=h, j=j: e.copy(out=aoT[:, h, j * 128:(j + 1) * 128], in_=pb[:, 0:128]),
                          reads=[pk], writes=["aoT"])

            if s + 1 < nst:
                load_q(s + 1)
            for j in range(4):
                pb = PB[j % 2]
                pk = "pb%d" % (j % 2)

                def tr(e, j=j, pb=pb):
                    for kc in range(8):
                        ins = e.transpose(out=pb[:, kc * 128:(kc + 1) * 128], in_=xn[:, j, kc * 128:(kc + 1) * 128],
                                          identity=ident_b[:])
                    return ins
                sc.op("tensor", tr, reads=["xn%d" % j, "ident_b"], writes=[pk])
                sc.op("vector", lambda e, j=j, pb=pb: e.tensor_copy(out=hT[:, :, j * 128:(j + 1) * 128],
                                                                   in_=pb[:].rearrange("p (k t) -> p k t", k=8)),
                      reads=[pk], writes=["hT%d" % j])
            hkeys = ["hT%d" % j for j in range(4)]

            if bstop < 5:
                return
            for m in range(8):
                for half in range(2):
                    bG, bP = nb(), nb()
                    wsrc, asrc, akey = (wco, cvT, "cvT") if half == 0 else (wao, aoT, "aoT")

                    def mm(e, m=m, half=half, bG=bG, bP=bP, wsrc=wsrc, asrc=asrc):
                        c0 = half * 1024 + m * 128
                        for kc in range(8):
                            e.matmul(PS[bG][:, :], lhsT=wgt[:, kc, c0:c0 + 128], rhs=hT[:, kc, :],
                                     start=(kc == 0), stop=(kc == 7))
                        for kc in range(4):
                            ins = e.matmul(PS[bP][:, :], lhsT=wsrc[:, kc, m * 128:(m + 1) * 128], rhs=asrc[:, kc, :],
                                           start=(kc == 0), stop=(kc == 3))
                        return ins
                    sc.op("tensor", mm, reads=hkeys + ["wgt", "wco", "wao", akey], writes=["ps%d" % bG, "ps%d" % bP])
                    sc.op("scalar", lambda e, bG=bG, half=half: e.activation(out=sgs[half][:], in_=PS[bG][:, :], func=AF.Sigmoid),
                          reads=["ps%d" % bG], writes=["sgs%d" % half])
                    sc.op("vector", lambda e, bP=bP, half=half: e.tensor_tensor(out=tu[half][:], in0=PS[bP][:, :],
                                                                                in1=sgs[half][:], op=ALU.mult),
                          reads=["ps%d" % bP, "sgs%d" % half], writes=["tu%d" % half])
                sc.op("gpsimd", lambda e, m=m: e.tensor_tensor(out=mT[:, m, :], in0=tu[0][:], in1=tu[1][:], op=ALU.add),
                      reads=["tu0", "tu1"], writes=["mT"])
            if s + 1 < nst:
                load_cv(s + 1)
            if bstop < 6:
                return
            for j in range(4):
                t = s * 4 + j
                xj = xr[j % 2]
                xk = "xr%d" % (j % 2)
                sc.dma("sync", "b_" + xk, lambda e, t=t, xj=xj: e.dma_start(out=xj[:], in_=x_v[t]), writes=[xk])
                for half in range(2):
                    b = nb()

                    def of(e, j=j, half=half, b=b):
                        for kc in range(8):
                            ins = e.matmul(PS[b][:, :], lhsT=mT[:, kc, j * 128:(j + 1) * 128],
                                           rhs=wout[:, kc, half * 512:(half + 1) * 512], start=(kc == 0), stop=(kc == 7))
                        return ins
                    sc.op("tensor", of, reads=["mT", "wout"], writes=["ps%d" % b])
                    sc.op("vector", lambda e, xj=xj, half=half, b=b: e.tensor_tensor(
                        out=xj[:, half * 512:(half + 1) * 512], in0=PS[b][:, :], in1=xj[:, half * 512:(half + 1) * 512],
                        op=ALU.add), reads=["ps%d" % b, xk], writes=[xk])
                sc.dma("sync", "b_x1st%d" % (j % 2), lambda e, t=t, xj=xj: e.dma_start(out=x1_v[t], in_=xj[:]), reads=[xk],
                       writes=["x1_s%d" % t])
                if bstop < 7:
                    continue
                sc.op("vector", lambda e, j=j: e.memset(ss[:, 4 + j:5 + j], 0.0), writes=["ss%d" % (4 + j)])
                sc.op("scalar", lambda e, j=j, xj=xj: e.activation(out=h2b, in_=xj[:], func=AF.Square,
                                                                   scale=1.0 / 32.0, accum_out=ss[:, 4 + j:5 + j]),
                      reads=[xk, "ss%d" % (4 + j)], writes=["h2b", "E0", "E1", "ss%d" % (4 + j)])
                sc.op("vector", lambda e, j=j: e.tensor_scalar(out=rstd[:, 4 + j:5 + j], in0=ss[:, 4 + j:5 + j], scalar1=EPS,
                                                               scalar2=None, op0=ALU.add),
                      reads=["ss%d" % (4 + j)], writes=["rstd%d" % (4 + j)])
                sc.op("gpsimd", lambda e, j=j: e.tensor_tensor(out=rstd[:, 4 + j:5 + j], in0=rstd[:, 4 + j:5 + j],
                                                               in1=nhalf[:, 0:1], op=ALU.pow),
                      reads=["rstd%d" % (4 + j), "nhalf"], writes=["rstd%d" % (4 + j)])
                sc.op("vector", lambda e, j=j, xj=xj: e.scalar_tensor_tensor(
                    out=h2[:], in0=xj[:], scalar=rstd[:, 4 + j:5 + j], in1=g2bc[:], op0=ALU.mult, op1=ALU.mult),
                    reads=[xk, "rstd%d" % (4 + j), "g2bc"], writes=["h2"])
                sc.op("gpsimd", lambda e: e.tensor_copy(out=h2b, in_=h2[:]), reads=["h2"], writes=["h2b", "E0", "E1"])
                sc.dma("sync", "b_h2st", lambda e, t=t: e.dma_start(out=h2_v[t], in_=h2b), reads=["h2b", "E0", "E1"],
                       writes=["h2_s%d" % t])
                if bstop < 7.5:
                    continue
                for half in range(2):
                    b = nb()

                    def trf(e, half=half, b=b):
                        for k4 in range(4):
                            kc = half * 4 + k4
                            ins = e.transpose(out=PS[b][:, k4 * 128:(k4 + 1) * 128], in_=h2[:, kc * 128:(kc + 1) * 128],
                                              identity=cst[:, 0:128])
                        return ins
                    sc.op("tensor", trf, reads=["h2", "cst"], writes=["ps%d" % b])
                    sc.op("scalar" if half else "vector",
                          (lambda e, half=half, b=b: e.copy(out=h2T[:, half * 4:(half + 1) * 4, :],
                                                            in_=PS[b][:, :].rearrange("p (k t) -> p k t", k=4))) if half else
                          (lambda e, half=half, b=b: e.tensor_copy(out=h2T[:, half * 4:(half + 1) * 4, :],
                                                                   in_=PS[b][:, :].rearrange("p (k t) -> p k t", k=4))),
                          reads=["ps%d" % b], writes=["h2T%d" % half])
                if bstop < 7.8:
                    continue
                b = nb()

                def rf(e, b=b):
                    for kc in range(8):
                        ins = e.matmul(PS[b][:, 0:36], lhsT=h2T[:, kc, :], rhs=wr[:, kc, :], start=(kc == 0), stop=(kc == 7))
                    return ins
                sc.op("tensor", rf, reads=["h2T0", "h2T1", "wr"], writes=["ps%d" % b])
                sc.op("vector", lambda e, t=t, b=b: e.tensor_tensor(out=LG[:, t, :], in0=PS[b][:, 0:36], in1=rbbc[:],
                                                                    op=ALU.add),
                      reads=["ps%d" % b, "rbbc"], writes=["LG%d" % t])

        for s in range(nst if bstop >= 2 else 0):
            tile(s)
        for en in sc.ENGS:
            sc.wait_all(en)


def phase_c(nc, sc, G):
    names = ["LG", "dest_i", "wts", "cst", "ident_b", "PS", "PB", "eoff_d", "h2_s", "XS", "YS", "w_g_d", "w_u_d", "w_d_d"]
    LG, dest_i, wts, cst, ident_b, PS, PB, eoff_d, h2_s, XS, YS, w_g_d, w_u_d, w_d_d = (G[k] for k in names)
    ntile = G["nst"] * 4
    NR = NE * CAP
    with contextlib.ExitStack() as st:
        sb = lambda name, shape, dt=F32: st.enter_context(nc.sbuf_tensor("sb_" + name, list(shape), dt))
        xb = [sb("c_xb%d" % i, [128, D], BF16) for i in range(2)]
        U_b = sb("c_Ub", [128, 128], BF16)
        ones_b = sb("c_onesb", [128, 128], BF16)
        eoff = sb("c_eoff", [128, NE])
        with contextlib.ExitStack() as st2:
            sb2 = lambda name, shape, dt=F32: st2.enter_context(nc.sbuf_tensor("sb_" + name, list(shape), dt))
            GLd = sb2("r_GLd", [128, 32, 4])
            ohg = sb2("r_ohg", [128, 32, 4])
            pen = sb2("r_pen", [128, 32, 4])
            sm = sb2("r_sm", [128, 8, 32])
            ELm = sb2("r_ELm", [128, 32, 4, 8])
            oh1 = sb2("r_oh1", [128, 32, 32])
            EL2 = sb2("r_EL2", [128, 32, 32])
            oh2 = sb2("r_oh2", [128, 32, 32])
            Mb = sb2("r_Mb", [128, 32, 32], BF16)
            pre = sb2("r_pre", [128, 32, 32])
            tot = sb2("r_tot", [128, 32, 32])
            base = sb2("r_base", [128, 32, 32])
            prod = sb2("r_prod", [128, 32, 32])
            destf = sb2("r_destf", [128, 2, 32])
            ELm3 = ELm[:].rearrange("p t g j -> p t (g j)")
            gmax, gsum, gw, m1, m2, dm, rsel, esel = (sm[:, i, :] for i in range(8))
            bc4 = lambda a: a.unsqueeze(2).broadcast_to([128, 32, 4])
            bc32 = lambda a: a.unsqueeze(2).broadcast_to([128, 32, 32])
            LK = ["LG%d" % i for i in range(32)]
            V_ = "vector"
            sc.dma("sync", "c_eoff", lambda e: e.dma_start(out=eoff[:], in_=eoff_d.ap()), writes=["eoff"])
            sc.op(V_, lambda e: e.tensor_copy(out=U_b[:], in_=cst[:, 256:384]), reads=["cst"], writes=["U_b"])
            sc.op(V_, lambda e: e.memset(ones_b[:], 1.0), writes=["ones_b"])
            sc.op(V_, lambda e: e.reduce_max(out=gmax, in_=LG[:, :, 0:4], axis=AX.X), reads=LK, writes=["sm"])
            sc.op(V_, lambda e: e.tensor_tensor(out=GLd[:], in0=LG[:, :, 0:4], in1=bc4(gmax), op=ALU.subtract),
                  reads=LK + ["sm"], writes=["GLd"])
            sc.op(V_, lambda e: e.tensor_scalar(out=ohg[:], in0=GLd[:], scalar1=0.0, scalar2=None, op0=ALU.is_equal),
                  reads=["GLd"], writes=["ohg"])
            sc.op("scalar", lambda e: e.activation(out=GLd[:], in_=GLd[:], func=AF.Exp), reads=["GLd", "ohg"], writes=["GLd"])
            sc.op(V_, lambda e: e.reduce_sum(out=gsum, in_=GLd[:], axis=AX.X), reads=["GLd", "sm"], writes=["sm"])
            sc.op(V_, lambda e: e.reciprocal(out=gw, in_=gsum), reads=["sm"], writes=["sm"])
            sc.op(V_, lambda e: e.tensor_scalar(out=pen[:], in0=ohg[:], scalar1=-1.0, scalar2=1e30, op0=ALU.add, op1=ALU.mult),
                  reads=["ohg"], writes=["pen"])
            sc.op(V_, lambda e: e.tensor_tensor(out=ELm[:], in0=LG[:, :, 4:36].rearrange("p t (g j) -> p t g j", g=4),
                                                in1=pen[:].unsqueeze(3).broadcast_to([128, 32, 4, 8]), op=ALU.add),
                  reads=LK + ["pen"], writes=["ELm"])
            sc.op(V_, lambda e: e.reduce_max(out=m1, in_=ELm3, axis=AX.X), reads=["ELm", "sm"], writes=["sm"])
            sc.op(V_, lambda e: e.tensor_tensor(out=oh1[:], in0=ELm3, in1=bc32(m1), op=ALU.is_equal),
                  reads=["ELm", "sm"], writes=["oh1"])
            sc.op(V_, lambda e: e.scalar_tensor_tensor(out=EL2[:], in0=oh1[:], scalar=-1e30, in1=ELm3, op0=ALU.mult,
                                                       op1=ALU.add), reads=["oh1", "ELm"], writes=["EL2"])
            sc.op(V_, lambda e: e.reduce_max(out=m2, in_=EL2[:], axis=AX.X), reads=["EL2", "sm"], writes=["sm"])
            sc.op(V_, lambda e: e.tensor_tensor(out=oh2[:], in0=EL2[:], in1=bc32(m2), op=ALU.is_equal),
                  reads=["EL2", "sm"], writes=["oh2"])
            sc.op(V_, lambda e: e.tensor_tensor(out=dm, in0=m2, in1=m1, op=ALU.subtract), reads=["sm"], writes=["sm"])
            sc.op("scalar", lambda e: e.activation(out=dm, in_=dm, func=AF.Exp), reads=["sm"], writes=["sm"])
            sc.op(V_, lambda e: e.tensor_scalar(out=dm, in0=dm, scalar1=1.0, scalar2=None, op0=ALU.add), reads=["sm"], writes=["sm"])
            sc.op(V_, lambda e: e.reciprocal(out=dm, in_=dm), reads=["sm"], writes=["sm"])
            sc.op(V_, lambda e: e.tensor_tensor(out=wts[:, 0, :], in0=gw, in1=dm, op=ALU.mult), reads=["sm"], writes=["wts"])
            sc.op(V_, lambda e: e.tensor_tensor(out=wts[:, 1, :], in0=gw, in1=wts[:, 0, :], op=ALU.subtract),
                  reads=["sm", "wts"], writes=["wts"])
            sc.op(V_, lambda e: e.tensor_tensor(out=Mb[:], in0=oh1[:], in1=oh2[:], op=ALU.add), reads=["oh1", "oh2"], writes=["Mb"])
            for half in range(2):
                rhs = Mb[:, half * 16:(half + 1) * 16, :].rearrange("p t e -> p (t e)")
                sc.op("tensor", lambda e, half=half, rhs=rhs: e.matmul(PS[half][:, :], lhsT=U_b[:], rhs=rhs, start=True, stop=True),
                      reads=["Mb", "U_b"], writes=["ps%d" % half])
                sc.op("tensor", lambda e, half=half, rhs=rhs: e.matmul(PS[2 + half][:, :], lhsT=ones_b[:], rhs=rhs, start=True,
                                                                       stop=True),
                      reads=["Mb", "ones_b"], writes=["ps%d" % (2 + half)])
                sc.op(V_, lambda e, half=half: e.tensor_copy(
                    out=pre[:, half * 16:(half + 1) * 16, :].rearrange("p t e -> p (t e)"), in_=PS[half][:, :]),
                    reads=["ps%d" % half], writes=["pre"])
                sc.op("scalar", lambda e, half=half: e.copy(
                    out=tot[:, half * 16:(half + 1) * 16, :].rearrange("p t e -> p (t e)"), in_=PS[2 + half][:, :]),
                    reads=["ps%d" % (2 + half)], writes=["tot"])
            sc.op(V_, lambda e: e.memset(base[:, 0, :], 0.0), writes=["base"])
            for t in range(1, 32):
                sc.op(V_, lambda e, t=t: e.tensor_tensor(out=base[:, t, :], in0=base[:, t - 1, :], in1=tot[:, t - 1, :],
                                                         op=ALU.add), reads=["base", "tot"], writes=["base"])
            sc.op(V_, lambda e: e.tensor_tensor(out=pre[:], in0=pre[:], in1=base[:], op=ALU.add), reads=["pre", "base"],
                  writes=["pre"])
            for k, oh in enumerate([oh1, oh2]):
                ohk = "oh%d" % (k + 1)
                sc.op(V_, lambda e, oh=oh: e.tensor_tensor(out=prod[:], in0=oh[:], in1=pre[:], op=ALU.mult),
                      reads=[ohk, "pre"], writes=["prod"])
                sc.op(V_, lambda e: e.reduce_sum(out=rsel, in_=prod[:], axis=AX.X), reads=["prod", "sm"], writes=["sm"])
                sc.op(V_, lambda e, oh=oh: e.tensor_tensor(out=prod[:], in0=oh[:],
                                                           in1=eoff[:].unsqueeze(1).broadcast_to([128, 32, 32]), op=ALU.mult),
                      reads=[ohk, "eoff", "sm"], writes=["prod"])
                sc.op(V_, lambda e: e.reduce_sum(out=esel, in_=prod[:], axis=AX.X), reads=["prod", "sm"], writes=["sm"])
                sc.op(V_, lambda e, k=k: e.tensor_tensor(out=destf[:, k, :], in0=rsel, in1=esel, op=ALU.add),
                      reads=["sm"], writes=["destf"])
                sc.op(V_, lambda e: e.tensor_scalar(out=rsel, in0=rsel, scalar1=float(CAP), scalar2=1e6, op0=ALU.is_ge,
                                                    op1=ALU.mult), reads=["sm", "destf"], writes=["sm"])
                sc.op(V_, lambda e, k=k: e.tensor_tensor(out=destf[:, k, :], in0=destf[:, k, :], in1=rsel, op=ALU.add),
                      reads=["sm", "destf"], writes=["destf"])
            sc.op(V_, lambda e: e.tensor_copy(out=dest_i[:], in_=destf[:]), reads=["destf"], writes=["dest"])
            if G.get("dbg"):
                rt_o = G["rt_o"]
                sc.dma("sync", "dbg3", lambda e: e.dma_start(out=rt_o.ap()[:, 0:2, :], in_=destf[:]), reads=["destf", "dest"],
                       writes=["rt_o"])
                sc.dma("sync", "dbg4", lambda e: e.dma_start(out=rt_o.ap()[:, 2:4, :], in_=wts[:]), reads=["wts"], writes=["rt_o2"])
            for en in sc.ENGS:
                sc.wait_all(en)

        stage_raw = [sb("c_stage%d" % i, [128, 4096]) for i in range(3)]
        wg = [sb("c_wg%d" % i, [128, 8, 512], BF16) for i in range(2)]
        wu = [sb("c_wu%d" % i, [128, 8, 512], BF16) for i in range(2)]
        wd = [sb("c_wd%d" % i, [128, 4, D], BF16) for i in range(2)]
        xb4 = [sb("c_xb4_%d" % i, [128, CAP // 128, D], BF16) for i in range(2)]
        xbT4 = [sb("c_xbT4_%d" % i, [128, 8, CAP], BF16) for i in range(2)]
        sgt2 = [sb("c_sgt%d" % i, [128, 512]) for i in range(2)]
        hidT4 = [sb("c_hid4_%d" % i, [128, 4, CAP], BF16) for i in range(2)]
        yo = [sb("c_yo%d" % i, [128, D]) for i in range(2)]
        h2_v = h2_s.ap().rearrange("(t p) d -> t p d", p=128)
        for t in range(ntile):
            x_ = xb[t % 2]
            xk = "xb%d" % (t % 2)
            sc.dma("sync", "c_" + xk, lambda e, t=t, x_=x_: e.dma_start(out=x_[:], in_=h2_v[t]),
                   reads=["h2_s%d" % t], writes=[xk])
            for k in range(2):
                sc.dma("gpsimd", "c_sc%d_%d" % (t % 2, k), lambda e, t=t, k=k, x_=x_: e.indirect_dma_start(
                    out=XS.ap(), out_offset=bass.IndirectOffsetOnAxis(ap=dest_i[:, k, t:t + 1], axis=0),
                    in_=x_[:], in_offset=None, bounds_check=G["bnd"]["r"], oob_is_err=False),
                    reads=[xk, "dest", "XS"], writes=["XSk%d" % k])

        nexp = G.get("nexp", NE)
        sg_v = stage_raw[0][:].rearrange("p (k n) -> p k n", k=8)
        su_v = stage_raw[1][:].rearrange("p (k n) -> p k n", k=8)
        sd_v = stage_raw[2][:].rearrange("p (k n) -> p k n", k=4)

        def load_w(ex):
            sc.dma("sync", "c_st0", lambda e: e.dma_start(out=sg_v, in_=w_g_d.ap()[ex].rearrange("(kc p) n -> p kc n", p=128)),
                   writes=["st0"])
            sc.dma("sync", "c_st1", lambda e: e.dma_start(out=su_v, in_=w_u_d.ap()[ex].rearrange("(kc p) n -> p kc n", p=128)),
                   writes=["st1"])
            sc.dma("sync", "c_st2", lambda e: e.dma_start(out=sd_v, in_=w_d_d.ap()[ex].rearrange("(kc p) n -> p kc n", p=128)),
                   writes=["st2"])

        def cast_w(ex):
            p2 = ex % 2
            sc.op("scalar", lambda e: e.copy(out=wg[p2][:], in_=sg_v), reads=["st0"], writes=["wg%d" % p2])
            sc.op("vector", lambda e: e.tensor_copy(out=wu[p2][:], in_=su_v), reads=["st1"], writes=["wu%d" % p2])
            sc.op("vector", lambda e: e.tensor_copy(out=wd[p2][:], in_=sd_v), reads=["st2"], writes=["wd%d" % p2])

        def load_xb(ex):
            p2 = ex % 2
            sc.dma("sync", "c_xb4_%d" % p2, lambda e: e.dma_start(
                out=xb4[p2][:], in_=XS.ap()[ex * CAP:(ex + 1) * CAP, :].rearrange("(r p) d -> p r d", p=128)),
                reads=["XS", "XSk0", "XSk1"], writes=["xb4_%d" % p2])

        load_xb(0)
        load_w(0)
        for ex in range(nexp):
            p2 = ex % 2
            cast_w(ex)
            if ex + 1 < nexp:
                load_xb(ex + 1)
                load_w(ex + 1)
            NB = CAP // 128
            for r in range(NB):
                pb = PB[r % 2]
                pk = "pb%d" % (r % 2)

                def tr(e, r=r, pb=pb, p2=p2):
                    for kc in range(8):
                        ins = e.transpose(out=pb[:, kc * 128:(kc + 1) * 128], in_=xb4[p2][:, r, kc * 128:(kc + 1) * 128],
                                          identity=ident_b[:])
                    return ins
                sc.op("tensor", tr, reads=["xb4_%d" % p2, "ident_b"], writes=[pk])
                sc.op("vector" if r % 2 else "scalar",
                      (lambda e, r=r, pb=pb, p2=p2: e.tensor_copy(out=xbT4[p2][:, :, r * 128:(r + 1) * 128],
                                                                 in_=pb[:].rearrange("p (k t) -> p k t", k=8))) if r % 2 else
                      (lambda e, r=r, pb=pb, p2=p2: e.copy(out=xbT4[p2][:, :, r * 128:(r + 1) * 128],
                                                           in_=pb[:].rearrange("p (k t) -> p k t", k=8))),
                      reads=[pk], writes=["xbT4_%d_%d" % (p2, r)])
            xkeys = ["xbT4_%d_%d" % (p2, r) for r in range(NB)]
            for f in range(4):
                bg, bu = (0, 1) if f % 2 == 0 else (2, 3)

                def gu(e, p2=p2, f=f, bg=bg, bu=bu):
                    for (bb, w) in ((bg, wg[p2]), (bu, wu[p2])):
                        for kc in range(8):
                            ins = e.matmul(PS[bb][:, 0:CAP], lhsT=w[:, kc, f * 128:(f + 1) * 128], rhs=xbT4[p2][:, kc, :],
                                           start=(kc == 0), stop=(kc == 7))
                    return ins
                sc.op("tensor", gu, reads=xkeys + ["wg%d" % p2, "wu%d" % p2], writes=["ps%d" % bg, "ps%d" % bu])
                sg_ = sgt2[f % 2]
                sc.op("scalar", lambda e, bg=bg, sg_=sg_: e.activation(out=sg_[:, 0:CAP], in_=PS[bg][:, 0:CAP], func=AF.Silu),
                      reads=["ps%d" % bg], writes=["sgt%d" % (f % 2)])
                sc.op("vector", lambda e, bu=bu, sg_=sg_, f=f, p2=p2: e.tensor_tensor(
                    out=hidT4[p2][:, f, :], in0=PS[bu][:, 0:CAP], in1=sg_[:, 0:CAP], op=ALU.mult),
                    reads=["ps%d" % bu, "sgt%d" % (f % 2)], writes=["hid4_%d" % p2])
            for r in range(NB):
                row0 = ex * CAP + r * 128
                i2 = r % 2
                for half in range(2):
                    bd = 4 + half

                    def dn(e, half=half, bd=bd, r=r, p2=p2):
                        for fc in range(4):
                            ins = e.matmul(PS[bd][:, :], lhsT=hidT4[p2][:, fc, r * 128:(r + 1) * 128],
                                           rhs=wd[p2][:, fc, half * 512:(half + 1) * 512], start=(fc == 0), stop=(fc == 3))
                        return ins
                    sc.op("tensor", dn, reads=["hid4_%d" % p2, "wd%d" % p2], writes=["ps%d" % bd])
                    if half == 0:
                        sc.op("scalar", lambda e, i2=i2, bd=bd: e.copy(out=yo[i2][:, 0:512], in_=PS[bd][:, :]),
                              reads=["ps%d" % bd], writes=["yo%d" % i2])
                    else:
                        sc.op("vector", lambda e, i2=i2, bd=bd: e.tensor_copy(out=yo[i2][:, 512:1024], in_=PS[bd][:, :]),
                              reads=["ps%d" % bd], writes=["yo%d" % i2])
                sc.dma("sync", "c_yo%d" % i2, lambda e, row0=row0, i2=i2: e.dma_start(out=YS.ap()[row0:row0 + 128, :], in_=yo[i2][:]),
                       reads=["yo%d" % i2], writes=["YS"])
        for en in sc.ENGS:
            sc.wait_all(en)


def phase_d(nc, sc, G):
    names = ["dest_i", "wts", "x1_s", "YS", "out_d"]
    dest_i, wts, x1_s, YS, out_d = (G[k] for k in names)
    ntile = G["nst"] * 4
    NBUF = 4
    with contextlib.ExitStack() as st:
        sb = lambda name, shape, dt=F32: st.enter_context(nc.sbuf_tensor("sb_" + name, list(shape), dt))
        x1t = [sb("d_x1%d" % i, [128, D]) for i in range(NBUF)]
        y1 = [sb("d_y1%d" % i, [128, D]) for i in range(NBUF)]
        y2 = [sb("d_y2%d" % i, [128, D]) for i in range(NBUF)]
        x1_v = x1_s.ap().rearrange("(t p) d -> t p d", p=128)
        out_v = out_d.ap().rearrange("(t p) d -> t p d", p=128)

        def prep(t):
            i2 = t % NBUF
            sc.dma("sync", "d_x1%d" % i2, lambda e: e.dma_start(out=x1t[i2][:], in_=x1_v[t]),
                   reads=["x1_s%d" % t], writes=["dx1%d" % i2])
            for k, yt in enumerate((y1, y2)):
                yk = "dy%d_%d" % (k, i2)
                sc.op("vector", lambda e, yt=yt: e.memset(yt[i2][:], 0.0), writes=[yk])
                sc.dma("gpsimd", "d_g%d_%d" % (k, i2), lambda e, yt=yt, k=k: e.indirect_dma_start(
                    out=yt[i2][:], out_offset=None, in_=YS.ap(),
                    in_offset=bass.IndirectOffsetOnAxis(ap=dest_i[:, k, t:t + 1], axis=0),
                    bounds_check=G["bnd"]["r"], oob_is_err=False), reads=["YS", "dest"], writes=[yk])

        def finish(t):
            i2 = t % NBUF
            sc.op("vector", lambda e: e.scalar_tensor_tensor(
                out=x1t[i2][:], in0=y1[i2][:], scalar=wts[:, 0, t:t + 1], in1=x1t[i2][:], op0=ALU.mult, op1=ALU.add),
                reads=["dy0_%d" % i2, "wts", "dx1%d" % i2], writes=["dx1%d" % i2])
            sc.op("vector", lambda e: e.scalar_tensor_tensor(
                out=x1t[i2][:], in0=y2[i2][:], scalar=wts[:, 1, t:t + 1], in1=x1t[i2][:], op0=ALU.mult, op1=ALU.add),
                reads=["dy1_%d" % i2, "wts", "dx1%d" % i2], writes=["dx1%d" % i2])
            sc.dma("sync", "d_o%d" % i2, lambda e: e.dma_start(out=out_v[t], in_=x1t[i2][:]),
                   reads=["dx1%d" % i2], writes=["out%d" % t])

        AHEAD = 2
        for t in range(min(AHEAD, ntile)):
            prep(t)
        for t in range(ntile):
            if t + AHEAD < ntile:
                prep(t + AHEAD)
            finish(t)
        for en in sc.ENGS:
            sc.wait_all(en)


def _consts():
    c = np.zeros((128, 512), np.float32)
    c[:, 0:128] = np.eye(128, dtype=np.float32)
    k = np.arange(128)[:, None]
    q = np.arange(128)[None, :]
    c[:, 128:256] = (k <= q).astype(np.float32)
    c[:, 256:384] = (k < q).astype(np.float32)
    c[:, 384:512] = ((k // 64) == (q // 64)).astype(np.float32)
    return c


def make_in_maps(inputs, cores):
    f = lambda a: np.ascontiguousarray(np.asarray(a, dtype=np.float32))
    w_in = f(inputs["w_in"][0])
    g1T = f(inputs["attn_norm_g"][0].reshape(8, 128).T)
    cw = f(inputs["conv_dw_w"][0].reshape(31, 4, 128).transpose(2, 1, 0))
    cvec = f(np.stack([inputs["conv_dw_b"][0].reshape(4, 128).T, inputs["conv_ln_g"][0].reshape(4, 128).T,
                       inputs["conv_ln_b"][0].reshape(4, 128).T], axis=-1))
    qkg = f(np.stack([np.tile(inputs["q_norm_g"][0], 2), np.tile(inputs["k_norm_g"][0], 2)], axis=-1))
    cst = _consts()
    rep = lambda v: f(np.broadcast_to(np.asarray(v, np.float32)[None], (128,) + tuple(np.shape(v))))
    shared = {
        "w_in": w_in, "cst": cst, "g1T": g1T, "cw": cw, "cvec": cvec, "qkg": qkg,
        "w_co": f(inputs["w_conv_out"][0]), "w_ao": f(inputs["w_attn_out"][0]), "w_out": f(inputs["w_out"][0]),
        "g2bc": rep(inputs["ffn_norm_g"][0]),
        "lamv": rep(np.stack([inputs["lambda_q1"][0], inputs["lambda_k1"][0], inputs["lambda_q2"][0], inputs["lambda_k2"][0]])),
        "sgbc": rep(inputs["subln_g"][0]),
        "wr": f(np.concatenate([inputs["w_router_group"][0], inputs["w_router_expert"][0]], axis=1)),
        "rbbc": rep(np.concatenate([inputs["b_router_group"][0], inputs["b_router_expert"][0]])),
        "eoff": rep(np.arange(NE, dtype=np.float32) * CAP),
        "w_g": f(inputs["w_gate_e"][0]), "w_u": f(inputs["w_up_e"][0]), "w_d": f(inputs["w_down_e"][0]),
    }
    maps = []
    for c in cores:
        m = dict(shared)
        m["x"] = f(inputs["x"][c])
        maps.append(m)
    return maps


def kernel(**inputs):
    nc = build_program()
    maps = make_in_maps(inputs, list(range(8)))
    res = run_bass_kernel_spmd(nc, maps, core_ids=list(range(8)))
    return np.stack([np.asarray(r["out"]).reshape(S, D) for r in res.results], axis=0).astype(np.float32)
```
